# Optimizing a Trainium2 kernel written in Bass

```python
import math
import jax, jax.numpy as jnp
from jax import lax
import numpy as np

D_MODEL = 1024
BATCH = 16
SEQ = 4096
DEPTH = 2

D_PLE = 256
D_FF = 2816
D_CONV = 512
CONV_W = 3
H_M = 4
DQK_M = 128
DV_M = 256
D_MV = H_M * DV_M
CHUNK = 64
H_A = 8
DH_A = 64
H_IDX = 4
D_IDX = 64
TOPK_MAX = 256
Q_BLOCK = 128
EPS = 1e-6

IN_SPLITS = (
    D_CONV, D_CONV, D_CONV,
    H_M * DQK_M, H_M * DQK_M, D_MV, D_MV,
    H_M, H_M,
    H_A * DH_A, DH_A, DH_A,
    H_IDX * D_IDX, D_IDX, H_IDX,
    3 * D_MODEL,
)
N_IN = sum(IN_SPLITS)
IN_OFFSETS = tuple(int(o) for o in np.cumsum(IN_SPLITS)[:-1])

kernel_name = "hybrid_conv_mlstm_dsa_macaron"


def rms_norm(x, g):
    xf = x.astype(jnp.float32)
    y = xf * lax.rsqrt(jnp.mean(xf * xf, axis=-1, keepdims=True) + EPS)
    return (y * g.astype(jnp.float32)).astype(x.dtype)


def swiglu(h, w_gu, w_down):
    g, u = jnp.split(h @ w_gu, 2, axis=-1)
    return (jax.nn.silu(g) * u) @ w_down


def short_conv_mixer(b_gate, c_gate, xc, conv_w, w_out):
    u = c_gate * xc
    S = u.shape[1]
    up = jnp.pad(u, ((0, 0), (CONV_W - 1, 0), (0, 0)))
    y = up[:, 0:S] * conv_w[0]
    for j in range(1, CONV_W):
        y = y + up[:, j:j + S] * conv_w[j]
    return (b_gate * y) @ w_out


def mlstm_chunkwise(q, k, v, i_pre, f_pre):
    B, S, H, dqk = q.shape
    dv = v.shape[-1]
    nc = S // CHUNK
    f32 = jnp.float32
    qf = q.astype(f32)
    kf = k.astype(f32) * (dqk ** -0.5)
    vf = v.astype(f32)
    ig = i_pre.astype(f32)
    lf = jax.nn.log_sigmoid(f_pre.astype(f32))

    def to_chunks(a):
        a = a.reshape((B, nc, CHUNK, H) + a.shape[3:])
        return jnp.moveaxis(a, (1, 3), (0, 2))

    causal = jnp.tril(jnp.ones((CHUNK, CHUNK), dtype=bool))

    def step(carry, inp):
        C, n, m = carry
        qc, kc, vc, ic, lfc = inp
        b = jnp.cumsum(lfc, axis=-1)
        Dlog = jnp.where(causal, b[..., :, None] - b[..., None, :] + ic[..., None, :], -jnp.inf)
        inter = b + m[..., None]
        m_t = jnp.maximum(inter, jnp.max(Dlog, axis=-1))
        sc = jnp.einsum('bhtd,bhsd->bhts', qc, kc) * jnp.exp(Dlog - m_t[..., None])
        w_inter = jnp.exp(inter - m_t)
        num = w_inter[..., None] * jnp.einsum('bhvd,bhtd->bhtv', C, qc) + jnp.einsum('bhts,bhsv->bhtv', sc, vc)
        den = w_inter * jnp.einsum('bhd,bhtd->bht', n, qc) + jnp.sum(sc, axis=-1)
        h = num / jnp.maximum(jnp.abs(den), jnp.exp(-m_t))[..., None]
        bL = b[..., -1]
        g = bL[..., None] - b + ic
        m_new = jnp.maximum(bL + m, jnp.max(g, axis=-1))
        wk = jnp.exp(g - m_new[..., None])
        decay = jnp.exp(bL + m - m_new)
        C_new = decay[..., None, None] * C + jnp.einsum('bhs,bhsv,bhsd->bhvd', wk, vc, kc)
        n_new = decay[..., None] * n + jnp.einsum('bhs,bhsd->bhd', wk, kc)
        return (C_new, n_new, m_new), h

    init = (jnp.zeros((B, H, dv, dqk), f32), jnp.zeros((B, H, dqk), f32), jnp.zeros((B, H), f32))
    _, h = lax.scan(step, init, (to_chunks(qf), to_chunks(kf), to_chunks(vf), to_chunks(ig), to_chunks(lf)))
    h = jnp.moveaxis(h, (0, 2), (1, 3))
    return h.reshape(B, S, H, dv)


def dsa_sparse_attention(q, k, v, q_idx, k_idx, w_idx):
    B, S = q.shape[:2]
    n_sel = min(TOPK_MAX, S // 4)
    nb = S // Q_BLOCK
    f32 = jnp.float32
    kf, vf, kif = k.astype(f32), v.astype(f32), k_idx.astype(f32)
    wf = w_idx.astype(f32) * (H_IDX ** -0.5) * (D_IDX ** -0.5)
    key_pos = jnp.arange(S)
    gather = jax.vmap(lambda a, i: a[i])

    def blocks(a):
        return jnp.moveaxis(a.reshape((B, nb, Q_BLOCK) + a.shape[2:]), 1, 0)

    def attend_block(inp):
        qb, qib, wb, t0 = inp
        qpos = t0 + jnp.arange(Q_BLOCK)
        logits = jnp.einsum('bqhd,bsd->bqhs', qib.astype(f32), kif)
        score = jnp.einsum('bqh,bqhs->bqs', wb, jax.nn.relu(logits))
        visible = key_pos[None, :] <= qpos[:, None]
        score = jnp.where(visible[None], score, -jnp.inf)
        _, idx = lax.top_k(score, n_sel)
        k_sel = gather(kf, idx)
        v_sel = gather(vf, idx)
        s = jnp.einsum('bqhd,bqkd->bqhk', qb.astype(f32), k_sel) * (DH_A ** -0.5)
        valid = idx <= qpos[None, :, None]
        s = jnp.where(valid[:, :, None, :], s, -jnp.inf)
        pr = jax.nn.softmax(s, axis=-1)
        return jnp.einsum('bqhk,bqkd->bqhd', pr, v_sel)

    out = lax.map(attend_block, (blocks(q), blocks(q_idx), blocks(wf), jnp.arange(nb) * Q_BLOCK))
    return jnp.moveaxis(out, 0, 1).reshape(B, S, H_A * DH_A)


def hybrid_mixer(h, w_in, conv_w, conv_w_out, mlstm_b_i, mlstm_b_f, mlstm_norm, mlstm_w_out, attn_w_out, w_o):
    B, S, _ = h.shape
    proj = h @ w_in
    (cb, cc, cx, mq, mk, mv, mo, mi, mf, aq, ak, av, iq, ik, iw, gates) = jnp.split(proj, IN_OFFSETS, axis=-1)
    y_a = short_conv_mixer(cb, cc, cx, conv_w, conv_w_out)
    hm = mlstm_chunkwise(mq.reshape(B, S, H_M, DQK_M), mk.reshape(B, S, H_M, DQK_M),
                         mv.reshape(B, S, H_M, DV_M), mi + mlstm_b_i, mf + mlstm_b_f)
    hm = hm * lax.rsqrt(jnp.mean(hm * hm, axis=-1, keepdims=True) + EPS)
    hm = hm.reshape(B, S, D_MV) * mlstm_norm.astype(jnp.float32)
    y_m = (jax.nn.sigmoid(mo) * hm.astype(h.dtype)) @ mlstm_w_out
    ha = dsa_sparse_attention(aq.reshape(B, S, H_A, DH_A), ak, av,
                              iq.reshape(B, S, H_IDX, D_IDX), ik, iw)
    y_c = ha.astype(h.dtype) @ attn_w_out
    g = jax.nn.sigmoid(gates).reshape(B, S, 3, D_MODEL)
    merged = g[:, :, 0] * y_a + g[:, :, 1] * y_m + g[:, :, 2] * y_c
    return merged @ w_o


def setup_inputs(seed: int = 0) -> dict:
    key = jax.random.key(seed)
    ks = iter(jax.random.split(key, 32))
    f32 = jnp.float32

    def nrm(shape, fan_in):
        return jax.random.normal(next(ks), shape, f32) * (fan_in ** -0.5)

    def gain(shape):
        return 1.0 + 0.02 * jax.random.normal(next(ks), shape, f32)

    return {
        "x": jax.random.normal(next(ks), (BATCH, SEQ, D_MODEL), f32),
        "p": jax.random.normal(next(ks), (DEPTH, BATCH, SEQ, D_PLE), f32),
        "norm_ffn1": gain((DEPTH, D_MODEL)),
        "ffn1_w_gu": nrm((DEPTH, D_MODEL, 2 * D_FF), D_MODEL),
        "ffn1_w_down": nrm((DEPTH, D_FF, D_MODEL), D_FF),
        "norm_mix": gain((DEPTH, D_MODEL)),
        "w_in": nrm((DEPTH, D_MODEL, N_IN), D_MODEL),
        "conv_w": nrm((DEPTH, CONV_W, D_CONV), CONV_W),
        "conv_w_out": nrm((DEPTH, D_CONV, D_MODEL), D_CONV),
        "mlstm_b_i": 0.1 * jax.random.normal(next(ks), (DEPTH, H_M), f32),
        "mlstm_b_f": jnp.linspace(3.0, 6.0, H_M, dtype=f32)[None, :] + 0.1 * jax.random.normal(next(ks), (DEPTH, H_M), f32),
        "mlstm_norm": gain((DEPTH, D_MV)),
        "mlstm_w_out": nrm((DEPTH, D_MV, D_MODEL), D_MV),
        "attn_w_out": nrm((DEPTH, H_A * DH_A, D_MODEL), H_A * DH_A),
        "w_o": nrm((DEPTH, D_MODEL, D_MODEL), D_MODEL),
        "norm_ffn2": gain((DEPTH, D_MODEL)),
        "ffn2_w_gu": nrm((DEPTH, D_MODEL, 2 * D_FF), D_MODEL),
        "ffn2_w_down": nrm((DEPTH, D_FF, D_MODEL), D_FF),
        "norm_ple": gain((DEPTH, D_MODEL)),
        "ple_w_gate": nrm((DEPTH, D_MODEL, D_MODEL), D_MODEL),
        "ple_w_proj": nrm((DEPTH, D_PLE, D_MODEL), D_PLE),
        "final_norm": gain((D_MODEL,)),
    }


def reference(x, p, norm_ffn1, ffn1_w_gu, ffn1_w_down, norm_mix, w_in, conv_w, conv_w_out,
              mlstm_b_i, mlstm_b_f, mlstm_norm, mlstm_w_out, attn_w_out, w_o,
              norm_ffn2, ffn2_w_gu, ffn2_w_down, norm_ple, ple_w_gate, ple_w_proj, final_norm):
    for l in range(DEPTH):
        x = x + 0.5 * swiglu(rms_norm(x, norm_ffn1[l]), ffn1_w_gu[l], ffn1_w_down[l])
        x = x + hybrid_mixer(rms_norm(x, norm_mix[l]), w_in[l], conv_w[l], conv_w_out[l],
                             mlstm_b_i[l], mlstm_b_f[l], mlstm_norm[l], mlstm_w_out[l],
                             attn_w_out[l], w_o[l])
        x = x + 0.5 * swiglu(rms_norm(x, norm_ffn2[l]), ffn2_w_gu[l], ffn2_w_down[l])
        gate = jax.nn.sigmoid(rms_norm(x, norm_ple[l]) @ ple_w_gate[l])
        x = x + gate * (p[l] @ ple_w_proj[l])
    return rms_norm(x, final_norm)
```

```python
import contextlib
import numpy as np
import concourse.bass as bass
import concourse.mybir as mybir
from concourse.bass_utils import run_bass_kernel_spmd

F32 = mybir.dt.float32
BF16 = mybir.dt.bfloat16
AF = mybir.ActivationFunctionType
ALU = mybir.AluOpType
AX = mybir.AxisListType

D = 1024
KD = 8
FF = 2816
KF = 22
D_PLE = 256
N_IN = 8652
EPS = 1e-6
N_CORES = 8


class Buf:
    __slots__ = ("w", "r", "name", "excl")

    def __init__(self, name="", excl=False):
        self.w = None
        self.r = {}
        self.name = name
        self.excl = excl


class Stream:
    def __init__(self, name, sem, inc):
        self.name, self.sem, self.inc = name, sem, inc
        self.ops = []


class Op:
    __slots__ = ("fn", "waits", "sig", "stream", "sigcount")

    def __init__(self, fn, stream):
        self.fn, self.stream = fn, stream
        self.waits = {}
        self.sig = False
        self.sigcount = 0


class Prog:
    ENGS = ("pe", "act", "dve", "pool", "sp")

    def __init__(self, nc, es):
        self.nc = nc
        self.es = es
        self.q = {e: [] for e in self.ENGS}
        self.seen = {e: {} for e in self.ENGS}
        self.streams = {}
        for e in ("pe", "act", "dve", "pool"):
            self.new_stream(e, 1)

    def new_stream(self, name, inc=16):
        sem = self.es.enter_context(self.nc.semaphore("s_" + name))
        self.streams[name] = Stream(name, sem, inc)
        return self.streams[name]

    def add(self, eng, fn, R=(), W=(), chan=None):
        st = self.streams[chan or eng]
        op = Op(fn, st)
        if st.inc == 16:
            op.sig = True
        st.ops.append(op)
        idx = len(st.ops)
        seen = self.seen[eng]
        waits = op.waits

        def need(dep):
            if dep is None:
                return
            s, j = dep
            if s is st and eng == "pe":
                return
            if seen.get(s, 0) >= j:
                return
            if waits.get(s, 0) < j:
                waits[s] = j

        for b in R:
            need(b.w)
            if b.excl:
                for s, j in b.r.items():
                    if s is not st:
                        need((s, j))
        for b in W:
            need(b.w)
            for s, j in b.r.items():
                if s is st and st.inc == 1:
                    continue
                need((s, j))
        for s, j in waits.items():
            seen[s] = j
            s.ops[j - 1].sig = True
        for b in R:
            if b.r.get(st, 0) < idx:
                b.r[st] = idx
        for b in W:
            b.w = (st, idx)
            b.r = {}
        self.q[eng].append(op)
        return op

    def barrier(self):
        tails = [(s, len(s.ops)) for s in self.streams.values() if s.ops]
        for e in self.ENGS:
            op = Op(None, None)
            seen = self.seen[e]
            for s, j in tails:
                if seen.get(s, 0) >= j:
                    continue
                op.waits[s] = j
                seen[s] = j
                s.ops[j - 1].sig = True
            self.q[e].append(op)

    def emit(self):
        for st in self.streams.values():
            c = 0
            for op in st.ops:
                if op.sig:
                    c += 1
                op.sigcount = c
        nc = self.nc
        q = self.q

        def run(e, ops):
            for op in ops:
                for s, j in op.waits.items():
                    e.wait_ge(s.sem, s.ops[j - 1].sigcount * s.inc)
                if op.fn is None:
                    continue
                ins = op.fn(e)
                if op.sig:
                    ins.then_inc(op.stream.sem, op.stream.inc)

        with nc.Block() as block:
            @block.tensor
            def _(e):
                run(e, q["pe"])

            @block.scalar
            def _(e):
                run(e, q["act"])

            @block.vector
            def _(e):
                run(e, q["dve"])

            @block.gpsimd
            def _(e):
                run(e, q["pool"])

            @block.sync
            def _(e):
                run(e, q["sp"])


class Ctx:
    def __init__(self, nc, es):
        self.nc, self.es = nc, es
        self.P = Prog(nc, es)
        self.P.new_stream("ld")
        self.P.new_stream("st")
        self.P.new_stream("wl")
        self.P.new_stream("pl")
        self._n = 0

    def sb(self, shape, dt, name=None):
        self._n += 1
        return self.es.enter_context(self.nc.sbuf_tensor(f"{name or 't'}_{self._n}", list(shape), dt))

    def ps(self, shape, dt=F32, name=None):
        self._n += 1
        return self.es.enter_context(self.nc.psum_tensor(f"{name or 'p'}_{self._n}", list(shape), dt))


class Scope:
    def __init__(self, cx):
        self.cx = cx
        self.es = contextlib.ExitStack()
        self.bufs = []

    def sb(self, shape, dt, name="t"):
        cx = self.cx
        cx._n += 1
        t = self.es.enter_context(cx.nc.sbuf_tensor(f"{name}_{cx._n}", list(shape), dt))
        b = Buf(name)
        self.bufs.append(b)
        return t, b

    def ps(self, shape, dt=F32, name="p"):
        cx = self.cx
        cx._n += 1
        t = self.es.enter_context(cx.nc.psum_tensor(f"{name}_{cx._n}", list(shape), dt))
        b = Buf(name, excl=True)
        self.bufs.append(b)
        return t, b

    def close(self):
        self.cx.P.barrier()
        self.es.close()


def load_w(cx, sc, w2d, kc, n0, n1, name, dst=None, dcol=0):
    P = cx.P
    n = n1 - n0
    if dst is None:
        t, b = sc.sb([128, kc, n], BF16, name)
    else:
        t, b = dst
    for c in range(kc):
        P.add("pool", lambda e, c=c: e.dma_start(out=t[:, c, dcol:dcol + n], in_=w2d[c * 128:(c + 1) * 128, n0:n1]),
              W=[b], chan="pl")
    return t, b


def load_vec_cols(cx, sc, v1d, kc, name):
    t, b = sc.sb([128, kc], F32, name)
    cx.P.add("sp", lambda e: e.dma_start(out=t[:, :], in_=v1d.rearrange("(c p) -> p c", p=128),
                                         allow_slow_non_contiguous=True), W=[b], chan="wl")
    return t, b


def rmsnorm_T(cx, sc, st, xt, xb, g, gb, TT):
    P = cx.P
    sq, sqb = st["sq"]
    hT, hb = st["hT"]
    ssp, sspb = st["ssp"]
    rs, rsb = st["rs"]
    ones, onesb = st["ones"]
    P.add("act", lambda e: e.activation(out=sq[:, :, :], in_=xt[:, :, :], func=AF.Square), R=[xb], W=[sqb])
    for c in range(KD):
        P.add("pe", lambda e, c=c: e.matmul(ssp[:, 0:TT], ones[:, :], sq[:, c, :], start=(c == 0), stop=(c == KD - 1)),
              R=[sqb, onesb], W=[sspb])
    P.add("act", lambda e: e.activation(out=rs[:, 0:TT], in_=ssp[:, 0:TT], func=AF.Sqrt, scale=1.0 / D, bias=st["eps"][0][:, 0:1]),
          R=[sspb, st["eps"][1]], W=[rsb])
    P.add("dve", lambda e: e.reciprocal(out=rs[:, 0:TT], in_=rs[:, 0:TT]), R=[rsb], W=[rsb])
    for c in range(KD):
        P.add("dve", lambda e, c=c: e.scalar_tensor_tensor(out=hT[:, c, :], in0=xt[:, c, :], scalar=g[:, c:c + 1],
                                                           in1=rs[:, 0:TT], op0=ALU.mult, op1=ALU.mult),
              R=[xb, gb, rsb], W=[hb])
    return hT, hb


def norm_scratch(cx, sc, TT):
    st = {}
    st["sq"] = sc.sb([128, KD, TT], BF16, "sq")
    st["hT"] = sc.sb([128, KD, TT], BF16, "hT")
    st["ssp"] = sc.ps([128, 512], F32, "ssp")
    st["rs"] = sc.sb([128, TT], F32, "rs")
    st["ones"] = sc.sb([128, 128], BF16, "ones")
    st["eps"] = sc.sb([128, 1], F32, "eps")
    ones, onesb = st["ones"]
    eps, epsb = st["eps"]
    cx.P.add("pool", lambda e: e.memset(ones[:, :], 1.0), W=[onesb])
    cx.P.add("pool", lambda e: e.memset(eps[:, :], EPS), W=[epsb])
    return st


def make_identity(cx, sc, dt, name="ident"):
    t, b = sc.sb([128, 128], dt, name)
    cx.P.add("pool", lambda e: e.memset(t[:, :], 1.0), W=[b])
    cx.P.add("pool", lambda e: e.affine_select(out=t[:, :], in_=t[:, :], pattern=[[-1, 128]], compare_op=ALU.is_equal,
                                               fill=0.0, base=0, channel_multiplier=1), R=[b], W=[b])
    return t, b


def phase_in(cx, G):
    P = cx.P
    TT = G["TT"]
    sc = Scope(cx)
    ident, identb = make_identity(cx, sc, F32)
    xin = [sc.sb([128, TT // 128, D], F32, "xin") for _ in range(2)]
    xo = [sc.sb([128, KD, TT], F32, "xo") for _ in range(2)]
    pp = [sc.ps([128, 2, TT], F32, "tp") for _ in range(4)]
    x = G["x"]
    for i in range(G["NT"]):
        xi, xib = xin[i % 2]
        xo_t, xob = xo[i % 2]
        P.add("sp", lambda e, i=i, xi=xi: e.dma_start(
            out=xi[:, :, :], in_=x[i * TT:(i + 1) * TT, :].rearrange("(a p) d -> p a d", p=128)), W=[xib], chan="ld")
        for c2 in range(KD // 2):
            pt, ptb = pp[c2 % 4]
            for cc in range(2):
                c = c2 * 2 + cc
                for a in range(TT // 128):
                    P.add("pe", lambda e, c=c, cc=cc, a=a, pt=pt, xi=xi: e.transpose(
                        pt[:, cc, a * 128:(a + 1) * 128], xi[:, a, c * 128:(c + 1) * 128], ident[:, :]),
                        R=[xib, identb], W=[ptb])
            eng = "act" if c2 % 2 == 0 else "dve"
            if eng == "act":
                P.add("act", lambda e, c2=c2, pt=pt, xo_t=xo_t: e.copy(out=xo_t[:, 2 * c2:2 * c2 + 2, :], in_=pt[:, :, :]),
                      R=[ptb], W=[xob])
            else:
                P.add("dve", lambda e, c2=c2, pt=pt, xo_t=xo_t: e.tensor_copy(out=xo_t[:, 2 * c2:2 * c2 + 2, :], in_=pt[:, :, :]),
                      R=[ptb], W=[xob])
        P.add("sp", lambda e, i=i, xo_t=xo_t: e.dma_start(
            out=G["XT"][:, i * TT:(i + 1) * TT].rearrange("(c p) t -> p c t", p=128), in_=xo_t[:, :, :]),
            R=[xob], W=[G["XTb"][i]], chan="st")
    sc.close()


def phase_out(cx, G):
    P = cx.P
    TT = G["TT"]
    sc = Scope(cx)
    ident, identb = make_identity(cx, sc, F32)
    st = norm_scratch(cx, sc, TT)
    g, gb = load_vec_cols(cx, sc, G["w"]["final_norm"], KD, "gfin")
    xin = [sc.sb([128, KD, TT], F32, "xin") for _ in range(2)]
    yn = sc.sb([128, KD, TT], F32, "yn")
    yo = [sc.sb([128, D], F32, "yo") for _ in range(2)]
    pp = [sc.ps([128, 512], F32, "tp") for _ in range(4)]
    rs, rsb = st["rs"]
    sq, sqb = st["sq"]
    ssp, sspb = st["ssp"]
    ones, onesb = st["ones"]
    k = 0
    for i in range(G["NT"]):
        xt, xb = xin[i % 2]
        P.add("sp", lambda e, i=i, xt=xt: e.dma_start(
            out=xt[:, :, :], in_=G["XT"][:, i * TT:(i + 1) * TT].rearrange("(c p) t -> p c t", p=128)),
            R=[G["XTb"][i]], W=[xb], chan="ld")
        P.add("act", lambda e, xt=xt: e.activation(out=sq[:, :, :], in_=xt[:, :, :], func=AF.Square), R=[xb], W=[sqb])
        for c in range(KD):
            P.add("pe", lambda e, c=c: e.matmul(ssp[:, 0:TT], ones[:, :], sq[:, c, :], start=(c == 0), stop=(c == KD - 1)),
                  R=[sqb, onesb], W=[sspb])
        P.add("act", lambda e: e.activation(out=rs[:, 0:TT], in_=ssp[:, 0:TT], func=AF.Sqrt, scale=1.0 / D, bias=st["eps"][0][:, 0:1]),
              R=[sspb, st["eps"][1]], W=[rsb])
        P.add("dve", lambda e: e.reciprocal(out=rs[:, 0:TT], in_=rs[:, 0:TT]), R=[rsb], W=[rsb])
        y, yb = yn
        for c in range(KD):
            P.add("dve", lambda e, c=c, xt=xt: e.scalar_tensor_tensor(out=y[:, c, :], in0=xt[:, c, :], scalar=g[:, c:c + 1],
                                                                      in1=rs[:, 0:TT], op0=ALU.mult, op1=ALU.mult),
                  R=[xb, gb, rsb], W=[yb])
        for a in range(TT // 128):
            yt, ytb = yo[k % 2]
            for hh in range(2):
                pt, ptb = pp[(2 * k + hh) % 4]
                for c4 in range(4):
                    c = hh * 4 + c4
                    P.add("pe", lambda e, c=c, c4=c4, a=a, pt=pt: e.transpose(
                        pt[:, c4 * 128:(c4 + 1) * 128], y[:, c, a * 128:(a + 1) * 128], ident[:, :]),
                        R=[yb, identb], W=[ptb])
                if hh == 0:
                    P.add("act", lambda e, pt=pt, yt=yt: e.copy(out=yt[:, 0:512], in_=pt[:, :]), R=[ptb], W=[ytb])
                else:
                    P.add("dve", lambda e, pt=pt, yt=yt: e.tensor_copy(out=yt[:, 512:1024], in_=pt[:, :]), R=[ptb], W=[ytb])
            r0 = i * TT + a * 128
            P.add("sp", lambda e, r0=r0, yt=yt: e.dma_start(out=G["out"][r0:r0 + 128, :], in_=yt[:, :]),
                  R=[ytb], W=[G["outb"]], chan="st")
            k += 1
    sc.close()


def phase_ffn(cx, G, l, which):
    P = cx.P
    TT = G["TT"]
    w = G["w"]
    sc = Scope(cx)
    pre = "ffn1" if which == 1 else "ffn2"
    wgu, wgub = load_w(cx, sc, w[pre + "_w_gu"][l], KD, 0, 2 * FF, "wgu")
    wd, wdb = load_w(cx, sc, w[pre + "_w_down"][l], KF, 0, D, "wd")
    g, gb = load_vec_cols(cx, sc, w["norm_" + pre][l], KD, "gn")
    st = norm_scratch(cx, sc, TT)
    xin = [sc.sb([128, KD, TT], F32, "xin") for _ in range(2)]
    m, mb = sc.sb([128, KF, TT], BF16, "m")
    av = [sc.sb([128, TT], F32, "a") for _ in range(2)]
    pgu = [sc.ps([128, 2, TT], F32, "pgu") for _ in range(3)]
    pdn = [sc.ps([128, 2, TT], F32, "pdn") for _ in range(2)]

    def load(i):
        xt, xb = xin[i % 2]
        P.add("sp", lambda e: e.dma_start(
            out=xt[:, :, :], in_=G["XT"][:, i * TT:(i + 1) * TT].rearrange("(c p) t -> p c t", p=128)),
            R=[G["XTb"][i]], W=[xb], chan="ld")

    load(0)
    for i in range(G["NT"]):
        xt, xb = xin[i % 2]
        if i + 1 < G["NT"]:
            load(i + 1)
        hT, hb = rmsnorm_T(cx, sc, st, xt, xb, g, gb, TT)
        for j in range(KF):
            pg, pgb = pgu[j % 3]
            for half in range(2):
                col = half * FF + j * 128
                for c in range(KD):
                    P.add("pe", lambda e, c=c, col=col, half=half, pg=pg: e.matmul(
                        pg[:, half, :], wgu[:, c, col:col + 128], hT[:, c, :], start=(c == 0), stop=(c == KD - 1)),
                        R=[wgub, hb], W=[pgb])
            a, ab = av[j % 2]
            P.add("act", lambda e, pg=pg, a=a: e.activation(out=a[:, :], in_=pg[:, 0, :], func=AF.Silu), R=[pgb], W=[ab])
            P.add("dve", lambda e, pg=pg, a=a, j=j: e.tensor_tensor(out=m[:, j, :], in0=a[:, :], in1=pg[:, 1, :], op=ALU.mult),
                  R=[pgb, ab], W=[mb])
        for o2 in range(KD // 2):
            pd, pdb = pdn[o2 % 2]
            for oo in range(2):
                oc = o2 * 2 + oo
                for j in range(KF):
                    P.add("pe", lambda e, j=j, oc=oc, oo=oo, pd=pd: e.matmul(
                        pd[:, oo, :], wd[:, j, oc * 128:(oc + 1) * 128], m[:, j, :], start=(j == 0), stop=(j == KF - 1)),
                        R=[wdb, mb], W=[pdb])
            P.add("dve", lambda e, o2=o2, pd=pd, xt=xt: e.scalar_tensor_tensor(
                out=xt[:, 2 * o2:2 * o2 + 2, :], in0=pd[:, :, :], scalar=0.5, in1=xt[:, 2 * o2:2 * o2 + 2, :],
                op0=ALU.mult, op1=ALU.add), R=[pdb, xb], W=[xb])
        P.add("sp", lambda e, i=i, xt=xt: e.dma_start(
            out=G["XT"][:, i * TT:(i + 1) * TT].rearrange("(c p) t -> p c t", p=128), in_=xt[:, :, :]),
            R=[xb], W=[G["XTb"][i]], chan="st")
    sc.close()


OFF_CB, OFF_CC, OFF_CX = 0, 512, 1024
OFF_MQ, OFF_MK, OFF_MV, OFF_MO, OFF_MI, OFF_MF = 1536, 2048, 2560, 3584, 4608, 4612
OFF_AQ, OFF_AK, OFF_AV, OFF_IQ, OFF_IK, OFF_IW = 4616, 5128, 5192, 5256, 5512, 5576
OFF_G = 5580


def xt_ap(G, i):
    TT = G["TT"]
    return G["XT"][:, i * TT:(i + 1) * TT].rearrange("(c p) t -> p c t", p=128)


def ht_ap(G, i):
    TT = G["TT"]
    return G["HT"][:, i * TT:(i + 1) * TT].rearrange("(c p) t -> p c t", p=128)


def phase_norm(cx, G, l):
    P = cx.P
    TT = G["TT"]
    sc = Scope(cx)
    g, gb = load_vec_cols(cx, sc, G["w"]["norm_mix"][l], KD, "gn")
    st = norm_scratch(cx, sc, TT)
    xin = [sc.sb([128, KD, TT], F32, "xin") for _ in range(2)]
    ho = [sc.sb([128, KD, TT], BF16, "ho") for _ in range(2)]
    for i in range(G["NT"]):
        xt, xb = xin[i % 2]
        P.add("sp", lambda e, i=i, xt=xt: e.dma_start(out=xt[:, :, :], in_=xt_ap(G, i)), R=[G["XTb"][i]], W=[xb], chan="ld")
        st["hT"] = ho[i % 2]
        hT, hb = rmsnorm_T(cx, sc, st, xt, xb, g, gb, TT)
        P.add("sp", lambda e, i=i, hT=hT: e.dma_start(out=ht_ap(G, i), in_=hT[:, :, :]), R=[hb], W=[G["HTb"][i]], chan="st")
    sc.close()


class Tail:
    def __init__(self, cx, sc, G, l, br, wout2d, kin, psA, psB):
        w = G["w"]
        TT = G["TT"]
        self.cx, self.G, self.kin = cx, G, kin
        self.wg = load_w(cx, sc, w["w_in"][l], KD, OFF_G + br * D, OFF_G + (br + 1) * D, "wgate")
        self.wout = load_w(cx, sc, wout2d, kin, 0, D, "wout")
        self.wo = load_w(cx, sc, w["w_o"][l], KD, 0, D, "wo")
        self.sg = [sc.sb([128, TT], F32, "sg") for _ in range(2)]
        self.mg = sc.sb([128, KD, TT], BF16, "mg")
        self.xin = [sc.sb([128, KD, TT], F32, "xres") for _ in range(2)]
        self.psA, self.psB = psA, psB

    def load_x(self, i):
        G = self.G
        xt, xb = self.xin[i % 2]
        self.cx.P.add("sp", lambda e: e.dma_start(out=xt[:, :, :], in_=xt_ap(G, i)), R=[G["XTb"][i]], W=[xb], chan="ld")

    def run(self, i, hT, hb, yT, yb):
        P = self.cx.P
        G = self.G
        wg, wgb = self.wg
        wout, woutb = self.wout
        wo, wob = self.wo
        mg, mgb = self.mg
        xt, xb = self.xin[i % 2]
        kin = self.kin
        for oc in range(KD):
            pa, pab = self.psA[oc % len(self.psA)]
            for c in range(kin):
                P.add("pe", lambda e, c=c, oc=oc, pa=pa: e.matmul(pa[:, 0, :], wout[:, c, oc * 128:(oc + 1) * 128], yT[:, c, :],
                                                               start=(c == 0), stop=(c == kin - 1)), R=[woutb, yb], W=[pab])
            for c in range(KD):
                P.add("pe", lambda e, c=c, oc=oc, pa=pa: e.matmul(pa[:, 1, :], wg[:, c, oc * 128:(oc + 1) * 128], hT[:, c, :],
                                                               start=(c == 0), stop=(c == KD - 1)), R=[wgb, hb], W=[pab])
            sg, sgb = self.sg[oc % 2]
            P.add("act", lambda e, pa=pa, sg=sg: e.activation(out=sg[:, :], in_=pa[:, 1, :], func=AF.Sigmoid), R=[pab], W=[sgb])
            P.add("dve", lambda e, pa=pa, sg=sg, oc=oc: e.tensor_tensor(out=mg[:, oc, :], in0=sg[:, :], in1=pa[:, 0, :], op=ALU.mult),
                  R=[pab, sgb], W=[mgb])
        for o2 in range(KD // 2):
            po, pob = self.psB[o2 % len(self.psB)]
            for oo in range(2):
                oc = o2 * 2 + oo
                for c in range(KD):
                    P.add("pe", lambda e, c=c, oc=oc, oo=oo, po=po: e.matmul(po[:, oo, :], wo[:, c, oc * 128:(oc + 1) * 128], mg[:, c, :],
                                                                         start=(c == 0), stop=(c == KD - 1)), R=[wob, mgb], W=[pob])
            P.add("dve", lambda e, o2=o2, po=po: e.tensor_tensor(out=xt[:, 2 * o2:2 * o2 + 2, :], in0=xt[:, 2 * o2:2 * o2 + 2, :],
                                                              in1=po[:, :, :], op=ALU.add), R=[pob, xb], W=[xb])
        P.add("sp", lambda e: e.dma_start(out=xt_ap(G, i), in_=xt[:, :, :]), R=[xb], W=[G["XTb"][i]], chan="st")


def phase_conv(cx, G, l):
    P = cx.P
    TT = G["TT"]
    w = G["w"]
    sc = Scope(cx)
    win, winb = load_w(cx, sc, w["w_in"][l], KD, 0, 1536, "winc")
    psA = [sc.ps([128, 2, TT], F32, "psA") for _ in range(2)]
    psB = [sc.ps([128, 2, TT], F32, "psB") for _ in range(2)]
    pcx = [sc.ps([128, 2, TT], F32, "pcx") for _ in range(2)]
    pb_ = [sc.ps([128, 2, TT], F32, "pbb") for _ in range(2)]
    tail = Tail(cx, sc, G, l, 0, w["conv_w_out"][l], 4, psA, psB)
    cw, cwb = sc.sb([128, 4, 3], F32, "cw")
    for j in range(3):
        P.add("sp", lambda e, j=j: e.dma_start(out=cw[:, :, j], in_=w["conv_w"][l][j].rearrange("(c p) -> p c", p=128),
                                               allow_slow_non_contiguous=True), W=[cwb], chan="wl")
    hin = [sc.sb([128, KD, TT], BF16, "hin") for _ in range(2)]
    u = [sc.sb([128, TT + 2], F32, "u") for _ in range(4)]
    ccs = [sc.sb([128, TT], F32, "ccs") for _ in range(2)]
    yv = [sc.sb([128, TT], F32, "yv") for _ in range(2)]
    z, zb = sc.sb([128, 4, TT], BF16, "z")
    tiles_per_seq = G["S"] // TT

    def load(i):
        hT, hb = hin[i % 2]
        P.add("sp", lambda e: e.dma_start(out=hT[:, :, :], in_=ht_ap(G, i)), R=[G["HTb"][i]], W=[hb], chan="ld")
        tail.load_x(i)

    load(0)
    for i in range(G["NT"]):
        hT, hb = hin[i % 2]
        if i + 1 < G["NT"]:
            load(i + 1)
        for q in range(4):
            ut, ub = u[q]
            if i % tiles_per_seq == 0:
                P.add("pool", lambda e, ut=ut: e.memset(ut[:, 0:2], 0.0), W=[ub])
            p1, p1b = pcx[q % 2]
            p2, p2b = pb_[q % 2]
            for k_, (pt, ptb, slot, off) in enumerate(((p1, p1b, 0, OFF_CC), (p1, p1b, 1, OFF_CX), (p2, p2b, 0, OFF_CB))):
                for c in range(KD):
                    P.add("pe", lambda e, c=c, pt=pt, slot=slot, col=off + q * 128, hT=hT: e.matmul(
                        pt[:, slot, :], win[:, c, col:col + 128], hT[:, c, :], start=(c == 0), stop=(c == KD - 1)),
                        R=[winb, hb], W=[ptb])
            cs, csb = ccs[q % 2]
            P.add("act", lambda e, p1=p1, cs=cs: e.copy(out=cs[:, :], in_=p1[:, 0, :]), R=[p1b], W=[csb])
            P.add("dve", lambda e, p1=p1, cs=cs, ut=ut: e.tensor_tensor(out=ut[:, 2:TT + 2], in0=cs[:, :], in1=p1[:, 1, :], op=ALU.mult),
                  R=[p1b, csb], W=[ub])
            y, yb_ = yv[q % 2]
            P.add("dve", lambda e, y=y, ut=ut, q=q: e.tensor_scalar(out=y[:, :], in0=ut[:, 0:TT], scalar1=cw[:, q, 0:1], scalar2=None,
                                                                  op0=ALU.mult), R=[ub, cwb], W=[yb_])
            P.add("dve", lambda e, y=y, ut=ut, q=q: e.scalar_tensor_tensor(out=y[:, :], in0=ut[:, 1:TT + 1], scalar=cw[:, q, 1:2],
                                                                         in1=y[:, :], op0=ALU.mult, op1=ALU.add), R=[ub, cwb, yb_], W=[yb_])
            P.add("dve", lambda e, y=y, ut=ut, q=q: e.scalar_tensor_tensor(out=y[:, :], in0=ut[:, 2:TT + 2], scalar=cw[:, q, 2:3],
                                                                         in1=y[:, :], op0=ALU.mult, op1=ALU.add), R=[ub, cwb, yb_], W=[yb_])
            P.add("pool", lambda e, ut=ut: e.tensor_copy(out=ut[:, 0:2], in_=ut[:, TT:TT + 2]), R=[ub], W=[ub])
            P.add("dve", lambda e, y=y, p2=p2, q=q: e.tensor_tensor(out=z[:, q, :], in0=y[:, :], in1=p2[:, 0, :], op=ALU.mult),
                  R=[yb_, p2b], W=[zb])
        tail.run(i, hT, hb, z, zb)
    sc.close()


def phase_ple(cx, G, l):
    P = cx.P
    TT = G["TT"]
    w = G["w"]
    sc = Scope(cx)
    wg, wgb = load_w(cx, sc, w["ple_w_gate"][l], KD, 0, D, "wpg")
    wp, wpb = load_w(cx, sc, w["ple_w_proj"][l], 2, 0, D, "wpp")
    g, gb = load_vec_cols(cx, sc, w["norm_ple"][l], KD, "gn")
    st = norm_scratch(cx, sc, TT)
    ident, identb = make_identity(cx, sc, F32)
    xin = [sc.sb([128, KD, TT], F32, "xin") for _ in range(2)]
    pin = [sc.sb([128, TT // 128, D_PLE], F32, "pin") for _ in range(2)]
    pT, pTb = sc.sb([128, 2, TT], BF16, "pT")
    sgs = [sc.sb([128, TT], F32, "sg") for _ in range(2)]
    ptp = sc.ps([128, 2, TT], F32, "ptp")
    psA = [sc.ps([128, 2, TT], F32, "psA") for _ in range(3)]

    def load(i):
        xt, xb = xin[i % 2]
        P.add("sp", lambda e: e.dma_start(out=xt[:, :, :], in_=xt_ap(G, i)), R=[G["XTb"][i]], W=[xb], chan="ld")
        pi, pib = pin[i % 2]
        P.add("sp", lambda e: e.dma_start(out=pi[:, :, :], in_=G["p"][l, i * TT:(i + 1) * TT, :].rearrange("(a p) d -> p a d", p=128)),
              W=[pib], chan="ld")

    load(0)
    for i in range(G["NT"]):
        xt, xb = xin[i % 2]
        pi, pib = pin[i % 2]
        if i + 1 < G["NT"]:
            load(i + 1)
        hT, hb = rmsnorm_T(cx, sc, st, xt, xb, g, gb, TT)
        pt, ptb = ptp
        for kc in range(2):
            for a in range(TT // 128):
                P.add("pe", lambda e, kc=kc, a=a, pi=pi: e.transpose(pt[:, kc, a * 128:(a + 1) * 128], pi[:, a, kc * 128:(kc + 1) * 128], ident[:, :]),
                      R=[pib, identb], W=[ptb])
        P.add("act", lambda e: e.copy(out=pT[:, :, :], in_=pt[:, :, :]), R=[ptb], W=[pTb])
        for oc in range(KD):
            pa, pab = psA[oc % 3]
            for c in range(KD):
                P.add("pe", lambda e, c=c, oc=oc, pa=pa: e.matmul(pa[:, 0, :], wg[:, c, oc * 128:(oc + 1) * 128], hT[:, c, :],
                                                               start=(c == 0), stop=(c == KD - 1)), R=[wgb, hb], W=[pab])
            for c in range(2):
                P.add("pe", lambda e, c=c, oc=oc, pa=pa: e.matmul(pa[:, 1, :], wp[:, c, oc * 128:(oc + 1) * 128], pT[:, c, :],
                                                               start=(c == 0), stop=(c == 1)), R=[wpb, pTb], W=[pab])
            sg, sgb = sgs[oc % 2]
            P.add("act", lambda e, pa=pa, sg=sg: e.activation(out=sg[:, :], in_=pa[:, 0, :], func=AF.Sigmoid), R=[pab], W=[sgb])
            if G.get("dbg_dump") and i == 0 and oc == 0:
                P.add("sp", lambda e, sg=sg: e.dma_start(out=G["dbgo"][:, 0:TT], in_=sg[:, :]), R=[sgb], W=[G["outb"]], chan="st")
                d2, d2b = sc.sb([128, TT], F32, "d2")
                P.add("act", lambda e, pa=pa: e.copy(out=d2[:, :], in_=pa[:, 1, :]), R=[pab], W=[d2b])
                P.add("sp", lambda e: e.dma_start(out=G["dbgo"][:, TT:2 * TT], in_=d2[:, :]), R=[d2b], W=[G["outb"]], chan="st")
                d3, d3b = sc.sb([128, 2, TT], F32, "d3")
                P.add("act", lambda e: e.copy(out=d3[:, :, :], in_=pT[:, :, :]), R=[pTb], W=[d3b])
                P.add("sp", lambda e: e.dma_start(out=G["dbgo"][:, 2 * TT:4 * TT], in_=d3[:, :, :].rearrange("p a t -> p (a t)")), R=[d3b], W=[G["outb"]], chan="st")
            P.add("dve", lambda e, pa=pa, sg=sg: e.tensor_tensor(out=sg[:, :], in0=sg[:, :], in1=pa[:, 1, :], op=ALU.mult),
                  R=[pab, sgb], W=[sgb])
            if not G.get("dbg_skip_add"):
                P.add("pool", lambda e, sg=sg, oc=oc, xt=xt: e.tensor_tensor(out=xt[:, oc, :], in0=xt[:, oc, :], in1=sg[:, :], op=ALU.add),
                      R=[sgb, xb], W=[xb])
        P.add("sp", lambda e, i=i, xt=xt: e.dma_start(out=xt_ap(G, i), in_=xt[:, :, :]), R=[xb], W=[G["XTb"][i]], chan="st")
    sc.close()


import math


def OP(cx, eng, method, *args, R=(), W=(), chan=None, **kw):
    return cx.P.add(eng, lambda e: getattr(e, method)(*args, **kw), R=R, W=W, chan=chan)


def flat(t):
    return t[:, :, :].rearrange("p a t -> p (a t)")


def make_tri(cx, sc, dt, val, name):
    t, b = sc.sb([128, 128], dt, name)
    OP(cx, "pool", "memset", t[:, :], val, W=[b])
    OP(cx, "pool", "affine_select", out=t[:, :], in_=t[:, :], pattern=[[1, 128]], compare_op=ALU.is_ge, fill=0.0, base=0,
       channel_multiplier=-1, R=[b], W=[b])
    return t, b


def phase_mlstm(cx, G, l):
    P = cx.P
    TT = G["TT"]
    w = G["w"]
    sc = Scope(cx)
    win, winb = load_w(cx, sc, w["w_in"][l], KD, OFF_MQ, OFF_AQ, "winm")
    pA, pAb = sc.ps([128, 2, TT], F32, "pA")
    pK, pKb = sc.ps([128, 2, TT], F32, "pK")
    pV, pVb = sc.ps([128, 2, TT], F32, "pV")
    pG, pGb = sc.ps([128, 2, TT], F32, "pG")
    pB, pBb = sc.ps([128, 4, 128], F32, "pB")
    pS, pSb = sc.ps([128, 4, 128], F32, "pS")
    pN = [sc.ps([128, 2, TT], F32, "pN") for _ in range(2)]
    tail = Tail(cx, sc, G, l, 1, w["mlstm_w_out"][l], 8, [(pK, pKb), (pV, pVb)], pN)
    identb16, identb16b = make_identity(cx, sc, BF16, "identb")
    NU, NUb = make_tri(cx, sc, BF16, -1.0, "NU")
    U, Ub = make_tri(cx, sc, F32, 1.0, "U")
    ones3, ones3b = sc.sb([128, 4, 128], BF16, "ones3")
    l4h, l4hb = sc.sb([128, 2, 4], BF16, "l4h")
    lbh, lbhb = sc.sb([128, 2, 4, 128], BF16, "lbh")
    OP(cx, "pool", "memset", ones3[:, :, :], 1.0, W=[ones3b])
    nw, nwb = sc.sb([128, D], F32, "nw")
    OP(cx, "sp", "dma_start", out=nw[:, :], in_=w["mlstm_norm"][l].partition_broadcast(128), W=[nwb], chan="wl")
    bi_t, bib = sc.sb([128, 4], F32, "bi")
    bf_t, bfb = sc.sb([128, 4], F32, "bf")
    OP(cx, "sp", "dma_start", out=bi_t[:, :], in_=w["mlstm_b_i"][l].partition_broadcast(128), W=[bib], chan="wl")
    OP(cx, "sp", "dma_start", out=bf_t[:, :], in_=w["mlstm_b_f"][l].partition_broadcast(128), W=[bfb], chan="wl")
    eps_t, epsb = sc.sb([128, 1], F32, "eps")
    OP(cx, "pool", "memset", eps_t[:, :], EPS, W=[epsb])
    hin = [sc.sb([128, KD, TT], BF16, "hin") for _ in range(2)]
    qT, qTb = sc.sb([128, 4, TT], BF16, "qT")
    kT, kTb = sc.sb([128, 4, TT], BF16, "kT")
    vp, vpb = sc.sb([128, 4, 257], BF16, "vp")
    OP(cx, "pool", "memset", vp[:, :, :], 1.0, W=[vpb])
    og, ogb = sc.sb([128, D], F32, "og")
    gi, gib = sc.sb([128, 4], F32, "gi")
    l4, l4b = sc.sb([128, 4], F32, "l4")
    colb, colbb = sc.sb([128, 4], F32, "colb")
    bl, blb = sc.sb([128, 4], F32, "bl")
    wk, wkb = sc.sb([128, 4], F32, "wk")
    r4, r4b = sc.sb([128, 4], F32, "r4")
    msq, msqb = sc.sb([128, 4], F32, "msq")
    lb, lbb = sc.sb([128, 4, 128], F32, "lb")
    eb, ebb = sc.sb([128, 4, 128], F32, "eb")
    DT, DTb = sc.sb([128, 4, 128], F32, "DT")
    AT, ATb = sc.sb([128, 4, 128], BF16, "AT")
    qs, qsb = sc.sb([128, 4, 128], BF16, "qs")
    kts, ktsb = sc.sb([128, 4, 128], BF16, "kts")
    C32, C32b = sc.sb([128, 4, 257], F32, "C32")
    Cbf, Cbfb = sc.sb([128, 4, 257], BF16, "Cbf")
    tmp, tmpb = sc.sb([128, D], F32, "tmp")
    tmp2, tmp2b = sc.sb([128, D], F32, "tmp2")
    junk, junkb = sc.sb([128, 256], F32, "junk")
    hn, hnb = sc.sb([128, D], BF16, "hn")
    ymT, ymTb = sc.sb([128, KD, TT], BF16, "ymT")
    pAf, pKf, pVf, pGf = flat(pA), flat(pK), flat(pV), flat(pG)
    pAT = pAf.bitcast(BF16).rearrange("p (c t) -> p c t", t=128)
    tiles_per_seq = G["S"] // TT
    kscale = 128 ** -0.5

    def load(i):
        hT, hb = hin[i % 2]
        OP(cx, "sp", "dma_start", out=hT[:, :, :], in_=ht_ap(G, i), R=[G["HTb"][i]], W=[hb], chan="ld")
        tail.load_x(i)

    load(0)
    for i in range(G["NT"]):
        hT, hb = hin[i % 2]
        if i + 1 < G["NT"]:
            load(i + 1)
        if i % tiles_per_seq == 0:
            OP(cx, "pool", "memset", C32[:, :, :], 0.0, W=[C32b])
            OP(cx, "pool", "memset", Cbf[:, :, :], 0.0, W=[Cbfb])
        for which, dst, dstb, off in ((0, qT, qTb, 0), (1, kT, kTb, 512)):
            for hp in range(2):
                for hh in range(2):
                    h = hp * 2 + hh
                    for c in range(KD):
                        OP(cx, "pe", "matmul", pA[:, hh, :], win[:, c, off + h * 128:off + (h + 1) * 128], hT[:, c, :],
                           start=(c == 0), stop=(c == KD - 1), R=[winb, hb], W=[pAb])
                if which == 0:
                    OP(cx, "act", "copy", out=dst[:, 2 * hp:2 * hp + 2, :], in_=pA[:, :, :], R=[pAb], W=[dstb])
                else:
                    OP(cx, "act", "mul", dst[:, 2 * hp:2 * hp + 2, :], pA[:, :, :], kscale, R=[pAb], W=[dstb])
        for a in range(TT // 128):
            ts = slice(a * 128, (a + 1) * 128)
            if G.get("dbg_stop", 99) <= 0:
                continue
            for c in range(KD):
                OP(cx, "pe", "matmul", pGf[:, 0:8], hT[:, c, ts], win[:, c, 3072:3080], start=(c == 0), stop=(c == KD - 1),
                   R=[winb, hb], W=[pGb])
            if G.get("dbg_stop", 99) == 0.5:
                continue
            OP(cx, "dve", "tensor_tensor", out=gi[:, :], in0=pGf[:, 0:4], in1=bi_t[:, :], op=ALU.add, R=[pGb, bib], W=[gib])
            OP(cx, "dve", "tensor_tensor", out=l4[:, :], in0=pGf[:, 4:8], in1=bf_t[:, :], op=ALU.add, R=[pGb, bfb], W=[l4b])
            OP(cx, "act", "activation", out=l4[:, :], in_=l4[:, :], func=AF.Exp, scale=-1.0, R=[l4b], W=[l4b])
            OP(cx, "act", "activation", out=l4[:, :], in_=l4[:, :], func=AF.Ln, bias=1.0, R=[l4b], W=[l4b])
            if G.get("dbg_stop", 99) == 0.7:
                continue
            OP(cx, "dve", "tensor_copy", out=l4h[:, 0, :], in_=l4[:, :], R=[l4b], W=[l4hb])
            OP(cx, "dve", "tensor_tensor", out=l4h[:, 1, :], in0=l4[:, :], in1=l4h[:, 0, :], op=ALU.subtract, R=[l4b, l4hb], W=[l4hb])
            for z in range(2):
                OP(cx, "dve", "tensor_tensor", out=lbh[:, z, :, :], in0=ones3[:, :, :],
                   in1=l4h[:, z, :].unsqueeze(2).to_broadcast([128, 4, 128]), op=ALU.mult, R=[ones3b, l4hb], W=[lbhb])
            if G.get("dbg_stop", 99) == 0.8:
                continue
            for z in range(2):
                OP(cx, "pe", "matmul", pGf[:, 8:12], NU[:, :], l4h[:, z, :], start=(z == 0), stop=(z == 1), R=[NUb, l4hb], W=[pGb])
            for h in range(4):
                for z in range(2):
                    OP(cx, "pe", "matmul", pB[:, h, :], lbh[:, z, h, :], NU[:, :], start=(z == 0), stop=(z == 1), R=[lbhb, NUb], W=[pBb])
            if G.get("dbg_stop", 99) == 0.9:
                continue
            OP(cx, "act", "activation", out=eb[:, :, :], in_=pB[:, :, :], func=AF.Exp, R=[pBb], W=[ebb])
            OP(cx, "dve", "tensor_tensor", out=colb[:, :], in0=gi[:, :], in1=pGf[:, 8:12], op=ALU.subtract, R=[gib, pGb], W=[colbb])
            OP(cx, "dve", "tensor_copy", out=bl[:, :], in_=pB[:, :, 127], R=[pBb], W=[blb])
            OP(cx, "dve", "tensor_tensor", out=wk[:, :], in0=colb[:, :], in1=bl[:, :], op=ALU.add, R=[colbb, blb], W=[wkb])
            OP(cx, "act", "activation", out=wk[:, :], in_=wk[:, :], func=AF.Exp, R=[wkb], W=[wkb])
            OP(cx, "dve", "tensor_scalar", out=wk[:, :], in0=wk[:, :], scalar1=kscale, scalar2=None, op0=ALU.mult, R=[wkb], W=[wkb])
            if G.get("dbg_stop", 99) <= 1:
                continue
            for h in range(4):
                OP(cx, "pe", "matmul", pS[:, h, :], kT[:, h, ts], qT[:, h, ts], start=True, stop=True, R=[kTb, qTb], W=[pSb])
            for h in range(4):
                OP(cx, "act", "activation", out=DT[:, h, :], in_=pB[:, h, :], func=AF.Exp, bias=colb[:, h:h + 1], R=[pBb, colbb], W=[DTb])
            OP(cx, "pool", "tensor_tensor", out=DT[:, :, :], in0=DT[:, :, :], in1=U[:, :].unsqueeze(1).to_broadcast([128, 4, 128]),
               op=ALU.mult, R=[DTb, Ub], W=[DTb])
            OP(cx, "dve", "tensor_tensor", out=AT[:, :, :], in0=DT[:, :, :], in1=pS[:, :, :], op=ALU.mult, R=[DTb, pSb], W=[ATb])
            OP(cx, "pool", "tensor_tensor", out=qs[:, :, :], in0=qT[:, :, ts], in1=eb[:, :, :], op=ALU.mult, R=[qTb, ebb], W=[qsb])
            if G.get("dbg_stop", 99) <= 2:
                continue
            for c in range(KD):
                OP(cx, "pe", "matmul", pKf[:, :], hT[:, c, ts], win[:, c, 512:1024], start=(c == 0), stop=(c == KD - 1),
                   R=[winb, hb], W=[pKb])
            OP(cx, "dve", "tensor_tensor", out=kts[:, :, :], in0=pKf.rearrange("p (h d) -> p h d", d=128),
               in1=wk[:, :].unsqueeze(2).to_broadcast([128, 4, 128]), op=ALU.mult, R=[pKb, wkb], W=[ktsb])
            for r in range(2):
                for c in range(KD):
                    OP(cx, "pe", "matmul", pVf[:, :], hT[:, c, ts], win[:, c, 1024 + r * 512:1024 + (r + 1) * 512],
                       start=(c == 0), stop=(c == KD - 1), R=[winb, hb], W=[pVb])
                OP(cx, "act", "copy", out=vp[:, 2 * r:2 * r + 2, 0:256], in_=pVf.rearrange("p (h d) -> p h d", d=256), R=[pVb], W=[vpb])
            for r in range(2):
                for c in range(KD):
                    OP(cx, "pe", "matmul", pVf[:, :], hT[:, c, ts], win[:, c, 2048 + r * 512:2048 + (r + 1) * 512],
                       start=(c == 0), stop=(c == KD - 1), R=[winb, hb], W=[pVb])
                OP(cx, "act", "activation", out=og[:, r * 512:(r + 1) * 512], in_=pVf[:, :], func=AF.Sigmoid, R=[pVb], W=[ogb])
            if G.get("dbg_stop", 99) <= 3:
                continue
            for h in range(4):
                pn, pnb = pN[h % 2]
                pnf = flat(pn)
                OP(cx, "pe", "matmul", pnf[:, 0:257], qs[:, h, :], Cbf[:, h, :], start=True, stop=False, R=[qsb, Cbfb], W=[pnb])
                OP(cx, "pe", "matmul", pnf[:, 0:257], AT[:, h, :], vp[:, h, :], start=False, stop=True, R=[ATb, vpb], W=[pnb])
                OP(cx, "act", "activation", out=r4[:, h:h + 1], in_=pnf[:, 256:257], func=AF.Abs, R=[pnb], W=[r4b])
                OP(cx, "dve", "tensor_scalar", out=r4[:, h:h + 1], in0=r4[:, h:h + 1], scalar1=1.0, scalar2=None, op0=ALU.max,
                   R=[r4b], W=[r4b])
                OP(cx, "dve", "reciprocal", out=r4[:, h:h + 1], in_=r4[:, h:h + 1], R=[r4b], W=[r4b])
                OP(cx, "dve", "tensor_scalar", out=tmp[:, h * 256:(h + 1) * 256], in0=pnf[:, 0:256], scalar1=r4[:, h:h + 1], scalar2=None,
                   op0=ALU.mult, R=[pnb, r4b], W=[tmpb])
                OP(cx, "act", "activation", out=junk[:, :], in_=tmp[:, h * 256:(h + 1) * 256], func=AF.Square, accum_out=msq[:, h:h + 1],
                   R=[tmpb], W=[junkb, msqb])
            OP(cx, "act", "activation", out=msq[:, :], in_=msq[:, :], func=AF.Sqrt, scale=1.0 / 256, bias=eps_t[:, 0:1], R=[msqb, epsb], W=[msqb])
            OP(cx, "dve", "reciprocal", out=msq[:, :], in_=msq[:, :], R=[msqb], W=[msqb])
            for h in range(4):
                OP(cx, "dve", "scalar_tensor_tensor", out=tmp2[:, h * 256:(h + 1) * 256], in0=tmp[:, h * 256:(h + 1) * 256],
                   scalar=msq[:, h:h + 1], in1=nw[:, h * 256:(h + 1) * 256], op0=ALU.mult, op1=ALU.mult, R=[tmpb, msqb, nwb], W=[tmp2b])
            OP(cx, "pool", "tensor_tensor", out=hn[:, :], in0=tmp2[:, :], in1=og[:, :], op=ALU.mult, R=[tmp2b, ogb], W=[hnb])
            if G.get("dbg_stop", 99) <= 4:
                continue
            for h in range(4):
                OP(cx, "pe", "matmul", pAf[:, 0:257], kts[:, h, :], vp[:, h, :], start=True, stop=True, R=[ktsb, vpb], W=[pAb])
                OP(cx, "dve", "scalar_tensor_tensor", out=C32[:, h, :], in0=C32[:, h, :], scalar=eb[:, h, 127:128], in1=pAf[:, 0:257],
                   op0=ALU.mult, op1=ALU.add, R=[C32b, ebb, pAb], W=[C32b])
            OP(cx, "act", "copy", out=Cbf[:, :, :], in_=C32[:, :, :], R=[C32b], W=[Cbfb])
            if G.get("dbg_stop", 99) <= 5:
                continue
            for c in range(KD):
                OP(cx, "pe", "transpose", pAT[:, c, :], hn[:, c * 128:(c + 1) * 128], identb16[:, :], R=[hnb, identb16b], W=[pAb])
            OP(cx, "act", "copy", out=ymT[:, :, ts], in_=pAT[:, :, :], R=[pAb], W=[ymTb])
        tail.run(i, hT, hb, ymT, ymTb)
    sc.close()


def phase_dsa(cx, G, l):
    P = cx.P
    TT = G["TT"]
    S = G["S"]
    w = G["w"]
    n_sel = G["n_sel"]
    KSEL = n_sel // 128
    NIT = 20
    sc = Scope(cx)
    wq, wqb = load_w(cx, sc, w["w_in"][l], KD, OFF_AQ, OFF_AQ + 512, "wq")
    wiq, wiqb = load_w(cx, sc, w["w_in"][l], KD, OFF_IQ, OFF_IQ + 256, "wiq")
    wkk = sc.sb([128, KD, 128], BF16, "wkk")
    wik = sc.sb([128, KD, 128], BF16, "wik")
    for dc in (0, 64):
        load_w(cx, sc, w["w_in"][l], KD, OFF_AK, OFF_AK + 64, "", dst=wkk, dcol=dc)
        load_w(cx, sc, w["w_in"][l], KD, OFF_IK, OFF_IK + 64, "", dst=wik, dcol=dc)
    wv, wvb = load_w(cx, sc, w["w_in"][l], KD, OFF_AV, OFF_AV + 64, "wv")
    wiw, wiwb = load_w(cx, sc, w["w_in"][l], KD, OFF_IW, OFF_IW + 4, "wiw")
    pA, pAb = sc.ps([128, 2, TT], F32, "pA")
    pL = [sc.ps([128, 2, TT], F32, "pL") for _ in range(2)]
    pMT, pMTb = sc.ps([128, 8, 128], BF16, "pMT")
    pST = [sc.ps([128, 2, TT], F32, "pST") for _ in range(2)]
    pO = [sc.ps([128, 4, 128], F32, "pO") for _ in range(2)]
    tail = Tail(cx, sc, G, l, 2, w["attn_w_out"][l], 4, pST, pL)
    identb, identbb = make_identity(cx, sc, BF16, "identb")
    ident4, ident4b = sc.sb([128, 4, 128], BF16, "ident4")
    for h in range(4):
        OP(cx, "pool", "tensor_copy", out=ident4[:, h, :], in_=identb[:, :], R=[identbb], W=[ident4b])
    NEGU, NEGUb = sc.sb([128, 128], BF16, "NEGU")
    OP(cx, "pool", "memset", NEGU[:, :], -1000.0, W=[NEGUb])
    OP(cx, "pool", "affine_select", out=NEGU[:, :], in_=NEGU[:, :], pattern=[[1, 128]], compare_op=ALU.is_gt, fill=0.0, base=0,
       channel_multiplier=-1, R=[NEGUb], W=[NEGUb])
    LT, LTb = sc.sb([128, 128], BF16, "LT")
    OP(cx, "pool", "memset", LT[:, :], 1.0, W=[LTb])
    OP(cx, "pool", "affine_select", out=LT[:, :], in_=LT[:, :], pattern=[[-1, 128]], compare_op=ALU.is_ge, fill=0.0, base=0,
       channel_multiplier=1, R=[LTb], W=[LTb])
    hin = [sc.sb([128, KD, TT], BF16, "hin") for _ in range(2)]
    qT, qTb = sc.sb([128, 4, TT], BF16, "qT")
    qiT, qiTb = sc.sb([128, 2, TT], BF16, "qiT")
    kT2, kT2b = sc.sb([128, S], BF16, "kT2")
    kiT2, kiT2b = sc.sb([128, S], BF16, "kiT2")
    vpd, vpdb = sc.sb([128, S // 128, 65], BF16, "vpd")
    OP(cx, "pool", "memset", vpd[:, :, :], 1.0, W=[vpdb])
    wf, wfb = sc.sb([128, 4], F32, "wf")
    dg, dgb = sc.sb([128, 4, 128], BF16, "dg")
    Rl = [sc.sb([128, 512], BF16, "Rl") for _ in range(4)]
    scs, scsb = sc.sb([128, S], F32, "scs")
    jnk, jnkb = sc.sb([128, S], BF16, "jnk")
    Mb, Mbb = sc.sb([128, S], BF16, "Mb")
    lo, lob = sc.sb([128, 1], F32, "lo")
    w0, w0b = sc.sb([128, 1], F32, "w0")
    mid, midb = sc.sb([128, 1], F32, "mid")
    cnt, cntb = sc.sb([128, 1], F32, "cnt")
    stp, stpb = sc.sb([128, 1], F32, "stp")
    NEG4, NEG4b = sc.sb([128, 4, 4, 128], BF16, "NEG4")
    PT = [sc.sb([128, 512], BF16, "PT") for _ in range(2)]
    rden, rdenb = sc.sb([128, 2, 4], F32, "rden")
    ha, hab = sc.sb([128, 4, 2, 64], BF16, "ha")
    yaT, yaTb = sc.sb([128, 4, TT], BF16, "yaT")
    pAf = flat(pA)
    tiles_per_seq = S // TT

    def load(i):
        hT, hb = hin[i % 2]
        OP(cx, "sp", "dma_start", out=hT[:, :, :], in_=ht_ap(G, i), R=[G["HTb"][i]], W=[hb], chan="ld")
        tail.load_x(i)

    load(0)
    for i in range(G["NT"]):
        hT, hb = hin[i % 2]
        if i + 1 < G["NT"]:
            load(i + 1)
        ti = i % tiles_per_seq
        cs = slice(ti * TT, (ti + 1) * TT)
        for hp in range(2):
            for hh in range(2):
                c4 = hp * 2 + hh
                for c in range(KD):
                    OP(cx, "pe", "matmul", pA[:, hh, :], wq[:, c, c4 * 128:(c4 + 1) * 128], hT[:, c, :], start=(c == 0), stop=(c == KD - 1),
                       R=[wqb, hb], W=[pAb])
            OP(cx, "act", "mul", qT[:, 2 * hp:2 * hp + 2, :], pA[:, :, :], 0.125, R=[pAb], W=[qTb])
        for hh, (wt, wtb) in enumerate((wkk, wik)):
            for c in range(KD):
                OP(cx, "pe", "matmul", pA[:, hh, :], wt[:, c, :], hT[:, c, :], start=(c == 0), stop=(c == KD - 1), R=[wtb, hb], W=[pAb])
        OP(cx, "act", "copy", out=kT2[:, cs], in_=pA[:, 0, :], R=[pAb], W=[kT2b])
        OP(cx, "act", "copy", out=kiT2[:, cs], in_=pA[:, 1, :], R=[pAb], W=[kiT2b])
        for hh in range(2):
            for c in range(KD):
                OP(cx, "pe", "matmul", pA[:, hh, :], wiq[:, c, hh * 128:(hh + 1) * 128], hT[:, c, :], start=(c == 0), stop=(c == KD - 1),
                   R=[wiqb, hb], W=[pAb])
        OP(cx, "act", "copy", out=qiT[:, :, :], in_=pA[:, :, :], R=[pAb], W=[qiTb])
        for a in range(TT // 128):
            ts = slice(a * 128, (a + 1) * 128)
            qi = ti * 2 + a
            nk = qi + 1
            SV = nk * 128
            for c in range(KD):
                OP(cx, "pe", "matmul", pAf[:, 0:64], hT[:, c, ts], wv[:, c, :], start=(c == 0), stop=(c == KD - 1), R=[wvb, hb], W=[pAb])
            OP(cx, "act", "copy", out=vpd[:, qi, 0:64], in_=pAf[:, 0:64], R=[pAb], W=[vpdb])
            if qi >= KSEL:
                for c in range(KD):
                    OP(cx, "pe", "matmul", pAf[:, 64:68], hT[:, c, ts], wiw[:, c, :], start=(c == 0), stop=(c == KD - 1), R=[wiwb, hb], W=[pAb])
                OP(cx, "act", "mul", wf[:, :], pAf[:, 64:68], 1.0 / 16.0, R=[pAb], W=[wfb])
                OP(cx, "dve", "tensor_tensor", out=dg[:, :, :], in0=ident4[:, :, :], in1=wf[:, :].unsqueeze(2).to_broadcast([128, 4, 128]),
                   op=ALU.mult, R=[ident4b, wfb], W=[dgb])
                for c0 in range(0, SV, 512):
                    cw = min(512, SV - c0)
                    for h in range(4):
                        half, ch = h % 2, h // 2
                        ps_ = slice(half * 64, (half + 1) * 64)
                        pl, plb = pL[h % 2]
                        OP(cx, "pe", "matmul", flat(pl)[:, 0:cw], qiT[ps_, ch, ts], kiT2[ps_, c0:c0 + cw], start=True, stop=True,
                           R=[qiTb, kiT2b], W=[plb])
                        OP(cx, "act", "activation", out=Rl[h][0][:, 0:cw], in_=flat(pl)[:, 0:cw], func=AF.Relu, R=[plb], W=[Rl[h][1]])
                    dcol = qi * 128 - c0
                    has_diag = 0 <= dcol < cw
                    for h in range(4):
                        OP(cx, "pe", "matmul", pAf[:, 0:cw], dg[:, h, :], Rl[h][0][:, 0:cw], start=(h == 0), stop=(h == 3 and not has_diag),
                           R=[dgb, Rl[h][1]], W=[pAb])
                    if has_diag:
                        OP(cx, "pe", "matmul", pAf[:, dcol:dcol + 128], identb[:, :], NEGU[:, :], start=False, stop=True,
                           R=[identbb, NEGUb], W=[pAb])
                    OP(cx, "act", "copy", out=scs[:, c0:c0 + cw], in_=pAf[:, 0:cw], R=[pAb], W=[scsb])
                OP(cx, "dve", "tensor_reduce", out=lo[:, :], in_=scs[:, 0:qi * 128], axis=AX.X, op=ALU.min, R=[scsb], W=[lob])
                OP(cx, "dve", "tensor_reduce", out=w0[:, :], in_=scs[:, 0:SV], axis=AX.X, op=ALU.max, R=[scsb], W=[w0b])
                OP(cx, "dve", "tensor_tensor", out=w0[:, :], in0=w0[:, :], in1=lo[:, :], op=ALU.subtract, R=[w0b, lob], W=[w0b])
                for it in range(1, NIT + 1):
                    f = 2.0 ** -it
                    OP(cx, "dve", "scalar_tensor_tensor", out=mid[:, :], in0=w0[:, :], scalar=f, in1=lo[:, :], op0=ALU.mult, op1=ALU.add,
                       R=[w0b, lob], W=[midb])
                    OP(cx, "dve", "tensor_scalar", out=jnk[:, 0:SV], in0=scs[:, 0:SV], scalar1=mid[:, 0:1], scalar2=0.0, op0=ALU.is_ge,
                       op1=ALU.add, accum_out=cnt[:, 0:1], R=[scsb, midb], W=[jnkb, cntb])
                    OP(cx, "dve", "tensor_scalar", out=stp[:, :], in0=cnt[:, :], scalar1=float(n_sel) - 0.5, scalar2=f, op0=ALU.is_ge,
                       op1=ALU.mult, R=[cntb], W=[stpb])
                    OP(cx, "dve", "scalar_tensor_tensor", out=lo[:, :], in0=stp[:, :], scalar=w0[:, 0:1], in1=lo[:, :], op0=ALU.mult,
                       op1=ALU.add, R=[stpb, w0b, lob], W=[lob])
                OP(cx, "dve", "tensor_scalar", out=Mb[:, 0:SV], in0=scs[:, 0:SV], scalar1=lo[:, 0:1], scalar2=None, op0=ALU.is_ge,
                   R=[scsb, lob], W=[Mbb])
            else:
                if qi > 0:
                    OP(cx, "pool", "memset", Mb[:, 0:qi * 128], 1.0, W=[Mbb])
                OP(cx, "pool", "tensor_copy", out=Mb[:, qi * 128:SV], in_=LT[:, :], R=[LTb], W=[Mbb])
            for g0 in range(0, nk, 4):
                kts = list(range(g0, min(nk, g0 + 4)))
                ng = len(kts)
                for j, kt in enumerate(kts):
                    OP(cx, "pe", "transpose", pMT[:, j, :], Mb[:, kt * 128:(kt + 1) * 128], identb[:, :], R=[Mbb, identbb], W=[pMTb])
                OP(cx, "dve", "tensor_scalar", out=NEG4[:, 0:ng, :, :], in0=pMT[:, 0:ng, :].unsqueeze(2).to_broadcast([128, ng, 4, 128]),
                   scalar1=1.0, scalar2=30000.0, op0=ALU.subtract, op1=ALU.mult, R=[pMTb], W=[NEG4b])
                for j, kt in enumerate(kts):
                    for half in range(2):
                        ps_ = slice(half * 64, (half + 1) * 64)
                        pst, pstb = pST[half]
                        OP(cx, "pe", "matmul", flat(pst)[:, :], kT2[ps_, kt * 128:(kt + 1) * 128], qT[ps_, :, ts], start=True, stop=False,
                           R=[kT2b, qTb], W=[pstb])
                        OP(cx, "pe", "matmul", flat(pst)[:, :], identb[:, :], NEG4[:, j, :, :], start=False, stop=True,
                           R=[identbb, NEG4b], W=[pstb])
                        pt_, ptb_ = PT[half]
                        OP(cx, "act", "activation", out=pt_[:, :], in_=flat(pst)[:, :], func=AF.Exp, R=[pstb], W=[ptb_])
                        po, pob = pO[half]
                        for c in range(4):
                            OP(cx, "pe", "matmul", po[:, c, 0:65], pt_[:, c * 128:(c + 1) * 128], vpd[:, kt, :], start=(kt == 0 and c == 0),
                               stop=(kt == qi), skip_group_check=True, R=[ptb_, vpdb], W=[pob])
            for half in range(2):
                po, pob = pO[half]
                OP(cx, "dve", "reciprocal", out=rden[:, half, :], in_=po[:, :, 64], R=[pob], W=[rdenb])
                OP(cx, "dve", "tensor_tensor", out=ha[:, :, half, :], in0=po[:, :, 0:64],
                   in1=rden[:, half, :].unsqueeze(2).to_broadcast([128, 4, 64]), op=ALU.mult, R=[pob, rdenb], W=[hab])
            haf = ha[:, :, :, :].rearrange("p c h d -> p (c h d)")
            for c in range(4):
                OP(cx, "pe", "transpose", pMT[:, 4 + c, :], haf[:, c * 128:(c + 1) * 128], identb[:, :], R=[hab, identbb], W=[pMTb])
            OP(cx, "act", "copy", out=yaT[:, :, ts], in_=pMT[:, 4:8, :], R=[pMTb], W=[yaTb])
        tail.run(i, hT, hb, yaT, yaTb)
    sc.close()


WNAMES = ["norm_ffn1", "ffn1_w_gu", "ffn1_w_down", "norm_mix", "w_in", "conv_w", "conv_w_out", "mlstm_b_i", "mlstm_b_f",
          "mlstm_norm", "mlstm_w_out", "attn_w_out", "w_o", "norm_ffn2", "ffn2_w_gu", "ffn2_w_down", "norm_ple",
          "ple_w_gate", "ple_w_proj", "final_norm"]


def build(S, nseq, depth, wshapes, phases=None, n_sel=256, dbg=None):
    nc = bass.Bass("TRN2", target_bir_lowering=False)
    NTC = S * nseq
    TT = 256
    G = {"S": S, "nseq": nseq, "NTC": NTC, "TT": TT, "NT": NTC // TT, "depth": depth, "n_sel": n_sel}
    G.update(dbg or {})
    G["x"] = nc.dram_tensor("x", [NTC, D], F32, kind="ExternalInput").ap()
    G["p"] = nc.dram_tensor("p", [depth, NTC, D_PLE], F32, kind="ExternalInput").ap()
    G["w"] = {k: nc.dram_tensor(k, list(wshapes[k]), F32, kind="ExternalInput").ap() for k in WNAMES}
    G["out"] = nc.dram_tensor("out", [NTC, D], F32, kind="ExternalOutput").ap()
    G["outb"] = Buf("out")
    if G.get("dbg_dump"):
        G["dbgo"] = nc.dram_tensor("dbgo", [128, 4096], F32, kind="ExternalOutput").ap()
    G["XT"] = nc.dram_tensor("XT", [D, NTC], F32).ap()
    G["XTb"] = [Buf("XT%d" % i) for i in range(G["NT"])]
    G["HT"] = nc.dram_tensor("HT", [D, NTC], BF16).ap()
    G["HTb"] = [Buf("HT%d" % i) for i in range(G["NT"])]
    with contextlib.ExitStack() as es:
        cx = Ctx(nc, es)
        phase_in(cx, G)
        for l in range(depth):
            if phases is None or "ffn1" in phases:
                phase_ffn(cx, G, l, 1)
            if phases is None or "conv" in phases or "mlstm" in phases or "dsa" in phases:
                phase_norm(cx, G, l)
            if phases is None or "conv" in phases:
                phase_conv(cx, G, l)
            if phases is None or "mlstm" in phases:
                phase_mlstm(cx, G, l)
            if phases is None or "dsa" in phases:
                phase_dsa(cx, G, l)
            if phases is None or "ffn2" in phases:
                phase_ffn(cx, G, l, 2)
            if phases is None or "ple" in phases:
                phase_ple(cx, G, l)
        phase_out(cx, G)
        cx.P.add("sp", lambda e: e.nop(), R=[G["outb"]], chan=None) if False else None
        cx.P.barrier()
        cx.P.emit()
    return nc


def kernel(**inputs):
    x = np.ascontiguousarray(inputs["x"], dtype=np.float32)
    p = np.ascontiguousarray(inputs["p"], dtype=np.float32)
    B, S, _ = x.shape
    depth = p.shape[0]
    nseq = B // N_CORES
    wshapes = {k: inputs[k].shape for k in WNAMES}
    nc = build(S, nseq, depth, wshapes)
    in_maps = []
    for c in range(N_CORES):
        m = {k: np.ascontiguousarray(inputs[k], dtype=np.float32) for k in WNAMES}
        m["x"] = x[c * nseq:(c + 1) * nseq].reshape(nseq * S, D)
        m["p"] = np.ascontiguousarray(p[:, c * nseq:(c + 1) * nseq].reshape(depth, nseq * S, D_PLE))
        in_maps.append(m)
    res = run_bass_kernel_spmd(nc, in_maps, core_ids=list(range(N_CORES)))
    out = np.concatenate([r["out"].reshape(nseq, S, D) for r in res.results], axis=0)
    return out.astype(np.float32)
```

```python
import contextlib
import numpy as np
import concourse.bass as bass
import concourse.mybir as mybir
from concourse.bass_utils import run_bass_kernel_spmd

F32 = mybir.dt.float32
BF16 = mybir.dt.bfloat16
AF = mybir.ActivationFunctionType
ALU = mybir.AluOpType
AX = mybir.AxisListType

D = 1024
KD = 8
FF = 2816
KF = 22
D_PLE = 256
N_IN = 8652
EPS = 1e-6
N_CORES = 8


class Buf:
    __slots__ = ("w", "r", "name", "excl")

    def __init__(self, name="", excl=False):
        self.w = None
        self.r = {}
        self.name = name
        self.excl = excl


class Stream:
    def __init__(self, name, sem, inc):
        self.name, self.sem, self.inc = name, sem, inc
        self.ops = []


class Op:
    __slots__ = ("fn", "waits", "sig", "stream", "sigcount")

    def __init__(self, fn, stream):
        self.fn, self.stream = fn, stream
        self.waits = {}
        self.sig = False
        self.sigcount = 0


class Prog:
    ENGS = ("pe", "act", "dve", "pool", "sp")

    def __init__(self, nc, es):
        self.nc = nc
        self.es = es
        self.q = {e: [] for e in self.ENGS}
        self.seen = {e: {} for e in self.ENGS}
        self.streams = {}
        self.chan_n = {}
        for e in ("pe", "act", "dve", "pool"):
            self.new_stream(e, 1)

    def new_stream(self, name, inc=16):
        sem = self.es.enter_context(self.nc.semaphore("s_" + name))
        self.streams[name] = Stream(name, sem, inc)
        return self.streams[name]

    NSLOT = 8

    def new_channel(self, name):
        self.chan_n[name] = 0
        for k in range(self.NSLOT):
            self.new_stream("%s%d" % (name, k), 16)

    def add(self, eng, fn, R=(), W=(), chan=None):
        if chan is not None:
            k = self.chan_n[chan]
            self.chan_n[chan] = k + 1
            st = self.streams["%s%d" % (chan, k % self.NSLOT)]
        else:
            st = self.streams[eng]
        op = Op(fn, st)
        if st.inc == 16:
            op.sig = True
        st.ops.append(op)
        idx = len(st.ops)
        seen = self.seen[eng]
        waits = op.waits

        def need(dep):
            if dep is None:
                return
            s, j = dep
            if s is st and eng == "pe":
                return
            if seen.get(s, 0) >= j:
                return
            if waits.get(s, 0) < j:
                waits[s] = j

        if st.inc == 16 and idx > 1:
            need((st, idx - 1))
        for b in R:
            need(b.w)
            if b.excl:
                for s, j in b.r.items():
                    if s is not st:
                        need((s, j))
        for b in W:
            need(b.w)
            for s, j in b.r.items():
                if s is st and st.inc == 1:
                    continue
                need((s, j))
        for s, j in waits.items():
            seen[s] = j
            s.ops[j - 1].sig = True
        for b in R:
            if b.r.get(st, 0) < idx:
                b.r[st] = idx
        for b in W:
            b.w = (st, idx)
            b.r = {}
        self.q[eng].append(op)
        return op

    def barrier(self):
        tails = [(s, len(s.ops)) for s in self.streams.values() if s.ops]
        for e in self.ENGS:
            op = Op(None, None)
            seen = self.seen[e]
            for s, j in tails:
                if seen.get(s, 0) >= j:
                    continue
                op.waits[s] = j
                seen[s] = j
                s.ops[j - 1].sig = True
            self.q[e].append(op)

    def emit(self):
        for st in self.streams.values():
            c = 0
            for op in st.ops:
                if op.sig:
                    c += 1
                op.sigcount = c
        nc = self.nc
        q = self.q

        def run(e, ops):
            for op in ops:
                for s, j in op.waits.items():
                    e.wait_ge(s.sem, s.ops[j - 1].sigcount * s.inc)
                if op.fn is None:
                    continue
                ins = op.fn(e)
                if op.sig:
                    ins.then_inc(op.stream.sem, op.stream.inc)

        with nc.Block() as block:
            @block.tensor
            def _(e):
                run(e, q["pe"])

            @block.scalar
            def _(e):
                run(e, q["act"])

            @block.vector
            def _(e):
                run(e, q["dve"])

            @block.gpsimd
            def _(e):
                run(e, q["pool"])

            @block.sync
            def _(e):
                run(e, q["sp"])


class Ctx:
    def __init__(self, nc, es):
        self.nc, self.es = nc, es
        self.P = Prog(nc, es)
        self.P.new_channel("ld")
        self.P.new_channel("st")
        self.P.new_channel("wl")
        self.P.new_channel("pl")
        self._n = 0

    def sb(self, shape, dt, name=None):
        self._n += 1
        return self.es.enter_context(self.nc.sbuf_tensor(f"{name or 't'}_{self._n}", list(shape), dt))

    def ps(self, shape, dt=F32, name=None):
        self._n += 1
        return self.es.enter_context(self.nc.psum_tensor(f"{name or 'p'}_{self._n}", list(shape), dt))


class Scope:
    def __init__(self, cx):
        self.cx = cx
        self.es = contextlib.ExitStack()
        self.bufs = []

    def sb(self, shape, dt, name="t"):
        cx = self.cx
        cx._n += 1
        t = self.es.enter_context(cx.nc.sbuf_tensor(f"{name}_{cx._n}", list(shape), dt))
        b = Buf(name)
        self.bufs.append(b)
        return t, b

    def ps(self, shape, dt=F32, name="p"):
        cx = self.cx
        cx._n += 1
        t = self.es.enter_context(cx.nc.psum_tensor(f"{name}_{cx._n}", list(shape), dt))
        b = Buf(name, excl=True)
        self.bufs.append(b)
        return t, b

    def close(self):
        self.cx.P.barrier()
        self.es.close()


def load_w(cx, sc, w2d, kc, n0, n1, name, dst=None, dcol=0):
    P = cx.P
    n = n1 - n0
    if dst is None:
        t, b = sc.sb([128, kc, n], BF16, name)
    else:
        t, b = dst
    for c in range(kc):
        P.add("pool", lambda e, c=c: e.dma_start(out=t[:, c, dcol:dcol + n], in_=w2d[c * 128:(c + 1) * 128, n0:n1]),
              W=[b], chan="pl")
    return t, b


def load_vec_cols(cx, sc, v1d, kc, name):
    t, b = sc.sb([128, kc], F32, name)
    cx.P.add("sp", lambda e: e.dma_start(out=t[:, :], in_=v1d.rearrange("(c p) -> p c", p=128),
                                         allow_slow_non_contiguous=True), W=[b], chan="wl")
    return t, b


def rmsnorm_T(cx, sc, st, xt, xb, g, gb, TT):
    P = cx.P
    sq, sqb = st["sq"]
    hT, hb = st["hT"]
    ssp, sspb = st["ssp"]
    rs, rsb = st["rs"]
    ones, onesb = st["ones"]
    P.add("act", lambda e: e.activation(out=sq[:, :, :], in_=xt[:, :, :], func=AF.Square), R=[xb], W=[sqb])
    for c in range(KD):
        P.add("pe", lambda e, c=c: e.matmul(ssp[:, 0:TT], ones[:, :], sq[:, c, :], start=(c == 0), stop=(c == KD - 1)),
              R=[sqb, onesb], W=[sspb])
    P.add("act", lambda e: e.activation(out=rs[:, 0:TT], in_=ssp[:, 0:TT], func=AF.Sqrt, scale=1.0 / D, bias=st["eps"][0][:, 0:1]),
          R=[sspb, st["eps"][1]], W=[rsb])
    P.add("dve", lambda e: e.reciprocal(out=rs[:, 0:TT], in_=rs[:, 0:TT]), R=[rsb], W=[rsb])
    for c in range(KD):
        P.add("dve", lambda e, c=c: e.scalar_tensor_tensor(out=hT[:, c, :], in0=xt[:, c, :], scalar=g[:, c:c + 1],
                                                           in1=rs[:, 0:TT], op0=ALU.mult, op1=ALU.mult),
              R=[xb, gb, rsb], W=[hb])
    return hT, hb


def norm_scratch(cx, sc, TT):
    st = {}
    st["sq"] = sc.sb([128, KD, TT], BF16, "sq")
    st["hT"] = sc.sb([128, KD, TT], BF16, "hT")
    st["ssp"] = sc.ps([128, 512], F32, "ssp")
    st["rs"] = sc.sb([128, TT], F32, "rs")
    st["ones"] = sc.sb([128, 128], BF16, "ones")
    st["eps"] = sc.sb([128, 1], F32, "eps")
    ones, onesb = st["ones"]
    eps, epsb = st["eps"]
    cx.P.add("pool", lambda e: e.memset(ones[:, :], 1.0), W=[onesb])
    cx.P.add("pool", lambda e: e.memset(eps[:, :], EPS), W=[epsb])
    return st


def make_identity(cx, sc, dt, name="ident"):
    t, b = sc.sb([128, 128], dt, name)
    cx.P.add("pool", lambda e: e.memset(t[:, :], 1.0), W=[b])
    cx.P.add("pool", lambda e: e.affine_select(out=t[:, :], in_=t[:, :], pattern=[[-1, 128]], compare_op=ALU.is_equal,
                                               fill=0.0, base=0, channel_multiplier=1), R=[b], W=[b])
    return t, b


def phase_in(cx, G):
    P = cx.P
    TT = G["TT"]
    sc = Scope(cx)
    ident, identb = make_identity(cx, sc, F32)
    xin = [sc.sb([128, TT // 128, D], F32, "xin") for _ in range(2)]
    xo = [sc.sb([128, KD, TT], F32, "xo") for _ in range(2)]
    pp = [sc.ps([128, 2, TT], F32, "tp") for _ in range(4)]
    x = G["x"]
    for i in range(G["NT"]):
        xi, xib = xin[i % 2]
        xo_t, xob = xo[i % 2]
        P.add("sp", lambda e, i=i, xi=xi: e.dma_start(
            out=xi[:, :, :], in_=x[i * TT:(i + 1) * TT, :].rearrange("(a p) d -> p a d", p=128)), W=[xib], chan="ld")
        for c2 in range(KD // 2):
            pt, ptb = pp[c2 % 4]
            for cc in range(2):
                c = c2 * 2 + cc
                for a in range(TT // 128):
                    P.add("pe", lambda e, c=c, cc=cc, a=a, pt=pt, xi=xi: e.transpose(
                        pt[:, cc, a * 128:(a + 1) * 128], xi[:, a, c * 128:(c + 1) * 128], ident[:, :]),
                        R=[xib, identb], W=[ptb])
            eng = "act" if c2 % 2 == 0 else "dve"
            if eng == "act":
                P.add("act", lambda e, c2=c2, pt=pt, xo_t=xo_t: e.copy(out=xo_t[:, 2 * c2:2 * c2 + 2, :], in_=pt[:, :, :]),
                      R=[ptb], W=[xob])
            else:
                P.add("dve", lambda e, c2=c2, pt=pt, xo_t=xo_t: e.tensor_copy(out=xo_t[:, 2 * c2:2 * c2 + 2, :], in_=pt[:, :, :]),
                      R=[ptb], W=[xob])
        P.add("sp", lambda e, i=i, xo_t=xo_t: e.dma_start(
            out=G["XT"][:, i * TT:(i + 1) * TT].rearrange("(c p) t -> p c t", p=128), in_=xo_t[:, :, :]),
            R=[xob], W=[G["XTb"][i]], chan="st")
    sc.close()


def phase_out(cx, G):
    P = cx.P
    TT = G["TT"]
    sc = Scope(cx)
    ident, identb = make_identity(cx, sc, F32)
    st = norm_scratch(cx, sc, TT)
    g, gb = load_vec_cols(cx, sc, G["w"]["final_norm"], KD, "gfin")
    xin = [sc.sb([128, KD, TT], F32, "xin") for _ in range(2)]
    yn = sc.sb([128, KD, TT], F32, "yn")
    yo = [sc.sb([128, D], F32, "yo") for _ in range(2)]
    pp = [sc.ps([128, 512], F32, "tp") for _ in range(4)]
    rs, rsb = st["rs"]
    sq, sqb = st["sq"]
    ssp, sspb = st["ssp"]
    ones, onesb = st["ones"]
    k = 0
    for i in range(G["NT"]):
        xt, xb = xin[i % 2]
        P.add("sp", lambda e, i=i, xt=xt: e.dma_start(
            out=xt[:, :, :], in_=G["XT"][:, i * TT:(i + 1) * TT].rearrange("(c p) t -> p c t", p=128)),
            R=[G["XTb"][i]], W=[xb], chan="ld")
        P.add("act", lambda e, xt=xt: e.activation(out=sq[:, :, :], in_=xt[:, :, :], func=AF.Square), R=[xb], W=[sqb])
        for c in range(KD):
            P.add("pe", lambda e, c=c: e.matmul(ssp[:, 0:TT], ones[:, :], sq[:, c, :], start=(c == 0), stop=(c == KD - 1)),
                  R=[sqb, onesb], W=[sspb])
        P.add("act", lambda e: e.activation(out=rs[:, 0:TT], in_=ssp[:, 0:TT], func=AF.Sqrt, scale=1.0 / D, bias=st["eps"][0][:, 0:1]),
              R=[sspb, st["eps"][1]], W=[rsb])
        P.add("dve", lambda e: e.reciprocal(out=rs[:, 0:TT], in_=rs[:, 0:TT]), R=[rsb], W=[rsb])
        y, yb = yn
        for c in range(KD):
            P.add("dve", lambda e, c=c, xt=xt: e.scalar_tensor_tensor(out=y[:, c, :], in0=xt[:, c, :], scalar=g[:, c:c + 1],
                                                                      in1=rs[:, 0:TT], op0=ALU.mult, op1=ALU.mult),
                  R=[xb, gb, rsb], W=[yb])
        for a in range(TT // 128):
            yt, ytb = yo[k % 2]
            for hh in range(2):
                pt, ptb = pp[(2 * k + hh) % 4]
                for c4 in range(4):
                    c = hh * 4 + c4
                    P.add("pe", lambda e, c=c, c4=c4, a=a, pt=pt: e.transpose(
                        pt[:, c4 * 128:(c4 + 1) * 128], y[:, c, a * 128:(a + 1) * 128], ident[:, :]),
                        R=[yb, identb], W=[ptb])
                if hh == 0:
                    P.add("act", lambda e, pt=pt, yt=yt: e.copy(out=yt[:, 0:512], in_=pt[:, :]), R=[ptb], W=[ytb])
                else:
                    P.add("dve", lambda e, pt=pt, yt=yt: e.tensor_copy(out=yt[:, 512:1024], in_=pt[:, :]), R=[ptb], W=[ytb])
            r0 = i * TT + a * 128
            P.add("sp", lambda e, r0=r0, yt=yt: e.dma_start(out=G["out"][r0:r0 + 128, :], in_=yt[:, :]),
                  R=[ytb], W=[G["outb"]], chan="st")
            k += 1
    sc.close()


def phase_ffn(cx, G, l, which):
    P = cx.P
    TT = G["TT"]
    w = G["w"]
    sc = Scope(cx)
    pre = "ffn1" if which == 1 else "ffn2"
    wgu, wgub = load_w(cx, sc, w[pre + "_w_gu"][l], KD, 0, 2 * FF, "wgu")
    wd, wdb = load_w(cx, sc, w[pre + "_w_down"][l], KF, 0, D, "wd")
    g, gb = load_vec_cols(cx, sc, w["norm_" + pre][l], KD, "gn")
    st = norm_scratch(cx, sc, TT)
    xin = [sc.sb([128, KD, TT], F32, "xin") for _ in range(2)]
    m, mb = sc.sb([128, KF, TT], BF16, "m")
    av = [sc.sb([128, TT], F32, "a") for _ in range(2)]
    pgu = [sc.ps([128, 2, TT], F32, "pgu") for _ in range(3)]
    pdn = [sc.ps([128, 2, TT], F32, "pdn") for _ in range(2)]

    def load(i):
        xt, xb = xin[i % 2]
        P.add("sp", lambda e: e.dma_start(
            out=xt[:, :, :], in_=G["XT"][:, i * TT:(i + 1) * TT].rearrange("(c p) t -> p c t", p=128)),
            R=[G["XTb"][i]], W=[xb], chan="ld")

    load(0)
    for i in range(G["NT"]):
        xt, xb = xin[i % 2]
        if i + 1 < G["NT"]:
            load(i + 1)
        hT, hb = rmsnorm_T(cx, sc, st, xt, xb, g, gb, TT)
        for j in range(KF):
            pg, pgb = pgu[j % 3]
            for half in range(2):
                col = half * FF + j * 128
                for c in range(KD):
                    P.add("pe", lambda e, c=c, col=col, half=half, pg=pg: e.matmul(
                        pg[:, half, :], wgu[:, c, col:col + 128], hT[:, c, :], start=(c == 0), stop=(c == KD - 1)),
                        R=[wgub, hb], W=[pgb])
            a, ab = av[j % 2]
            P.add("act", lambda e, pg=pg, a=a: e.activation(out=a[:, :], in_=pg[:, 0, :], func=AF.Silu), R=[pgb], W=[ab])
            P.add("dve", lambda e, pg=pg, a=a, j=j: e.tensor_tensor(out=m[:, j, :], in0=a[:, :], in1=pg[:, 1, :], op=ALU.mult),
                  R=[pgb, ab], W=[mb])
        for o2 in range(KD // 2):
            pd, pdb = pdn[o2 % 2]
            for oo in range(2):
                oc = o2 * 2 + oo
                for j in range(KF):
                    P.add("pe", lambda e, j=j, oc=oc, oo=oo, pd=pd: e.matmul(
                        pd[:, oo, :], wd[:, j, oc * 128:(oc + 1) * 128], m[:, j, :], start=(j == 0), stop=(j == KF - 1)),
                        R=[wdb, mb], W=[pdb])
            P.add("dve", lambda e, o2=o2, pd=pd, xt=xt: e.scalar_tensor_tensor(
                out=xt[:, 2 * o2:2 * o2 + 2, :], in0=pd[:, :, :], scalar=0.5, in1=xt[:, 2 * o2:2 * o2 + 2, :],
                op0=ALU.mult, op1=ALU.add), R=[pdb, xb], W=[xb])
        P.add("sp", lambda e, i=i, xt=xt: e.dma_start(
            out=G["XT"][:, i * TT:(i + 1) * TT].rearrange("(c p) t -> p c t", p=128), in_=xt[:, :, :]),
            R=[xb], W=[G["XTb"][i]], chan="st")
    sc.close()


OFF_CB, OFF_CC, OFF_CX = 0, 512, 1024
OFF_MQ, OFF_MK, OFF_MV, OFF_MO, OFF_MI, OFF_MF = 1536, 2048, 2560, 3584, 4608, 4612
OFF_AQ, OFF_AK, OFF_AV, OFF_IQ, OFF_IK, OFF_IW = 4616, 5128, 5192, 5256, 5512, 5576
OFF_G = 5580


def xt_ap(G, i):
    TT = G["TT"]
    return G["XT"][:, i * TT:(i + 1) * TT].rearrange("(c p) t -> p c t", p=128)


def ht_ap(G, i):
    TT = G["TT"]
    return G["HT"][:, i * TT:(i + 1) * TT].rearrange("(c p) t -> p c t", p=128)


def phase_norm(cx, G, l):
    P = cx.P
    TT = G["TT"]
    sc = Scope(cx)
    g, gb = load_vec_cols(cx, sc, G["w"]["norm_mix"][l], KD, "gn")
    st = norm_scratch(cx, sc, TT)
    xin = [sc.sb([128, KD, TT], F32, "xin") for _ in range(2)]
    ho = [sc.sb([128, KD, TT], BF16, "ho") for _ in range(2)]
    for i in range(G["NT"]):
        xt, xb = xin[i % 2]
        P.add("sp", lambda e, i=i, xt=xt: e.dma_start(out=xt[:, :, :], in_=xt_ap(G, i)), R=[G["XTb"][i]], W=[xb], chan="ld")
        st["hT"] = ho[i % 2]
        hT, hb = rmsnorm_T(cx, sc, st, xt, xb, g, gb, TT)
        P.add("sp", lambda e, i=i, hT=hT: e.dma_start(out=ht_ap(G, i), in_=hT[:, :, :]), R=[hb], W=[G["HTb"][i]], chan="st")
    sc.close()


class Tail:
    def __init__(self, cx, sc, G, l, br, wout2d, kin, psA, psB):
        w = G["w"]
        TT = G["TT"]
        self.cx, self.G, self.kin = cx, G, kin
        self.wg = load_w(cx, sc, w["w_in"][l], KD, OFF_G + br * D, OFF_G + (br + 1) * D, "wgate")
        self.wout = load_w(cx, sc, wout2d, kin, 0, D, "wout")
        self.wo = load_w(cx, sc, w["w_o"][l], KD, 0, D, "wo")
        self.sg = [sc.sb([128, TT], F32, "sg") for _ in range(2)]
        self.mg = sc.sb([128, KD, TT], BF16, "mg")
        self.xin = [sc.sb([128, KD, TT], F32, "xres") for _ in range(2)]
        self.psA, self.psB = psA, psB

    def load_x(self, i):
        G = self.G
        xt, xb = self.xin[i % 2]
        self.cx.P.add("sp", lambda e: e.dma_start(out=xt[:, :, :], in_=xt_ap(G, i)), R=[G["XTb"][i]], W=[xb], chan="ld")

    def run(self, i, hT, hb, yT, yb):
        P = self.cx.P
        G = self.G
        wg, wgb = self.wg
        wout, woutb = self.wout
        wo, wob = self.wo
        mg, mgb = self.mg
        xt, xb = self.xin[i % 2]
        kin = self.kin
        for oc in range(KD):
            pa, pab = self.psA[oc % len(self.psA)]
            for c in range(kin):
                P.add("pe", lambda e, c=c, oc=oc, pa=pa: e.matmul(pa[:, 0, :], wout[:, c, oc * 128:(oc + 1) * 128], yT[:, c, :],
                                                               start=(c == 0), stop=(c == kin - 1)), R=[woutb, yb], W=[pab])
            for c in range(KD):
                P.add("pe", lambda e, c=c, oc=oc, pa=pa: e.matmul(pa[:, 1, :], wg[:, c, oc * 128:(oc + 1) * 128], hT[:, c, :],
                                                               start=(c == 0), stop=(c == KD - 1)), R=[wgb, hb], W=[pab])
            sg, sgb = self.sg[oc % 2]
            P.add("act", lambda e, pa=pa, sg=sg: e.activation(out=sg[:, :], in_=pa[:, 1, :], func=AF.Sigmoid), R=[pab], W=[sgb])
            P.add("dve", lambda e, pa=pa, sg=sg, oc=oc: e.tensor_tensor(out=mg[:, oc, :], in0=sg[:, :], in1=pa[:, 0, :], op=ALU.mult),
                  R=[pab, sgb], W=[mgb])
        for o2 in range(KD // 2):
            po, pob = self.psB[o2 % len(self.psB)]
            for oo in range(2):
                oc = o2 * 2 + oo
                for c in range(KD):
                    P.add("pe", lambda e, c=c, oc=oc, oo=oo, po=po: e.matmul(po[:, oo, :], wo[:, c, oc * 128:(oc + 1) * 128], mg[:, c, :],
                                                                         start=(c == 0), stop=(c == KD - 1)), R=[wob, mgb], W=[pob])
            P.add("dve", lambda e, o2=o2, po=po: e.tensor_tensor(out=xt[:, 2 * o2:2 * o2 + 2, :], in0=xt[:, 2 * o2:2 * o2 + 2, :],
                                                              in1=po[:, :, :], op=ALU.add), R=[pob, xb], W=[xb])
        P.add("sp", lambda e: e.dma_start(out=xt_ap(G, i), in_=xt[:, :, :]), R=[xb], W=[G["XTb"][i]], chan="st")


def phase_conv(cx, G, l):
    P = cx.P
    TT = G["TT"]
    w = G["w"]
    sc = Scope(cx)
    win, winb = load_w(cx, sc, w["w_in"][l], KD, 0, 1536, "winc")
    psA = [sc.ps([128, 2, TT], F32, "psA") for _ in range(2)]
    psB = [sc.ps([128, 2, TT], F32, "psB") for _ in range(2)]
    pcx = [sc.ps([128, 2, TT], F32, "pcx") for _ in range(2)]
    pb_ = [sc.ps([128, 2, TT], F32, "pbb") for _ in range(2)]
    tail = Tail(cx, sc, G, l, 0, w["conv_w_out"][l], 4, psA, psB)
    cw, cwb = sc.sb([128, 4, 3], F32, "cw")
    for j in range(3):
        P.add("sp", lambda e, j=j: e.dma_start(out=cw[:, :, j], in_=w["conv_w"][l][j].rearrange("(c p) -> p c", p=128),
                                               allow_slow_non_contiguous=True), W=[cwb], chan="wl")
    hin = [sc.sb([128, KD, TT], BF16, "hin") for _ in range(2)]
    u = [sc.sb([128, TT + 2], F32, "u") for _ in range(4)]
    ccs = [sc.sb([128, TT], F32, "ccs") for _ in range(2)]
    yv = [sc.sb([128, TT], F32, "yv") for _ in range(2)]
    z, zb = sc.sb([128, 4, TT], BF16, "z")
    tiles_per_seq = G["S"] // TT

    def load(i):
        hT, hb = hin[i % 2]
        P.add("sp", lambda e: e.dma_start(out=hT[:, :, :], in_=ht_ap(G, i)), R=[G["HTb"][i]], W=[hb], chan="ld")
        tail.load_x(i)

    load(0)
    for i in range(G["NT"]):
        hT, hb = hin[i % 2]
        if i + 1 < G["NT"]:
            load(i + 1)
        for q in range(4):
            ut, ub = u[q]
            if i % tiles_per_seq == 0:
                P.add("pool", lambda e, ut=ut: e.memset(ut[:, 0:2], 0.0), W=[ub])
            p1, p1b = pcx[q % 2]
            p2, p2b = pb_[q % 2]
            for k_, (pt, ptb, slot, off) in enumerate(((p1, p1b, 0, OFF_CC), (p1, p1b, 1, OFF_CX), (p2, p2b, 0, OFF_CB))):
                for c in range(KD):
                    P.add("pe", lambda e, c=c, pt=pt, slot=slot, col=off + q * 128, hT=hT: e.matmul(
                        pt[:, slot, :], win[:, c, col:col + 128], hT[:, c, :], start=(c == 0), stop=(c == KD - 1)),
                        R=[winb, hb], W=[ptb])
            cs, csb = ccs[q % 2]
            P.add("act", lambda e, p1=p1, cs=cs: e.copy(out=cs[:, :], in_=p1[:, 0, :]), R=[p1b], W=[csb])
            P.add("dve", lambda e, p1=p1, cs=cs, ut=ut: e.tensor_tensor(out=ut[:, 2:TT + 2], in0=cs[:, :], in1=p1[:, 1, :], op=ALU.mult),
                  R=[p1b, csb], W=[ub])
            y, yb_ = yv[q % 2]
            P.add("dve", lambda e, y=y, ut=ut, q=q: e.tensor_scalar(out=y[:, :], in0=ut[:, 0:TT], scalar1=cw[:, q, 0:1], scalar2=None,
                                                                  op0=ALU.mult), R=[ub, cwb], W=[yb_])
            P.add("dve", lambda e, y=y, ut=ut, q=q: e.scalar_tensor_tensor(out=y[:, :], in0=ut[:, 1:TT + 1], scalar=cw[:, q, 1:2],
                                                                         in1=y[:, :], op0=ALU.mult, op1=ALU.add), R=[ub, cwb, yb_], W=[yb_])
            P.add("dve", lambda e, y=y, ut=ut, q=q: e.scalar_tensor_tensor(out=y[:, :], in0=ut[:, 2:TT + 2], scalar=cw[:, q, 2:3],
                                                                         in1=y[:, :], op0=ALU.mult, op1=ALU.add), R=[ub, cwb, yb_], W=[yb_])
            P.add("pool", lambda e, ut=ut: e.tensor_copy(out=ut[:, 0:2], in_=ut[:, TT:TT + 2]), R=[ub], W=[ub])
            P.add("dve", lambda e, y=y, p2=p2, q=q: e.tensor_tensor(out=z[:, q, :], in0=y[:, :], in1=p2[:, 0, :], op=ALU.mult),
                  R=[yb_, p2b], W=[zb])
        tail.run(i, hT, hb, z, zb)
    sc.close()


def phase_ple(cx, G, l):
    P = cx.P
    TT = G["TT"]
    w = G["w"]
    sc = Scope(cx)
    wg, wgb = load_w(cx, sc, w["ple_w_gate"][l], KD, 0, D, "wpg")
    wp, wpb = load_w(cx, sc, w["ple_w_proj"][l], 2, 0, D, "wpp")
    g, gb = load_vec_cols(cx, sc, w["norm_ple"][l], KD, "gn")
    st = norm_scratch(cx, sc, TT)
    ident, identb = make_identity(cx, sc, F32)
    xin = [sc.sb([128, KD, TT], F32, "xin") for _ in range(2)]
    pin = [sc.sb([128, TT // 128, D_PLE], F32, "pin") for _ in range(2)]
    pT, pTb = sc.sb([128, 2, TT], BF16, "pT")
    sgs = [sc.sb([128, TT], F32, "sg") for _ in range(2)]
    ptp = sc.ps([128, 2, TT], F32, "ptp")
    psA = [sc.ps([128, 2, TT], F32, "psA") for _ in range(3)]

    def load(i):
        xt, xb = xin[i % 2]
        P.add("sp", lambda e: e.dma_start(out=xt[:, :, :], in_=xt_ap(G, i)), R=[G["XTb"][i]], W=[xb], chan="ld")
        pi, pib = pin[i % 2]
        P.add("sp", lambda e: e.dma_start(out=pi[:, :, :], in_=G["p"][l, i * TT:(i + 1) * TT, :].rearrange("(a p) d -> p a d", p=128)),
              W=[pib], chan="ld")

    load(0)
    for i in range(G["NT"]):
        xt, xb = xin[i % 2]
        pi, pib = pin[i % 2]
        if i + 1 < G["NT"]:
            load(i + 1)
        hT, hb = rmsnorm_T(cx, sc, st, xt, xb, g, gb, TT)
        pt, ptb = ptp
        for kc in range(2):
            for a in range(TT // 128):
                P.add("pe", lambda e, kc=kc, a=a, pi=pi: e.transpose(pt[:, kc, a * 128:(a + 1) * 128], pi[:, a, kc * 128:(kc + 1) * 128], ident[:, :]),
                      R=[pib, identb], W=[ptb])
        P.add("act", lambda e: e.copy(out=pT[:, :, :], in_=pt[:, :, :]), R=[ptb], W=[pTb])
        for oc in range(KD):
            pa, pab = psA[oc % 3]
            for c in range(KD):
                P.add("pe", lambda e, c=c, oc=oc, pa=pa: e.matmul(pa[:, 0, :], wg[:, c, oc * 128:(oc + 1) * 128], hT[:, c, :],
                                                               start=(c == 0), stop=(c == KD - 1)), R=[wgb, hb], W=[pab])
            for c in range(2):
                P.add("pe", lambda e, c=c, oc=oc, pa=pa: e.matmul(pa[:, 1, :], wp[:, c, oc * 128:(oc + 1) * 128], pT[:, c, :],
                                                               start=(c == 0), stop=(c == 1)), R=[wpb, pTb], W=[pab])
            sg, sgb = sgs[oc % 2]
            P.add("act", lambda e, pa=pa, sg=sg: e.activation(out=sg[:, :], in_=pa[:, 0, :], func=AF.Sigmoid), R=[pab], W=[sgb])
            if G.get("dbg_dump") and i == 0 and oc == 0:
                P.add("sp", lambda e, sg=sg: e.dma_start(out=G["dbgo"][:, 0:TT], in_=sg[:, :]), R=[sgb], W=[G["outb"]], chan="st")
                d2, d2b = sc.sb([128, TT], F32, "d2")
                P.add("act", lambda e, pa=pa: e.copy(out=d2[:, :], in_=pa[:, 1, :]), R=[pab], W=[d2b])
                P.add("sp", lambda e: e.dma_start(out=G["dbgo"][:, TT:2 * TT], in_=d2[:, :]), R=[d2b], W=[G["outb"]], chan="st")
                d3, d3b = sc.sb([128, 2, TT], F32, "d3")
                P.add("act", lambda e: e.copy(out=d3[:, :, :], in_=pT[:, :, :]), R=[pTb], W=[d3b])
                P.add("sp", lambda e: e.dma_start(out=G["dbgo"][:, 2 * TT:4 * TT], in_=d3[:, :, :].rearrange("p a t -> p (a t)")), R=[d3b], W=[G["outb"]], chan="st")
            P.add("dve", lambda e, pa=pa, sg=sg: e.tensor_tensor(out=sg[:, :], in0=sg[:, :], in1=pa[:, 1, :], op=ALU.mult),
                  R=[pab, sgb], W=[sgb])
            if not G.get("dbg_skip_add"):
                P.add("pool", lambda e, sg=sg, oc=oc, xt=xt: e.tensor_tensor(out=xt[:, oc, :], in0=xt[:, oc, :], in1=sg[:, :], op=ALU.add),
                      R=[sgb, xb], W=[xb])
        P.add("sp", lambda e, i=i, xt=xt: e.dma_start(out=xt_ap(G, i), in_=xt[:, :, :]), R=[xb], W=[G["XTb"][i]], chan="st")
    sc.close()


import math


def OP(cx, eng, method, *args, R=(), W=(), chan=None, **kw):
    return cx.P.add(eng, lambda e: getattr(e, method)(*args, **kw), R=R, W=W, chan=chan)


def flat(t):
    return t[:, :, :].rearrange("p a t -> p (a t)")


def make_tri(cx, sc, dt, val, name):
    t, b = sc.sb([128, 128], dt, name)
    OP(cx, "pool", "memset", t[:, :], val, W=[b])
    OP(cx, "pool", "affine_select", out=t[:, :], in_=t[:, :], pattern=[[1, 128]], compare_op=ALU.is_ge, fill=0.0, base=0,
       channel_multiplier=-1, R=[b], W=[b])
    return t, b


def phase_mlstm(cx, G, l):
    P = cx.P
    TT = G["TT"]
    w = G["w"]
    sc = Scope(cx)
    win, winb = load_w(cx, sc, w["w_in"][l], KD, OFF_MQ, OFF_AQ, "winm")
    pA, pAb = sc.ps([128, 2, TT], F32, "pA")
    pK, pKb = sc.ps([128, 2, TT], F32, "pK")
    pV, pVb = sc.ps([128, 2, TT], F32, "pV")
    pG, pGb = sc.ps([128, 2, TT], F32, "pG")
    pB, pBb = sc.ps([128, 4, 128], F32, "pB")
    pS, pSb = sc.ps([128, 4, 128], F32, "pS")
    pN = [sc.ps([128, 2, TT], F32, "pN") for _ in range(2)]
    tail = Tail(cx, sc, G, l, 1, w["mlstm_w_out"][l], 8, [(pK, pKb), (pV, pVb)], pN)
    identb16, identb16b = make_identity(cx, sc, BF16, "identb")
    NU, NUb = make_tri(cx, sc, BF16, -1.0, "NU")
    U, Ub = make_tri(cx, sc, F32, 1.0, "U")
    ones3, ones3b = sc.sb([128, 4, 128], BF16, "ones3")
    l4h, l4hb = sc.sb([128, 2, 4], BF16, "l4h")
    lbh, lbhb = sc.sb([128, 2, 4, 128], BF16, "lbh")
    OP(cx, "pool", "memset", ones3[:, :, :], 1.0, W=[ones3b])
    nw, nwb = sc.sb([128, D], F32, "nw")
    OP(cx, "sp", "dma_start", out=nw[:, :], in_=w["mlstm_norm"][l].partition_broadcast(128), W=[nwb], chan="wl")
    bi_t, bib = sc.sb([128, 4], F32, "bi")
    bf_t, bfb = sc.sb([128, 4], F32, "bf")
    OP(cx, "sp", "dma_start", out=bi_t[:, :], in_=w["mlstm_b_i"][l].partition_broadcast(128), W=[bib], chan="wl")
    OP(cx, "sp", "dma_start", out=bf_t[:, :], in_=w["mlstm_b_f"][l].partition_broadcast(128), W=[bfb], chan="wl")
    eps_t, epsb = sc.sb([128, 1], F32, "eps")
    OP(cx, "pool", "memset", eps_t[:, :], EPS, W=[epsb])
    hin = [sc.sb([128, KD, TT], BF16, "hin") for _ in range(2)]
    qT, qTb = sc.sb([128, 4, TT], BF16, "qT")
    kT, kTb = sc.sb([128, 4, TT], BF16, "kT")
    vp, vpb = sc.sb([128, 4, 257], BF16, "vp")
    OP(cx, "pool", "memset", vp[:, :, :], 1.0, W=[vpb])
    og, ogb = sc.sb([128, D], F32, "og")
    gi, gib = sc.sb([128, 4], F32, "gi")
    l4, l4b = sc.sb([128, 4], F32, "l4")
    colb, colbb = sc.sb([128, 4], F32, "colb")
    bl, blb = sc.sb([128, 4], F32, "bl")
    wk, wkb = sc.sb([128, 4], F32, "wk")
    r4, r4b = sc.sb([128, 4], F32, "r4")
    msq, msqb = sc.sb([128, 4], F32, "msq")
    lb, lbb = sc.sb([128, 4, 128], F32, "lb")
    eb, ebb = sc.sb([128, 4, 128], F32, "eb")
    DT, DTb = sc.sb([128, 4, 128], F32, "DT")
    AT, ATb = sc.sb([128, 4, 128], BF16, "AT")
    qs, qsb = sc.sb([128, 4, 128], BF16, "qs")
    kts, ktsb = sc.sb([128, 4, 128], BF16, "kts")
    C32, C32b = sc.sb([128, 4, 257], F32, "C32")
    Cbf, Cbfb = sc.sb([128, 4, 257], BF16, "Cbf")
    tmp, tmpb = sc.sb([128, D], F32, "tmp")
    tmp2, tmp2b = sc.sb([128, D], F32, "tmp2")
    junk, junkb = sc.sb([128, 256], F32, "junk")
    hn, hnb = sc.sb([128, D], BF16, "hn")
    ymT, ymTb = sc.sb([128, KD, TT], BF16, "ymT")
    pAf, pKf, pVf, pGf = flat(pA), flat(pK), flat(pV), flat(pG)
    pAT = pAf.bitcast(BF16).rearrange("p (c t) -> p c t", t=128)
    tiles_per_seq = G["S"] // TT
    kscale = 128 ** -0.5

    def load(i):
        hT, hb = hin[i % 2]
        OP(cx, "sp", "dma_start", out=hT[:, :, :], in_=ht_ap(G, i), R=[G["HTb"][i]], W=[hb], chan="ld")
        tail.load_x(i)

    load(0)
    for i in range(G["NT"]):
        hT, hb = hin[i % 2]
        if i + 1 < G["NT"]:
            load(i + 1)
        if i % tiles_per_seq == 0:
            OP(cx, "pool", "memset", C32[:, :, :], 0.0, W=[C32b])
            OP(cx, "pool", "memset", Cbf[:, :, :], 0.0, W=[Cbfb])
        for which, dst, dstb, off in ((0, qT, qTb, 0), (1, kT, kTb, 512)):
            for hp in range(2):
                for hh in range(2):
                    h = hp * 2 + hh
                    for c in range(KD):
                        OP(cx, "pe", "matmul", pA[:, hh, :], win[:, c, off + h * 128:off + (h + 1) * 128], hT[:, c, :],
                           start=(c == 0), stop=(c == KD - 1), R=[winb, hb], W=[pAb])
                if which == 0:
                    OP(cx, "act", "copy", out=dst[:, 2 * hp:2 * hp + 2, :], in_=pA[:, :, :], R=[pAb], W=[dstb])
                else:
                    OP(cx, "act", "mul", dst[:, 2 * hp:2 * hp + 2, :], pA[:, :, :], kscale, R=[pAb], W=[dstb])
        for a in range(TT // 128):
            ts = slice(a * 128, (a + 1) * 128)
            if G.get("dbg_stop", 99) <= 0:
                continue
            for c in range(KD):
                OP(cx, "pe", "matmul", pGf[:, 0:8], hT[:, c, ts], win[:, c, 3072:3080], start=(c == 0), stop=(c == KD - 1),
                   R=[winb, hb], W=[pGb])
            if G.get("dbg_stop", 99) == 0.5:
                continue
            OP(cx, "dve", "tensor_tensor", out=gi[:, :], in0=pGf[:, 0:4], in1=bi_t[:, :], op=ALU.add, R=[pGb, bib], W=[gib])
            OP(cx, "dve", "tensor_tensor", out=l4[:, :], in0=pGf[:, 4:8], in1=bf_t[:, :], op=ALU.add, R=[pGb, bfb], W=[l4b])
            OP(cx, "act", "activation", out=l4[:, :], in_=l4[:, :], func=AF.Exp, scale=-1.0, R=[l4b], W=[l4b])
            OP(cx, "act", "activation", out=l4[:, :], in_=l4[:, :], func=AF.Ln, bias=1.0, R=[l4b], W=[l4b])
            if G.get("dbg_stop", 99) == 0.7:
                continue
            OP(cx, "dve", "tensor_copy", out=l4h[:, 0, :], in_=l4[:, :], R=[l4b], W=[l4hb])
            OP(cx, "dve", "tensor_tensor", out=l4h[:, 1, :], in0=l4[:, :], in1=l4h[:, 0, :], op=ALU.subtract, R=[l4b, l4hb], W=[l4hb])
            for z in range(2):
                OP(cx, "dve", "tensor_tensor", out=lbh[:, z, :, :], in0=ones3[:, :, :],
                   in1=l4h[:, z, :].unsqueeze(2).to_broadcast([128, 4, 128]), op=ALU.mult, R=[ones3b, l4hb], W=[lbhb])
            if G.get("dbg_stop", 99) == 0.8:
                continue
            for z in range(2):
                OP(cx, "pe", "matmul", pGf[:, 8:12], NU[:, :], l4h[:, z, :], start=(z == 0), stop=(z == 1), R=[NUb, l4hb], W=[pGb])
            for h in range(4):
                for z in range(2):
                    OP(cx, "pe", "matmul", pB[:, h, :], lbh[:, z, h, :], NU[:, :], start=(z == 0), stop=(z == 1), R=[lbhb, NUb], W=[pBb])
            if G.get("dbg_stop", 99) == 0.9:
                continue
            OP(cx, "act", "activation", out=eb[:, :, :], in_=pB[:, :, :], func=AF.Exp, R=[pBb], W=[ebb])
            OP(cx, "dve", "tensor_tensor", out=colb[:, :], in0=gi[:, :], in1=pGf[:, 8:12], op=ALU.subtract, R=[gib, pGb], W=[colbb])
            OP(cx, "dve", "tensor_copy", out=bl[:, :], in_=pB[:, :, 127], R=[pBb], W=[blb])
            OP(cx, "dve", "tensor_tensor", out=wk[:, :], in0=colb[:, :], in1=bl[:, :], op=ALU.add, R=[colbb, blb], W=[wkb])
            OP(cx, "act", "activation", out=wk[:, :], in_=wk[:, :], func=AF.Exp, R=[wkb], W=[wkb])
            OP(cx, "dve", "tensor_scalar", out=wk[:, :], in0=wk[:, :], scalar1=kscale, scalar2=None, op0=ALU.mult, R=[wkb], W=[wkb])
            if G.get("dbg_stop", 99) <= 1:
                continue
            for h in range(4):
                OP(cx, "pe", "matmul", pS[:, h, :], kT[:, h, ts], qT[:, h, ts], start=True, stop=True, R=[kTb, qTb], W=[pSb])
            for h in range(4):
                OP(cx, "act", "activation", out=DT[:, h, :], in_=pB[:, h, :], func=AF.Exp, bias=colb[:, h:h + 1], R=[pBb, colbb], W=[DTb])
            OP(cx, "pool", "tensor_tensor", out=DT[:, :, :], in0=DT[:, :, :], in1=U[:, :].unsqueeze(1).to_broadcast([128, 4, 128]),
               op=ALU.mult, R=[DTb, Ub], W=[DTb])
            OP(cx, "dve", "tensor_tensor", out=AT[:, :, :], in0=DT[:, :, :], in1=pS[:, :, :], op=ALU.mult, R=[DTb, pSb], W=[ATb])
            OP(cx, "pool", "tensor_tensor", out=qs[:, :, :], in0=qT[:, :, ts], in1=eb[:, :, :], op=ALU.mult, R=[qTb, ebb], W=[qsb])
            if G.get("dbg_stop", 99) <= 2:
                continue
            for c in range(KD):
                OP(cx, "pe", "matmul", pKf[:, :], hT[:, c, ts], win[:, c, 512:1024], start=(c == 0), stop=(c == KD - 1),
                   R=[winb, hb], W=[pKb])
            OP(cx, "dve", "tensor_tensor", out=kts[:, :, :], in0=pKf.rearrange("p (h d) -> p h d", d=128),
               in1=wk[:, :].unsqueeze(2).to_broadcast([128, 4, 128]), op=ALU.mult, R=[pKb, wkb], W=[ktsb])
            for r in range(2):
                for c in range(KD):
                    OP(cx, "pe", "matmul", pVf[:, :], hT[:, c, ts], win[:, c, 1024 + r * 512:1024 + (r + 1) * 512],
                       start=(c == 0), stop=(c == KD - 1), R=[winb, hb], W=[pVb])
                OP(cx, "act", "copy", out=vp[:, 2 * r:2 * r + 2, 0:256], in_=pVf.rearrange("p (h d) -> p h d", d=256), R=[pVb], W=[vpb])
            for r in range(2):
                for c in range(KD):
                    OP(cx, "pe", "matmul", pVf[:, :], hT[:, c, ts], win[:, c, 2048 + r * 512:2048 + (r + 1) * 512],
                       start=(c == 0), stop=(c == KD - 1), R=[winb, hb], W=[pVb])
                OP(cx, "act", "activation", out=og[:, r * 512:(r + 1) * 512], in_=pVf[:, :], func=AF.Sigmoid, R=[pVb], W=[ogb])
            if G.get("dbg_stop", 99) <= 3:
                continue
            for h in range(4):
                pn, pnb = pN[h % 2]
                pnf = flat(pn)
                OP(cx, "pe", "matmul", pnf[:, 0:257], qs[:, h, :], Cbf[:, h, :], start=True, stop=False, R=[qsb, Cbfb], W=[pnb])
                OP(cx, "pe", "matmul", pnf[:, 0:257], AT[:, h, :], vp[:, h, :], start=False, stop=True, R=[ATb, vpb], W=[pnb])
                OP(cx, "act", "activation", out=r4[:, h:h + 1], in_=pnf[:, 256:257], func=AF.Abs, R=[pnb], W=[r4b])
                OP(cx, "dve", "tensor_scalar", out=r4[:, h:h + 1], in0=r4[:, h:h + 1], scalar1=1.0, scalar2=None, op0=ALU.max,
                   R=[r4b], W=[r4b])
                OP(cx, "dve", "reciprocal", out=r4[:, h:h + 1], in_=r4[:, h:h + 1], R=[r4b], W=[r4b])
                OP(cx, "dve", "tensor_scalar", out=tmp[:, h * 256:(h + 1) * 256], in0=pnf[:, 0:256], scalar1=r4[:, h:h + 1], scalar2=None,
                   op0=ALU.mult, R=[pnb, r4b], W=[tmpb])
                OP(cx, "act", "activation", out=junk[:, :], in_=tmp[:, h * 256:(h + 1) * 256], func=AF.Square, accum_out=msq[:, h:h + 1],
                   R=[tmpb], W=[junkb, msqb])
            OP(cx, "act", "activation", out=msq[:, :], in_=msq[:, :], func=AF.Sqrt, scale=1.0 / 256, bias=eps_t[:, 0:1], R=[msqb, epsb], W=[msqb])
            OP(cx, "dve", "reciprocal", out=msq[:, :], in_=msq[:, :], R=[msqb], W=[msqb])
            for h in range(4):
                OP(cx, "dve", "scalar_tensor_tensor", out=tmp2[:, h * 256:(h + 1) * 256], in0=tmp[:, h * 256:(h + 1) * 256],
                   scalar=msq[:, h:h + 1], in1=nw[:, h * 256:(h + 1) * 256], op0=ALU.mult, op1=ALU.mult, R=[tmpb, msqb, nwb], W=[tmp2b])
            OP(cx, "pool", "tensor_tensor", out=hn[:, :], in0=tmp2[:, :], in1=og[:, :], op=ALU.mult, R=[tmp2b, ogb], W=[hnb])
            if G.get("dbg_stop", 99) <= 4:
                continue
            for h in range(4):
                OP(cx, "pe", "matmul", pAf[:, 0:257], kts[:, h, :], vp[:, h, :], start=True, stop=True, R=[ktsb, vpb], W=[pAb])
                OP(cx, "dve", "scalar_tensor_tensor", out=C32[:, h, :], in0=C32[:, h, :], scalar=eb[:, h, 127:128], in1=pAf[:, 0:257],
                   op0=ALU.mult, op1=ALU.add, R=[C32b, ebb, pAb], W=[C32b])
            OP(cx, "act", "copy", out=Cbf[:, :, :], in_=C32[:, :, :], R=[C32b], W=[Cbfb])
            if G.get("dbg_stop", 99) <= 5:
                continue
            for c in range(KD):
                OP(cx, "pe", "transpose", pAT[:, c, :], hn[:, c * 128:(c + 1) * 128], identb16[:, :], R=[hnb, identb16b], W=[pAb])
            OP(cx, "act", "copy", out=ymT[:, :, ts], in_=pAT[:, :, :], R=[pAb], W=[ymTb])
        tail.run(i, hT, hb, ymT, ymTb)
    sc.close()


def phase_dsa(cx, G, l):
    P = cx.P
    TT = G["TT"]
    S = G["S"]
    w = G["w"]
    n_sel = G["n_sel"]
    KSEL = n_sel // 128
    NIT = 16
    BIG = 1000.0
    sc = Scope(cx)
    wq, wqb = load_w(cx, sc, w["w_in"][l], KD, OFF_AQ, OFF_AQ + 512, "wq")
    wiq, wiqb = load_w(cx, sc, w["w_in"][l], KD, OFF_IQ, OFF_IQ + 256, "wiq")
    wkk = sc.sb([128, KD, 128], BF16, "wkk")
    wik = sc.sb([128, KD, 128], BF16, "wik")
    for dc in (0, 64):
        load_w(cx, sc, w["w_in"][l], KD, OFF_AK, OFF_AK + 64, "", dst=wkk, dcol=dc)
        load_w(cx, sc, w["w_in"][l], KD, OFF_IK, OFF_IK + 64, "", dst=wik, dcol=dc)
    wv, wvb = load_w(cx, sc, w["w_in"][l], KD, OFF_AV, OFF_AV + 64, "wv")
    wiw, wiwb = load_w(cx, sc, w["w_in"][l], KD, OFF_IW, OFF_IW + 4, "wiw")
    pA, pAb = sc.ps([128, 2, TT], F32, "pA")
    pL = [sc.ps([128, 2, TT], F32, "pL") for _ in range(2)]
    pMT, pMTb = sc.ps([128, 8, 128], BF16, "pMT")
    pST = [sc.ps([128, 2, TT], F32, "pST") for _ in range(2)]
    pO = [sc.ps([128, 4, 128], F32, "pO") for _ in range(2)]
    tail = Tail(cx, sc, G, l, 2, w["attn_w_out"][l], 4, pST, pL)
    identb, identbb = make_identity(cx, sc, BF16, "identb")
    ident4, ident4b = sc.sb([128, 4, 128], BF16, "ident4")
    bigi4, bigi4b = sc.sb([128, 4, 128], BF16, "bigi4")
    for h in range(4):
        OP(cx, "pool", "tensor_copy", out=ident4[:, h, :], in_=identb[:, :], R=[identbb], W=[ident4b])
    OP(cx, "pool", "tensor_scalar", out=bigi4[:, :, :], in0=ident4[:, :, :], scalar1=BIG, scalar2=None, op0=ALU.mult, R=[ident4b], W=[bigi4b])
    negb, negbb = sc.sb([128, 1], F32, "negb")
    OP(cx, "pool", "memset", negb[:, :], -BIG, W=[negbb])
    NEGU, NEGUb = sc.sb([128, 128], BF16, "NEGU")
    OP(cx, "pool", "memset", NEGU[:, :], -1000.0, W=[NEGUb])
    OP(cx, "pool", "affine_select", out=NEGU[:, :], in_=NEGU[:, :], pattern=[[1, 128]], compare_op=ALU.is_gt, fill=0.0, base=0,
       channel_multiplier=-1, R=[NEGUb], W=[NEGUb])
    LT, LTb = sc.sb([128, 128], BF16, "LT")
    OP(cx, "pool", "memset", LT[:, :], 1.0, W=[LTb])
    OP(cx, "pool", "affine_select", out=LT[:, :], in_=LT[:, :], pattern=[[-1, 128]], compare_op=ALU.is_ge, fill=0.0, base=0,
       channel_multiplier=1, R=[LTb], W=[LTb])
    hin = [sc.sb([128, KD, TT], BF16, "hin") for _ in range(3)]
    qTs = [sc.sb([128, 4, TT], BF16, "qT") for _ in range(2)]
    qiTs = [sc.sb([128, 2, TT], BF16, "qiT") for _ in range(2)]
    kT2s = [sc.sb([128, S], BF16, "kT2") for _ in range(2)]
    kiT2s = [sc.sb([128, S], BF16, "kiT2") for _ in range(2)]
    vpds = [sc.sb([128, S // 128, 65], BF16, "vpd") for _ in range(2)]
    for vpd, vpdb in vpds:
        OP(cx, "pool", "memset", vpd[:, :, :], 1.0, W=[vpdb])
    wf, wfb = sc.sb([128, 4], F32, "wf")
    dg, dgb = sc.sb([128, 4, 128], BF16, "dg")
    Rl = [sc.sb([128, 512], BF16, "Rl") for _ in range(4)]
    scs, scsb = sc.sb([128, S], F32, "scs")
    jnk, jnkb = sc.sb([128, S], BF16, "jnk")
    Mbs = [sc.sb([128, S], BF16, "Mb") for _ in range(2)]
    lo, lob = sc.sb([128, 1], F32, "lo")
    w0, w0b = sc.sb([128, 1], F32, "w0")
    mid, midb = sc.sb([128, 1], F32, "mid")
    cnt, cntb = sc.sb([128, 1], F32, "cnt")
    stp, stpb = sc.sb([128, 1], F32, "stp")
    PT = [sc.sb([128, 512], BF16, "PT") for _ in range(4)]
    rden, rdenb = sc.sb([128, 2, 4], F32, "rden")
    ha, hab = sc.sb([128, 4, 2, 64], BF16, "ha")
    yaTs = [sc.sb([128, 4, TT], BF16, "yaT") for _ in range(2)]
    pAf = flat(pA)
    tiles_per_seq = S // TT
    NT = G["NT"]

    def load_h(i):
        hT, hb = hin[i % 3]
        OP(cx, "sp", "dma_start", out=hT[:, :, :], in_=ht_ap(G, i), R=[G["HTb"][i]], W=[hb], chan="ld")

    def proj(i):
        hT, hb = hin[i % 3]
        qT, qTb = qTs[i % 2]
        qiT, qiTb = qiTs[i % 2]
        sq_ = (i // tiles_per_seq) % 2
        kT2, kT2b = kT2s[sq_]
        kiT2, kiT2b = kiT2s[sq_]
        ti = i % tiles_per_seq
        cs = slice(ti * TT, (ti + 1) * TT)
        for hp in range(2):
            for hh in range(2):
                c4 = hp * 2 + hh
                for c in range(KD):
                    OP(cx, "pe", "matmul", pA[:, hh, :], wq[:, c, c4 * 128:(c4 + 1) * 128], hT[:, c, :], start=(c == 0), stop=(c == KD - 1),
                       R=[wqb, hb], W=[pAb])
            OP(cx, "act", "mul", qT[:, 2 * hp:2 * hp + 2, :], pA[:, :, :], 0.125, R=[pAb], W=[qTb])
        for hh, (wt, wtb) in enumerate((wkk, wik)):
            for c in range(KD):
                OP(cx, "pe", "matmul", pA[:, hh, :], wt[:, c, :], hT[:, c, :], start=(c == 0), stop=(c == KD - 1), R=[wtb, hb], W=[pAb])
        OP(cx, "act", "copy", out=kT2[:, cs], in_=pA[:, 0, :], R=[pAb], W=[kT2b])
        OP(cx, "act", "copy", out=kiT2[:, cs], in_=pA[:, 1, :], R=[pAb], W=[kiT2b])
        for hh in range(2):
            for c in range(KD):
                OP(cx, "pe", "matmul", pA[:, hh, :], wiq[:, c, hh * 128:(hh + 1) * 128], hT[:, c, :], start=(c == 0), stop=(c == KD - 1),
                   R=[wiqb, hb], W=[pAb])
        OP(cx, "act", "copy", out=qiT[:, :, :], in_=pA[:, :, :], R=[pAb], W=[qiTb])

    def prep(i, a):
        hT, hb = hin[i % 3]
        qiT, qiTb = qiTs[i % 2]
        sq_ = (i // tiles_per_seq) % 2
        kiT2, kiT2b = kiT2s[sq_]
        vpd, vpdb = vpds[sq_]
        ti = i % tiles_per_seq
        ts = slice(a * 128, (a + 1) * 128)
        qi = ti * 2 + a
        SV = (qi + 1) * 128
        Mb, Mbb = Mbs[qi % 2]
        for c in range(KD):
            OP(cx, "pe", "matmul", pAf[:, 0:64], hT[:, c, ts], wv[:, c, :], start=(c == 0), stop=(c == KD - 1), R=[wvb, hb], W=[pAb])
        OP(cx, "act", "copy", out=vpd[:, qi, 0:64], in_=pAf[:, 0:64], R=[pAb], W=[vpdb])
        if qi < KSEL:
            if qi > 0:
                OP(cx, "pool", "memset", Mb[:, 0:qi * 128], 1.0, W=[Mbb])
            OP(cx, "pool", "tensor_copy", out=Mb[:, qi * 128:SV], in_=LT[:, :], R=[LTb], W=[Mbb])
            return
        for c in range(KD):
            OP(cx, "pe", "matmul", pAf[:, 64:68], hT[:, c, ts], wiw[:, c, :], start=(c == 0), stop=(c == KD - 1), R=[wiwb, hb], W=[pAb])
        OP(cx, "act", "mul", wf[:, :], pAf[:, 64:68], 1.0 / 16.0, R=[pAb], W=[wfb])
        OP(cx, "dve", "tensor_tensor", out=dg[:, :, :], in0=ident4[:, :, :], in1=wf[:, :].unsqueeze(2).to_broadcast([128, 4, 128]),
           op=ALU.mult, R=[ident4b, wfb], W=[dgb])
        for c0 in range(0, SV, 512):
            cw = min(512, SV - c0)
            for h in range(4):
                half, ch = h % 2, h // 2
                ps_ = slice(half * 64, (half + 1) * 64)
                pl, plb = pL[h % 2]
                OP(cx, "pe", "matmul", flat(pl)[:, 0:cw], qiT[ps_, ch, ts], kiT2[ps_, c0:c0 + cw], start=True, stop=True,
                   R=[qiTb, kiT2b], W=[plb])
                OP(cx, "act", "activation", out=Rl[h][0][:, 0:cw], in_=flat(pl)[:, 0:cw], func=AF.Relu, R=[plb], W=[Rl[h][1]])
            dcol = qi * 128 - c0
            has_diag = 0 <= dcol < cw
            for h in range(4):
                OP(cx, "pe", "matmul", pAf[:, 0:cw], dg[:, h, :], Rl[h][0][:, 0:cw], start=(h == 0), stop=(h == 3 and not has_diag),
                   R=[dgb, Rl[h][1]], W=[pAb])
            if has_diag:
                OP(cx, "pe", "matmul", pAf[:, dcol:dcol + 128], identb[:, :], NEGU[:, :], start=False, stop=True,
                   R=[identbb, NEGUb], W=[pAb])
            OP(cx, "act", "copy", out=scs[:, c0:c0 + cw], in_=pAf[:, 0:cw], R=[pAb], W=[scsb])
        OP(cx, "dve", "tensor_reduce", out=lo[:, :], in_=scs[:, 0:qi * 128], axis=AX.X, op=ALU.min, R=[scsb], W=[lob])
        OP(cx, "dve", "tensor_reduce", out=w0[:, :], in_=scs[:, 0:SV], axis=AX.X, op=ALU.max, R=[scsb], W=[w0b])
        OP(cx, "dve", "tensor_tensor", out=w0[:, :], in0=w0[:, :], in1=lo[:, :], op=ALU.subtract, R=[w0b, lob], W=[w0b])
        for it in range(1, NIT + 1):
            f = 2.0 ** -it
            OP(cx, "dve", "scalar_tensor_tensor", out=mid[:, :], in0=w0[:, :], scalar=f, in1=lo[:, :], op0=ALU.mult, op1=ALU.add,
               R=[w0b, lob], W=[midb])
            OP(cx, "dve", "tensor_scalar", out=jnk[:, 0:SV], in0=scs[:, 0:SV], scalar1=mid[:, 0:1], scalar2=0.0, op0=ALU.is_ge,
               op1=ALU.add, accum_out=cnt[:, 0:1], R=[scsb, midb], W=[jnkb, cntb])
            OP(cx, "dve", "tensor_scalar", out=stp[:, :], in0=cnt[:, :], scalar1=float(n_sel) - 0.5, scalar2=f, op0=ALU.is_ge,
               op1=ALU.mult, R=[cntb], W=[stpb])
            OP(cx, "dve", "scalar_tensor_tensor", out=lo[:, :], in0=stp[:, :], scalar=w0[:, 0:1], in1=lo[:, :], op0=ALU.mult,
               op1=ALU.add, R=[stpb, w0b, lob], W=[lob])
        OP(cx, "dve", "tensor_scalar", out=Mb[:, 0:SV], in0=scs[:, 0:SV], scalar1=lo[:, 0:1], scalar2=None, op0=ALU.is_ge,
           R=[scsb, lob], W=[Mbb])

    def attn(i, a):
        qT, qTb = qTs[i % 2]
        sq_ = (i // tiles_per_seq) % 2
        kT2, kT2b = kT2s[sq_]
        vpd, vpdb = vpds[sq_]
        yaT, yaTb = yaTs[i % 2]
        ti = i % tiles_per_seq
        ts = slice(a * 128, (a + 1) * 128)
        qi = ti * 2 + a
        nk = qi + 1
        Mb, Mbb = Mbs[qi % 2]
        def S_(kt):
            for half in range(2):
                ps_ = slice(half * 64, (half + 1) * 64)
                pst, pstb = pST[half]
                OP(cx, "pe", "matmul", flat(pst)[:, :], kT2[ps_, kt * 128:(kt + 1) * 128], qT[ps_, :, ts], start=True, stop=False,
                   R=[kT2b, qTb], W=[pstb])
                OP(cx, "pe", "matmul", flat(pst)[:, :], Mb[:, kt * 128:(kt + 1) * 128], bigi4[:, :, :], start=False, stop=True,
                   R=[Mbb, bigi4b], W=[pstb])

        def E_(kt):
            for half in range(2):
                pst, pstb = pST[half]
                pt_, ptb_ = PT[(kt % 2) * 2 + half]
                OP(cx, "act", "activation", out=pt_[:, :], in_=flat(pst)[:, :], func=AF.Exp, bias=negb[:, 0:1], R=[pstb, negbb], W=[ptb_])

        def V_(kt):
            for half in range(2):
                pt_, ptb_ = PT[(kt % 2) * 2 + half]
                po, pob = pO[half]
                for c in range(4):
                    OP(cx, "pe", "matmul", po[:, c, 0:65], pt_[:, c * 128:(c + 1) * 128], vpd[:, kt, :], start=(kt == 0 and c == 0),
                       stop=(kt == qi), skip_group_check=True, R=[ptb_, vpdb], W=[pob])

        S_(0)
        for kt in range(nk):
            E_(kt)
            if kt + 1 < nk:
                S_(kt + 1)
            V_(kt)
        for half in range(2):
            po, pob = pO[half]
            OP(cx, "dve", "reciprocal", out=rden[:, half, :], in_=po[:, :, 64], R=[pob], W=[rdenb])
            OP(cx, "dve", "tensor_tensor", out=ha[:, :, half, :], in0=po[:, :, 0:64],
               in1=rden[:, half, :].unsqueeze(2).to_broadcast([128, 4, 64]), op=ALU.mult, R=[pob, rdenb], W=[hab])
        haf = ha[:, :, :, :].rearrange("p c h d -> p (c h d)")
        for c in range(4):
            OP(cx, "pe", "transpose", pMT[:, 4 + c, :], haf[:, c * 128:(c + 1) * 128], identb[:, :], R=[hab, identbb], W=[pMTb])
        OP(cx, "act", "copy", out=yaT[:, :, ts], in_=pMT[:, 4:8, :], R=[pMTb], W=[yaTb])

    steps = [(i, a) for i in range(NT) for a in range(TT // 128)]
    load_h(0)
    if NT > 1:
        load_h(1)
    tail.load_x(0)
    proj(0)
    prep(0, 0)
    for k, (i, a) in enumerate(steps):
        if k + 1 < len(steps):
            i2, a2 = steps[k + 1]
            if a2 == 0:
                if i2 + 1 < NT:
                    load_h(i2 + 1)
                tail.load_x(i2)
                proj(i2)
            prep(i2, a2)
        attn(i, a)
        if a == TT // 128 - 1:
            hT, hb = hin[i % 3]
            tail.run(i, hT, hb, yaTs[i % 2][0], yaTs[i % 2][1])
    sc.close()


WNAMES = ["norm_ffn1", "ffn1_w_gu", "ffn1_w_down", "norm_mix", "w_in", "conv_w", "conv_w_out", "mlstm_b_i", "mlstm_b_f",
          "mlstm_norm", "mlstm_w_out", "attn_w_out", "w_o", "norm_ffn2", "ffn2_w_gu", "ffn2_w_down", "norm_ple",
          "ple_w_gate", "ple_w_proj", "final_norm"]


def build(S, nseq, depth, wshapes, phases=None, n_sel=256, dbg=None):
    nc = bass.Bass("TRN2", target_bir_lowering=False)
    NTC = S * nseq
    TT = 256
    G = {"S": S, "nseq": nseq, "NTC": NTC, "TT": TT, "NT": NTC // TT, "depth": depth, "n_sel": n_sel}
    G.update(dbg or {})
    G["x"] = nc.dram_tensor("x", [NTC, D], F32, kind="ExternalInput").ap()
    G["p"] = nc.dram_tensor("p", [depth, NTC, D_PLE], F32, kind="ExternalInput").ap()
    G["w"] = {k: nc.dram_tensor(k, list(wshapes[k]), F32, kind="ExternalInput").ap() for k in WNAMES}
    G["out"] = nc.dram_tensor("out", [NTC, D], F32, kind="ExternalOutput").ap()
    G["outb"] = Buf("out")
    if G.get("dbg_dump"):
        G["dbgo"] = nc.dram_tensor("dbgo", [128, 4096], F32, kind="ExternalOutput").ap()
    G["XT"] = nc.dram_tensor("XT", [D, NTC], F32).ap()
    G["XTb"] = [Buf("XT%d" % i) for i in range(G["NT"])]
    G["HT"] = nc.dram_tensor("HT", [D, NTC], BF16).ap()
    G["HTb"] = [Buf("HT%d" % i) for i in range(G["NT"])]
    with contextlib.ExitStack() as es:
        cx = Ctx(nc, es)
        phase_in(cx, G)
        for l in range(depth):
            if phases is None or "ffn1" in phases:
                phase_ffn(cx, G, l, 1)
            if phases is None or "conv" in phases or "mlstm" in phases or "dsa" in phases:
                phase_norm(cx, G, l)
            if phases is None or "conv" in phases:
                phase_conv(cx, G, l)
            if phases is None or "mlstm" in phases:
                phase_mlstm(cx, G, l)
            if phases is None or "dsa" in phases:
                phase_dsa(cx, G, l)
            if phases is None or "ffn2" in phases:
                phase_ffn(cx, G, l, 2)
            if phases is None or "ple" in phases:
                phase_ple(cx, G, l)
        phase_out(cx, G)
        cx.P.add("sp", lambda e: e.nop(), R=[G["outb"]], chan=None) if False else None
        cx.P.barrier()
        cx.P.emit()
    return nc


def kernel(**inputs):
    x = np.ascontiguousarray(inputs["x"], dtype=np.float32)
    p = np.ascontiguousarray(inputs["p"], dtype=np.float32)
    B, S, _ = x.shape
    depth = p.shape[0]
    nseq = B // N_CORES
    wshapes = {k: inputs[k].shape for k in WNAMES}
    nc = build(S, nseq, depth, wshapes)
    in_maps = []
    for c in range(N_CORES):
        m = {k: np.ascontiguousarray(inputs[k], dtype=np.float32) for k in WNAMES}
        m["x"] = x[c * nseq:(c + 1) * nseq].reshape(nseq * S, D)
        m["p"] = np.ascontiguousarray(p[:, c * nseq:(c + 1) * nseq].reshape(depth, nseq * S, D_PLE))
        in_maps.append(m)
    res = run_bass_kernel_spmd(nc, in_maps, core_ids=list(range(N_CORES)))
    out = np.concatenate([r["out"].reshape(nseq, S, D) for r in res.results], axis=0)
    return out.astype(np.float32)
```

```python
import contextlib
import numpy as np
import concourse.bass as bass
import concourse.mybir as mybir
from concourse.bass_utils import run_bass_kernel_spmd

F32 = mybir.dt.float32
BF16 = mybir.dt.bfloat16
AF = mybir.ActivationFunctionType
ALU = mybir.AluOpType
AX = mybir.AxisListType

D = 1024
KD = 8
FF = 2816
KF = 22
D_PLE = 256
N_IN = 8652
EPS = 1e-6
N_CORES = 8


class Buf:
    __slots__ = ("w", "r", "name", "excl")

    def __init__(self, name="", excl=False):
        self.w = None
        self.r = {}
        self.name = name
        self.excl = excl


class Stream:
    def __init__(self, name, sem, inc):
        self.name, self.sem, self.inc = name, sem, inc
        self.ops = []


class Op:
    __slots__ = ("fn", "waits", "sig", "stream", "sigcount")

    def __init__(self, fn, stream):
        self.fn, self.stream = fn, stream
        self.waits = {}
        self.sig = False
        self.sigcount = 0


class Prog:
    ENGS = ("pe", "act", "dve", "pool", "sp")

    def __init__(self, nc, es):
        self.nc = nc
        self.es = es
        self.q = {e: [] for e in self.ENGS}
        self.seen = {e: {} for e in self.ENGS}
        self.streams = {}
        self.chan_n = {}
        for e in ("pe", "act", "dve", "pool"):
            self.new_stream(e, 1)

    def new_stream(self, name, inc=16):
        sem = self.es.enter_context(self.nc.semaphore("s_" + name))
        self.streams[name] = Stream(name, sem, inc)
        return self.streams[name]

    NSLOT = 8

    def new_channel(self, name):
        self.chan_n[name] = 0
        for k in range(self.NSLOT):
            self.new_stream("%s%d" % (name, k), 16)

    def add(self, eng, fn, R=(), W=(), chan=None):
        if chan is not None:
            k = self.chan_n[chan]
            self.chan_n[chan] = k + 1
            st = self.streams["%s%d" % (chan, k % self.NSLOT)]
        else:
            st = self.streams[eng]
        op = Op(fn, st)
        if st.inc == 16:
            op.sig = True
        st.ops.append(op)
        idx = len(st.ops)
        seen = self.seen[eng]
        waits = op.waits

        def need(dep):
            if dep is None:
                return
            s, j = dep
            if s is st and eng == "pe":
                return
            if seen.get(s, 0) >= j:
                return
            if waits.get(s, 0) < j:
                waits[s] = j

        if st.inc == 16 and idx > 1:
            need((st, idx - 1))
        for b in R:
            need(b.w)
            if b.excl:
                for s, j in b.r.items():
                    if s is not st:
                        need((s, j))
        for b in W:
            need(b.w)
            for s, j in b.r.items():
                if s is st and st.inc == 1:
                    continue
                need((s, j))
        for s, j in waits.items():
            seen[s] = j
            s.ops[j - 1].sig = True
        for b in R:
            if b.r.get(st, 0) < idx:
                b.r[st] = idx
        for b in W:
            b.w = (st, idx)
            b.r = {}
        self.q[eng].append(op)
        return op

    def barrier(self):
        tails = [(s, len(s.ops)) for s in self.streams.values() if s.ops]
        for e in self.ENGS:
            op = Op(None, None)
            seen = self.seen[e]
            for s, j in tails:
                if seen.get(s, 0) >= j:
                    continue
                op.waits[s] = j
                seen[s] = j
                s.ops[j - 1].sig = True
            self.q[e].append(op)

    def emit(self):
        for st in self.streams.values():
            c = 0
            for op in st.ops:
                if op.sig:
                    c += 1
                op.sigcount = c
        nc = self.nc
        q = self.q

        def run(e, ops):
            for op in ops:
                for s, j in op.waits.items():
                    e.wait_ge(s.sem, s.ops[j - 1].sigcount * s.inc)
                if op.fn is None:
                    continue
                ins = op.fn(e)
                if op.sig:
                    ins.then_inc(op.stream.sem, op.stream.inc)

        with nc.Block() as block:
            @block.tensor
            def _(e):
                run(e, q["pe"])

            @block.scalar
            def _(e):
                run(e, q["act"])

            @block.vector
            def _(e):
                run(e, q["dve"])

            @block.gpsimd
            def _(e):
                run(e, q["pool"])

            @block.sync
            def _(e):
                run(e, q["sp"])


class Ctx:
    def __init__(self, nc, es):
        self.nc, self.es = nc, es
        self.P = Prog(nc, es)
        self.P.new_channel("ld")
        self.P.new_channel("st")
        self.P.new_channel("wl")
        self.P.new_channel("pl")
        self._n = 0

    def sb(self, shape, dt, name=None):
        self._n += 1
        return self.es.enter_context(self.nc.sbuf_tensor(f"{name or 't'}_{self._n}", list(shape), dt))

    def ps(self, shape, dt=F32, name=None):
        self._n += 1
        return self.es.enter_context(self.nc.psum_tensor(f"{name or 'p'}_{self._n}", list(shape), dt))


class Scope:
    def __init__(self, cx):
        self.cx = cx
        self.es = contextlib.ExitStack()
        self.bufs = []

    def sb(self, shape, dt, name="t"):
        cx = self.cx
        cx._n += 1
        t = self.es.enter_context(cx.nc.sbuf_tensor(f"{name}_{cx._n}", list(shape), dt))
        b = Buf(name)
        self.bufs.append(b)
        return t, b

    def ps(self, shape, dt=F32, name="p"):
        cx = self.cx
        cx._n += 1
        t = self.es.enter_context(cx.nc.psum_tensor(f"{name}_{cx._n}", list(shape), dt))
        b = Buf(name, excl=True)
        self.bufs.append(b)
        return t, b

    def close(self):
        self.cx.P.barrier()
        self.es.close()


def load_w(cx, sc, w2d, kc, n0, n1, name, dst=None, dcol=0):
    P = cx.P
    n = n1 - n0
    if dst is None:
        t, b = sc.sb([128, kc, n], BF16, name)
    else:
        t, b = dst
    for c in range(kc):
        P.add("pool", lambda e, c=c: e.dma_start(out=t[:, c, dcol:dcol + n], in_=w2d[c * 128:(c + 1) * 128, n0:n1]),
              W=[b], chan="pl")
    return t, b


def load_vec_cols(cx, sc, v1d, kc, name):
    t, b = sc.sb([128, kc], F32, name)
    cx.P.add("sp", lambda e: e.dma_start(out=t[:, :], in_=v1d.rearrange("(c p) -> p c", p=128),
                                         allow_slow_non_contiguous=True), W=[b], chan="wl")
    return t, b


def rmsnorm_T(cx, sc, st, xt, xb, g, gb, TT):
    P = cx.P
    sq, sqb = st["sq"]
    hT, hb = st["hT"]
    ssp, sspb = st["ssp"]
    rs, rsb = st["rs"]
    ones, onesb = st["ones"]
    P.add("act", lambda e: e.activation(out=sq[:, :, :], in_=xt[:, :, :], func=AF.Square), R=[xb], W=[sqb])
    for c in range(KD):
        P.add("pe", lambda e, c=c: e.matmul(ssp[:, 0:TT], ones[:, :], sq[:, c, :], start=(c == 0), stop=(c == KD - 1)),
              R=[sqb, onesb], W=[sspb])
    P.add("act", lambda e: e.activation(out=rs[:, 0:TT], in_=ssp[:, 0:TT], func=AF.Sqrt, scale=1.0 / D, bias=st["eps"][0][:, 0:1]),
          R=[sspb, st["eps"][1]], W=[rsb])
    P.add("dve", lambda e: e.reciprocal(out=rs[:, 0:TT], in_=rs[:, 0:TT]), R=[rsb], W=[rsb])
    for c in range(KD):
        P.add("dve", lambda e, c=c: e.scalar_tensor_tensor(out=hT[:, c, :], in0=xt[:, c, :], scalar=g[:, c:c + 1],
                                                           in1=rs[:, 0:TT], op0=ALU.mult, op1=ALU.mult),
              R=[xb, gb, rsb], W=[hb])
    return hT, hb


def norm_scratch(cx, sc, TT):
    st = {}
    st["sq"] = sc.sb([128, KD, TT], BF16, "sq")
    st["hT"] = sc.sb([128, KD, TT], BF16, "hT")
    st["ssp"] = sc.ps([128, 512], F32, "ssp")
    st["rs"] = sc.sb([128, TT], F32, "rs")
    st["ones"] = sc.sb([128, 128], BF16, "ones")
    st["eps"] = sc.sb([128, 1], F32, "eps")
    ones, onesb = st["ones"]
    eps, epsb = st["eps"]
    cx.P.add("pool", lambda e: e.memset(ones[:, :], 1.0), W=[onesb])
    cx.P.add("pool", lambda e: e.memset(eps[:, :], EPS), W=[epsb])
    return st


def make_identity(cx, sc, dt, name="ident"):
    t, b = sc.sb([128, 128], dt, name)
    cx.P.add("pool", lambda e: e.memset(t[:, :], 1.0), W=[b])
    cx.P.add("pool", lambda e: e.affine_select(out=t[:, :], in_=t[:, :], pattern=[[-1, 128]], compare_op=ALU.is_equal,
                                               fill=0.0, base=0, channel_multiplier=1), R=[b], W=[b])
    return t, b


def phase_in(cx, G):
    P = cx.P
    TT = G["TT"]
    sc = Scope(cx)
    ident, identb = make_identity(cx, sc, F32)
    xin = [sc.sb([128, TT // 128, D], F32, "xin") for _ in range(2)]
    xo = [sc.sb([128, KD, TT], F32, "xo") for _ in range(2)]
    pp = [sc.ps([128, 2, TT], F32, "tp") for _ in range(4)]
    x = G["x"]
    for i in range(G["NT"]):
        xi, xib = xin[i % 2]
        xo_t, xob = xo[i % 2]
        P.add("sp", lambda e, i=i, xi=xi: e.dma_start(
            out=xi[:, :, :], in_=x[i * TT:(i + 1) * TT, :].rearrange("(a p) d -> p a d", p=128)), W=[xib], chan="ld")
        for c2 in range(KD // 2):
            pt, ptb = pp[c2 % 4]
            for cc in range(2):
                c = c2 * 2 + cc
                for a in range(TT // 128):
                    P.add("pe", lambda e, c=c, cc=cc, a=a, pt=pt, xi=xi: e.transpose(
                        pt[:, cc, a * 128:(a + 1) * 128], xi[:, a, c * 128:(c + 1) * 128], ident[:, :]),
                        R=[xib, identb], W=[ptb])
            eng = "act" if c2 % 2 == 0 else "dve"
            if eng == "act":
                P.add("act", lambda e, c2=c2, pt=pt, xo_t=xo_t: e.copy(out=xo_t[:, 2 * c2:2 * c2 + 2, :], in_=pt[:, :, :]),
                      R=[ptb], W=[xob])
            else:
                P.add("dve", lambda e, c2=c2, pt=pt, xo_t=xo_t: e.tensor_copy(out=xo_t[:, 2 * c2:2 * c2 + 2, :], in_=pt[:, :, :]),
                      R=[ptb], W=[xob])
        P.add("sp", lambda e, i=i, xo_t=xo_t: e.dma_start(
            out=G["XT"][:, i * TT:(i + 1) * TT].rearrange("(c p) t -> p c t", p=128), in_=xo_t[:, :, :]),
            R=[xob], W=[G["XTb"][i]], chan="st")
    sc.close()


def phase_out(cx, G):
    P = cx.P
    TT = G["TT"]
    sc = Scope(cx)
    ident, identb = make_identity(cx, sc, F32)
    st = norm_scratch(cx, sc, TT)
    g, gb = load_vec_cols(cx, sc, G["w"]["final_norm"], KD, "gfin")
    xin = [sc.sb([128, KD, TT], F32, "xin") for _ in range(2)]
    yn = sc.sb([128, KD, TT], F32, "yn")
    yo = [sc.sb([128, D], F32, "yo") for _ in range(2)]
    pp = [sc.ps([128, 512], F32, "tp") for _ in range(4)]
    rs, rsb = st["rs"]
    sq, sqb = st["sq"]
    ssp, sspb = st["ssp"]
    ones, onesb = st["ones"]
    k = 0
    for i in range(G["NT"]):
        xt, xb = xin[i % 2]
        P.add("sp", lambda e, i=i, xt=xt: e.dma_start(
            out=xt[:, :, :], in_=G["XT"][:, i * TT:(i + 1) * TT].rearrange("(c p) t -> p c t", p=128)),
            R=[G["XTb"][i]], W=[xb], chan="ld")
        P.add("act", lambda e, xt=xt: e.activation(out=sq[:, :, :], in_=xt[:, :, :], func=AF.Square), R=[xb], W=[sqb])
        for c in range(KD):
            P.add("pe", lambda e, c=c: e.matmul(ssp[:, 0:TT], ones[:, :], sq[:, c, :], start=(c == 0), stop=(c == KD - 1)),
                  R=[sqb, onesb], W=[sspb])
        P.add("act", lambda e: e.activation(out=rs[:, 0:TT], in_=ssp[:, 0:TT], func=AF.Sqrt, scale=1.0 / D, bias=st["eps"][0][:, 0:1]),
              R=[sspb, st["eps"][1]], W=[rsb])
        P.add("dve", lambda e: e.reciprocal(out=rs[:, 0:TT], in_=rs[:, 0:TT]), R=[rsb], W=[rsb])
        y, yb = yn
        for c in range(KD):
            P.add("dve", lambda e, c=c, xt=xt: e.scalar_tensor_tensor(out=y[:, c, :], in0=xt[:, c, :], scalar=g[:, c:c + 1],
                                                                      in1=rs[:, 0:TT], op0=ALU.mult, op1=ALU.mult),
                  R=[xb, gb, rsb], W=[yb])
        for a in range(TT // 128):
            yt, ytb = yo[k % 2]
            for hh in range(2):
                pt, ptb = pp[(2 * k + hh) % 4]
                for c4 in range(4):
                    c = hh * 4 + c4
                    P.add("pe", lambda e, c=c, c4=c4, a=a, pt=pt: e.transpose(
                        pt[:, c4 * 128:(c4 + 1) * 128], y[:, c, a * 128:(a + 1) * 128], ident[:, :]),
                        R=[yb, identb], W=[ptb])
                if hh == 0:
                    P.add("act", lambda e, pt=pt, yt=yt: e.copy(out=yt[:, 0:512], in_=pt[:, :]), R=[ptb], W=[ytb])
                else:
                    P.add("dve", lambda e, pt=pt, yt=yt: e.tensor_copy(out=yt[:, 512:1024], in_=pt[:, :]), R=[ptb], W=[ytb])
            r0 = i * TT + a * 128
            P.add("sp", lambda e, r0=r0, yt=yt: e.dma_start(out=G["out"][r0:r0 + 128, :], in_=yt[:, :]),
                  R=[ytb], W=[G["outb"]], chan="st")
            k += 1
    sc.close()


def phase_ffn(cx, G, l, which):
    P = cx.P
    TT = G["TT"]
    w = G["w"]
    sc = Scope(cx)
    pre = "ffn1" if which == 1 else "ffn2"
    wgu, wgub = load_w(cx, sc, w[pre + "_w_gu"][l], KD, 0, 2 * FF, "wgu")
    wd, wdb = load_w(cx, sc, w[pre + "_w_down"][l], KF, 0, D, "wd")
    g, gb = load_vec_cols(cx, sc, w["norm_" + pre][l], KD, "gn")
    st = norm_scratch(cx, sc, TT)
    xin = [sc.sb([128, KD, TT], F32, "xin") for _ in range(2)]
    m, mb = sc.sb([128, KF, TT], BF16, "m")
    av = [sc.sb([128, TT], F32, "a") for _ in range(2)]
    pgu = [sc.ps([128, 2, TT], F32, "pgu") for _ in range(3)]
    pdn = [sc.ps([128, 2, TT], F32, "pdn") for _ in range(2)]

    def load(i):
        xt, xb = xin[i % 2]
        P.add("sp", lambda e: e.dma_start(
            out=xt[:, :, :], in_=G["XT"][:, i * TT:(i + 1) * TT].rearrange("(c p) t -> p c t", p=128)),
            R=[G["XTb"][i]], W=[xb], chan="ld")

    hTs = [st["hT"], sc.sb([128, KD, TT], BF16, "hT2")]

    def norm(i):
        st["hT"] = hTs[i % 2]
        return rmsnorm_T(cx, sc, st, xin[i % 2][0], xin[i % 2][1], g, gb, TT)

    load(0)
    nxt = norm(0)
    for i in range(G["NT"]):
        xt, xb = xin[i % 2]
        if i + 1 < G["NT"]:
            load(i + 1)
        hT, hb = nxt
        for j in range(KF):
            pg, pgb = pgu[j % 3]
            for half in range(2):
                col = half * FF + j * 128
                for c in range(KD):
                    P.add("pe", lambda e, c=c, col=col, half=half, pg=pg, hT=hT: e.matmul(
                        pg[:, half, :], wgu[:, c, col:col + 128], hT[:, c, :], start=(c == 0), stop=(c == KD - 1)),
                        R=[wgub, hb], W=[pgb])
            a, ab = av[j % 2]
            P.add("act", lambda e, pg=pg, a=a: e.activation(out=a[:, :], in_=pg[:, 0, :], func=AF.Silu), R=[pgb], W=[ab])
            P.add("dve", lambda e, pg=pg, a=a, j=j: e.tensor_tensor(out=m[:, j, :], in0=a[:, :], in1=pg[:, 1, :], op=ALU.mult),
                  R=[pgb, ab], W=[mb])
        if i + 1 < G["NT"]:
            nxt = norm(i + 1)
        for o2 in range(KD // 2):
            pd, pdb = pdn[o2 % 2]
            for oo in range(2):
                oc = o2 * 2 + oo
                for j in range(KF):
                    P.add("pe", lambda e, j=j, oc=oc, oo=oo, pd=pd: e.matmul(
                        pd[:, oo, :], wd[:, j, oc * 128:(oc + 1) * 128], m[:, j, :], start=(j == 0), stop=(j == KF - 1)),
                        R=[wdb, mb], W=[pdb])
            P.add("dve", lambda e, o2=o2, pd=pd, xt=xt: e.scalar_tensor_tensor(
                out=xt[:, 2 * o2:2 * o2 + 2, :], in0=pd[:, :, :], scalar=0.5, in1=xt[:, 2 * o2:2 * o2 + 2, :],
                op0=ALU.mult, op1=ALU.add), R=[pdb, xb], W=[xb])
        P.add("sp", lambda e, i=i, xt=xt: e.dma_start(
            out=G["XT"][:, i * TT:(i + 1) * TT].rearrange("(c p) t -> p c t", p=128), in_=xt[:, :, :]),
            R=[xb], W=[G["XTb"][i]], chan="st")
    sc.close()


OFF_CB, OFF_CC, OFF_CX = 0, 512, 1024
OFF_MQ, OFF_MK, OFF_MV, OFF_MO, OFF_MI, OFF_MF = 1536, 2048, 2560, 3584, 4608, 4612
OFF_AQ, OFF_AK, OFF_AV, OFF_IQ, OFF_IK, OFF_IW = 4616, 5128, 5192, 5256, 5512, 5576
OFF_G = 5580


def xt_ap(G, i):
    TT = G["TT"]
    return G["XT"][:, i * TT:(i + 1) * TT].rearrange("(c p) t -> p c t", p=128)


def ht_ap(G, i):
    TT = G["TT"]
    return G["HT"][:, i * TT:(i + 1) * TT].rearrange("(c p) t -> p c t", p=128)


def phase_norm(cx, G, l):
    P = cx.P
    TT = G["TT"]
    sc = Scope(cx)
    g, gb = load_vec_cols(cx, sc, G["w"]["norm_mix"][l], KD, "gn")
    st = norm_scratch(cx, sc, TT)
    xin = [sc.sb([128, KD, TT], F32, "xin") for _ in range(2)]
    ho = [sc.sb([128, KD, TT], BF16, "ho") for _ in range(2)]
    for i in range(G["NT"]):
        xt, xb = xin[i % 2]
        P.add("sp", lambda e, i=i, xt=xt: e.dma_start(out=xt[:, :, :], in_=xt_ap(G, i)), R=[G["XTb"][i]], W=[xb], chan="ld")
        st["hT"] = ho[i % 2]
        hT, hb = rmsnorm_T(cx, sc, st, xt, xb, g, gb, TT)
        P.add("sp", lambda e, i=i, hT=hT: e.dma_start(out=ht_ap(G, i), in_=hT[:, :, :]), R=[hb], W=[G["HTb"][i]], chan="st")
    sc.close()


class Tail:
    def __init__(self, cx, sc, G, l, br, wout2d, kin, psA, psB):
        w = G["w"]
        TT = G["TT"]
        self.cx, self.G, self.kin = cx, G, kin
        self.wg = load_w(cx, sc, w["w_in"][l], KD, OFF_G + br * D, OFF_G + (br + 1) * D, "wgate")
        self.wout = load_w(cx, sc, wout2d, kin, 0, D, "wout")
        self.wo = load_w(cx, sc, w["w_o"][l], KD, 0, D, "wo")
        self.sg = [sc.sb([128, TT], F32, "sg") for _ in range(2)]
        self.mg = sc.sb([128, KD, TT], BF16, "mg")
        self.xin = [sc.sb([128, KD, TT], F32, "xres") for _ in range(2)]
        self.psA, self.psB = psA, psB

    def load_x(self, i):
        G = self.G
        xt, xb = self.xin[i % 2]
        self.cx.P.add("sp", lambda e: e.dma_start(out=xt[:, :, :], in_=xt_ap(G, i)), R=[G["XTb"][i]], W=[xb], chan="ld")

    def run(self, i, hT, hb, yT, yb):
        P = self.cx.P
        G = self.G
        wg, wgb = self.wg
        wout, woutb = self.wout
        wo, wob = self.wo
        mg, mgb = self.mg
        xt, xb = self.xin[i % 2]
        kin = self.kin
        for oc in range(KD):
            pa, pab = self.psA[oc % len(self.psA)]
            for c in range(kin):
                P.add("pe", lambda e, c=c, oc=oc, pa=pa: e.matmul(pa[:, 0, :], wout[:, c, oc * 128:(oc + 1) * 128], yT[:, c, :],
                                                               start=(c == 0), stop=(c == kin - 1)), R=[woutb, yb], W=[pab])
            for c in range(KD):
                P.add("pe", lambda e, c=c, oc=oc, pa=pa: e.matmul(pa[:, 1, :], wg[:, c, oc * 128:(oc + 1) * 128], hT[:, c, :],
                                                               start=(c == 0), stop=(c == KD - 1)), R=[wgb, hb], W=[pab])
            sg, sgb = self.sg[oc % 2]
            P.add("act", lambda e, pa=pa, sg=sg: e.activation(out=sg[:, :], in_=pa[:, 1, :], func=AF.Sigmoid), R=[pab], W=[sgb])
            P.add("dve", lambda e, pa=pa, sg=sg, oc=oc: e.tensor_tensor(out=mg[:, oc, :], in0=sg[:, :], in1=pa[:, 0, :], op=ALU.mult),
                  R=[pab, sgb], W=[mgb])
        for o2 in range(KD // 2):
            po, pob = self.psB[o2 % len(self.psB)]
            for oo in range(2):
                oc = o2 * 2 + oo
                for c in range(KD):
                    P.add("pe", lambda e, c=c, oc=oc, oo=oo, po=po: e.matmul(po[:, oo, :], wo[:, c, oc * 128:(oc + 1) * 128], mg[:, c, :],
                                                                         start=(c == 0), stop=(c == KD - 1)), R=[wob, mgb], W=[pob])
            P.add("dve", lambda e, o2=o2, po=po: e.tensor_tensor(out=xt[:, 2 * o2:2 * o2 + 2, :], in0=xt[:, 2 * o2:2 * o2 + 2, :],
                                                              in1=po[:, :, :], op=ALU.add), R=[pob, xb], W=[xb])
        P.add("sp", lambda e: e.dma_start(out=xt_ap(G, i), in_=xt[:, :, :]), R=[xb], W=[G["XTb"][i]], chan="st")


def phase_conv(cx, G, l):
    P = cx.P
    TT = G["TT"]
    w = G["w"]
    sc = Scope(cx)
    win, winb = load_w(cx, sc, w["w_in"][l], KD, 0, 1536, "winc")
    psA = [sc.ps([128, 2, TT], F32, "psA") for _ in range(2)]
    psB = [sc.ps([128, 2, TT], F32, "psB") for _ in range(2)]
    pcx = [sc.ps([128, 2, TT], F32, "pcx") for _ in range(2)]
    pb_ = [sc.ps([128, 2, TT], F32, "pbb") for _ in range(2)]
    tail = Tail(cx, sc, G, l, 0, w["conv_w_out"][l], 4, psA, psB)
    cw, cwb = sc.sb([128, 4, 3], F32, "cw")
    for j in range(3):
        P.add("sp", lambda e, j=j: e.dma_start(out=cw[:, :, j], in_=w["conv_w"][l][j].rearrange("(c p) -> p c", p=128),
                                               allow_slow_non_contiguous=True), W=[cwb], chan="wl")
    hin = [sc.sb([128, KD, TT], BF16, "hin") for _ in range(2)]
    u = [sc.sb([128, TT + 2], F32, "u") for _ in range(4)]
    ccs = [sc.sb([128, TT], F32, "ccs") for _ in range(2)]
    yv = [sc.sb([128, TT], F32, "yv") for _ in range(2)]
    z, zb = sc.sb([128, 4, TT], BF16, "z")
    tiles_per_seq = G["S"] // TT

    def load(i):
        hT, hb = hin[i % 2]
        P.add("sp", lambda e: e.dma_start(out=hT[:, :, :], in_=ht_ap(G, i)), R=[G["HTb"][i]], W=[hb], chan="ld")
        tail.load_x(i)

    load(0)
    for i in range(G["NT"]):
        hT, hb = hin[i % 2]
        if i + 1 < G["NT"]:
            load(i + 1)
        for q in range(4):
            ut, ub = u[q]
            if i % tiles_per_seq == 0:
                P.add("pool", lambda e, ut=ut: e.memset(ut[:, 0:2], 0.0), W=[ub])
            p1, p1b = pcx[q % 2]
            p2, p2b = pb_[q % 2]
            for k_, (pt, ptb, slot, off) in enumerate(((p1, p1b, 0, OFF_CC), (p1, p1b, 1, OFF_CX), (p2, p2b, 0, OFF_CB))):
                for c in range(KD):
                    P.add("pe", lambda e, c=c, pt=pt, slot=slot, col=off + q * 128, hT=hT: e.matmul(
                        pt[:, slot, :], win[:, c, col:col + 128], hT[:, c, :], start=(c == 0), stop=(c == KD - 1)),
                        R=[winb, hb], W=[ptb])
            cs, csb = ccs[q % 2]
            P.add("act", lambda e, p1=p1, cs=cs: e.copy(out=cs[:, :], in_=p1[:, 0, :]), R=[p1b], W=[csb])
            P.add("dve", lambda e, p1=p1, cs=cs, ut=ut: e.tensor_tensor(out=ut[:, 2:TT + 2], in0=cs[:, :], in1=p1[:, 1, :], op=ALU.mult),
                  R=[p1b, csb], W=[ub])
            y, yb_ = yv[q % 2]
            P.add("dve", lambda e, y=y, ut=ut, q=q: e.tensor_scalar(out=y[:, :], in0=ut[:, 0:TT], scalar1=cw[:, q, 0:1], scalar2=None,
                                                                  op0=ALU.mult), R=[ub, cwb], W=[yb_])
            P.add("dve", lambda e, y=y, ut=ut, q=q: e.scalar_tensor_tensor(out=y[:, :], in0=ut[:, 1:TT + 1], scalar=cw[:, q, 1:2],
                                                                         in1=y[:, :], op0=ALU.mult, op1=ALU.add), R=[ub, cwb, yb_], W=[yb_])
            P.add("dve", lambda e, y=y, ut=ut, q=q: e.scalar_tensor_tensor(out=y[:, :], in0=ut[:, 2:TT + 2], scalar=cw[:, q, 2:3],
                                                                         in1=y[:, :], op0=ALU.mult, op1=ALU.add), R=[ub, cwb, yb_], W=[yb_])
            P.add("pool", lambda e, ut=ut: e.tensor_copy(out=ut[:, 0:2], in_=ut[:, TT:TT + 2]), R=[ub], W=[ub])
            P.add("dve", lambda e, y=y, p2=p2, q=q: e.tensor_tensor(out=z[:, q, :], in0=y[:, :], in1=p2[:, 0, :], op=ALU.mult),
                  R=[yb_, p2b], W=[zb])
        tail.run(i, hT, hb, z, zb)
    sc.close()


def phase_ple(cx, G, l):
    P = cx.P
    TT = G["TT"]
    w = G["w"]
    sc = Scope(cx)
    wg, wgb = load_w(cx, sc, w["ple_w_gate"][l], KD, 0, D, "wpg")
    wp, wpb = load_w(cx, sc, w["ple_w_proj"][l], 2, 0, D, "wpp")
    g, gb = load_vec_cols(cx, sc, w["norm_ple"][l], KD, "gn")
    st = norm_scratch(cx, sc, TT)
    ident, identb = make_identity(cx, sc, F32)
    xin = [sc.sb([128, KD, TT], F32, "xin") for _ in range(2)]
    pin = [sc.sb([128, TT // 128, D_PLE], F32, "pin") for _ in range(2)]
    pT, pTb = sc.sb([128, 2, TT], BF16, "pT")
    sgs = [sc.sb([128, TT], F32, "sg") for _ in range(2)]
    ptp = sc.ps([128, 2, TT], F32, "ptp")
    psA = [sc.ps([128, 2, TT], F32, "psA") for _ in range(3)]

    def load(i):
        xt, xb = xin[i % 2]
        P.add("sp", lambda e: e.dma_start(out=xt[:, :, :], in_=xt_ap(G, i)), R=[G["XTb"][i]], W=[xb], chan="ld")
        pi, pib = pin[i % 2]
        P.add("sp", lambda e: e.dma_start(out=pi[:, :, :], in_=G["p"][l, i * TT:(i + 1) * TT, :].rearrange("(a p) d -> p a d", p=128)),
              W=[pib], chan="ld")

    hTs = [st["hT"], sc.sb([128, KD, TT], BF16, "hT2")]

    def norm(i):
        st["hT"] = hTs[i % 2]
        return rmsnorm_T(cx, sc, st, xin[i % 2][0], xin[i % 2][1], g, gb, TT)

    load(0)
    nxt = norm(0)
    for i in range(G["NT"]):
        xt, xb = xin[i % 2]
        pi, pib = pin[i % 2]
        if i + 1 < G["NT"]:
            load(i + 1)
        hT, hb = nxt
        pt, ptb = ptp
        for kc in range(2):
            for a in range(TT // 128):
                P.add("pe", lambda e, kc=kc, a=a, pi=pi: e.transpose(pt[:, kc, a * 128:(a + 1) * 128], pi[:, a, kc * 128:(kc + 1) * 128], ident[:, :]),
                      R=[pib, identb], W=[ptb])
        P.add("act", lambda e: e.copy(out=pT[:, :, :], in_=pt[:, :, :]), R=[ptb], W=[pTb])
        for oc in range(KD):
            if oc == 4 and i + 1 < G["NT"]:
                nxt = norm(i + 1)
            pa, pab = psA[oc % 3]
            for c in range(KD):
                P.add("pe", lambda e, c=c, oc=oc, pa=pa, hT=hT: e.matmul(pa[:, 0, :], wg[:, c, oc * 128:(oc + 1) * 128], hT[:, c, :],
                                                               start=(c == 0), stop=(c == KD - 1)), R=[wgb, hb], W=[pab])
            for c in range(2):
                P.add("pe", lambda e, c=c, oc=oc, pa=pa: e.matmul(pa[:, 1, :], wp[:, c, oc * 128:(oc + 1) * 128], pT[:, c, :],
                                                               start=(c == 0), stop=(c == 1)), R=[wpb, pTb], W=[pab])
            sg, sgb = sgs[oc % 2]
            P.add("act", lambda e, pa=pa, sg=sg: e.activation(out=sg[:, :], in_=pa[:, 0, :], func=AF.Sigmoid), R=[pab], W=[sgb])
            if G.get("dbg_dump") and i == 0 and oc == 0:
                P.add("sp", lambda e, sg=sg: e.dma_start(out=G["dbgo"][:, 0:TT], in_=sg[:, :]), R=[sgb], W=[G["outb"]], chan="st")
                d2, d2b = sc.sb([128, TT], F32, "d2")
                P.add("act", lambda e, pa=pa: e.copy(out=d2[:, :], in_=pa[:, 1, :]), R=[pab], W=[d2b])
                P.add("sp", lambda e: e.dma_start(out=G["dbgo"][:, TT:2 * TT], in_=d2[:, :]), R=[d2b], W=[G["outb"]], chan="st")
                d3, d3b = sc.sb([128, 2, TT], F32, "d3")
                P.add("act", lambda e: e.copy(out=d3[:, :, :], in_=pT[:, :, :]), R=[pTb], W=[d3b])
                P.add("sp", lambda e: e.dma_start(out=G["dbgo"][:, 2 * TT:4 * TT], in_=d3[:, :, :].rearrange("p a t -> p (a t)")), R=[d3b], W=[G["outb"]], chan="st")
            P.add("dve", lambda e, pa=pa, sg=sg: e.tensor_tensor(out=sg[:, :], in0=sg[:, :], in1=pa[:, 1, :], op=ALU.mult),
                  R=[pab, sgb], W=[sgb])
            if not G.get("dbg_skip_add"):
                P.add("pool", lambda e, sg=sg, oc=oc, xt=xt: e.tensor_tensor(out=xt[:, oc, :], in0=xt[:, oc, :], in1=sg[:, :], op=ALU.add),
                      R=[sgb, xb], W=[xb])
        P.add("sp", lambda e, i=i, xt=xt: e.dma_start(out=xt_ap(G, i), in_=xt[:, :, :]), R=[xb], W=[G["XTb"][i]], chan="st")
    sc.close()


import math


def OP(cx, eng, method, *args, R=(), W=(), chan=None, **kw):
    return cx.P.add(eng, lambda e: getattr(e, method)(*args, **kw), R=R, W=W, chan=chan)


def flat(t):
    return t[:, :, :].rearrange("p a t -> p (a t)")


def make_tri(cx, sc, dt, val, name):
    t, b = sc.sb([128, 128], dt, name)
    OP(cx, "pool", "memset", t[:, :], val, W=[b])
    OP(cx, "pool", "affine_select", out=t[:, :], in_=t[:, :], pattern=[[1, 128]], compare_op=ALU.is_ge, fill=0.0, base=0,
       channel_multiplier=-1, R=[b], W=[b])
    return t, b


def phase_mlstm(cx, G, l):
    P = cx.P
    TT = G["TT"]
    w = G["w"]
    sc = Scope(cx)
    win, winb = load_w(cx, sc, w["w_in"][l], KD, OFF_MQ, OFF_AQ, "winm")
    pA, pAb = sc.ps([128, 2, TT], F32, "pA")
    pK, pKb = sc.ps([128, 2, TT], F32, "pK")
    pV, pVb = sc.ps([128, 2, TT], F32, "pV")
    pG, pGb = sc.ps([128, 2, TT], F32, "pG")
    pB, pBb = sc.ps([128, 4, 128], F32, "pB")
    pS, pSb = sc.ps([128, 4, 128], F32, "pS")
    pN = [sc.ps([128, 2, TT], F32, "pN") for _ in range(2)]
    tail = Tail(cx, sc, G, l, 1, w["mlstm_w_out"][l], 8, [(pK, pKb), (pV, pVb)], pN)
    identb16, identb16b = make_identity(cx, sc, BF16, "identb")
    NU, NUb = make_tri(cx, sc, BF16, -1.0, "NU")
    U, Ub = make_tri(cx, sc, F32, 1.0, "U")
    ones3, ones3b = sc.sb([128, 4, 128], BF16, "ones3")
    l4h, l4hb = sc.sb([128, 2, 4], BF16, "l4h")
    lbh, lbhb = sc.sb([128, 2, 4, 128], BF16, "lbh")
    OP(cx, "pool", "memset", ones3[:, :, :], 1.0, W=[ones3b])
    nw, nwb = sc.sb([128, D], F32, "nw")
    OP(cx, "sp", "dma_start", out=nw[:, :], in_=w["mlstm_norm"][l].partition_broadcast(128), W=[nwb], chan="wl")
    bi_t, bib = sc.sb([128, 4], F32, "bi")
    bf_t, bfb = sc.sb([128, 4], F32, "bf")
    OP(cx, "sp", "dma_start", out=bi_t[:, :], in_=w["mlstm_b_i"][l].partition_broadcast(128), W=[bib], chan="wl")
    OP(cx, "sp", "dma_start", out=bf_t[:, :], in_=w["mlstm_b_f"][l].partition_broadcast(128), W=[bfb], chan="wl")
    eps_t, epsb = sc.sb([128, 1], F32, "eps")
    OP(cx, "pool", "memset", eps_t[:, :], EPS, W=[epsb])
    hin = [sc.sb([128, KD, TT], BF16, "hin") for _ in range(2)]
    qT, qTb = sc.sb([128, 4, TT], BF16, "qT")
    kT, kTb = sc.sb([128, 4, TT], BF16, "kT")
    vp, vpb = sc.sb([128, 4, 257], BF16, "vp")
    OP(cx, "pool", "memset", vp[:, :, :], 1.0, W=[vpb])
    og, ogb = sc.sb([128, D], F32, "og")
    gi, gib = sc.sb([128, 4], F32, "gi")
    l4, l4b = sc.sb([128, 4], F32, "l4")
    colb, colbb = sc.sb([128, 4], F32, "colb")
    bl, blb = sc.sb([128, 4], F32, "bl")
    wk, wkb = sc.sb([128, 4], F32, "wk")
    r4, r4b = sc.sb([128, 4], F32, "r4")
    msq, msqb = sc.sb([128, 4], F32, "msq")
    lb, lbb = sc.sb([128, 4, 128], F32, "lb")
    eb, ebb = sc.sb([128, 4, 128], F32, "eb")
    DT, DTb = sc.sb([128, 4, 128], F32, "DT")
    AT, ATb = sc.sb([128, 4, 128], BF16, "AT")
    qs, qsb = sc.sb([128, 4, 128], BF16, "qs")
    kts, ktsb = sc.sb([128, 4, 128], BF16, "kts")
    C32, C32b = sc.sb([128, 4, 257], F32, "C32")
    Cbf, Cbfb = sc.sb([128, 4, 257], BF16, "Cbf")
    tmp, tmpb = sc.sb([128, D], F32, "tmp")
    tmp2, tmp2b = sc.sb([128, D], F32, "tmp2")
    junk, junkb = sc.sb([128, 256], F32, "junk")
    hn, hnb = sc.sb([128, D], BF16, "hn")
    ymT, ymTb = sc.sb([128, KD, TT], BF16, "ymT")
    pAf, pKf, pVf, pGf = flat(pA), flat(pK), flat(pV), flat(pG)
    pAT = pAf.bitcast(BF16).rearrange("p (c t) -> p c t", t=128)
    tiles_per_seq = G["S"] // TT
    kscale = 128 ** -0.5

    def load(i):
        hT, hb = hin[i % 2]
        OP(cx, "sp", "dma_start", out=hT[:, :, :], in_=ht_ap(G, i), R=[G["HTb"][i]], W=[hb], chan="ld")
        tail.load_x(i)

    load(0)
    for i in range(G["NT"]):
        hT, hb = hin[i % 2]
        if i + 1 < G["NT"]:
            load(i + 1)
        if i % tiles_per_seq == 0:
            OP(cx, "pool", "memset", C32[:, :, :], 0.0, W=[C32b])
            OP(cx, "pool", "memset", Cbf[:, :, :], 0.0, W=[Cbfb])
        for which, dst, dstb, off in ((0, qT, qTb, 0), (1, kT, kTb, 512)):
            for hp in range(2):
                for hh in range(2):
                    h = hp * 2 + hh
                    for c in range(KD):
                        OP(cx, "pe", "matmul", pA[:, hh, :], win[:, c, off + h * 128:off + (h + 1) * 128], hT[:, c, :],
                           start=(c == 0), stop=(c == KD - 1), R=[winb, hb], W=[pAb])
                if which == 0:
                    OP(cx, "act", "copy", out=dst[:, 2 * hp:2 * hp + 2, :], in_=pA[:, :, :], R=[pAb], W=[dstb])
                else:
                    OP(cx, "act", "mul", dst[:, 2 * hp:2 * hp + 2, :], pA[:, :, :], kscale, R=[pAb], W=[dstb])
        for a in range(TT // 128):
            ts = slice(a * 128, (a + 1) * 128)
            if G.get("dbg_stop", 99) <= 0:
                continue
            for c in range(KD):
                OP(cx, "pe", "matmul", pGf[:, 0:8], hT[:, c, ts], win[:, c, 3072:3080], start=(c == 0), stop=(c == KD - 1),
                   R=[winb, hb], W=[pGb])
            if G.get("dbg_stop", 99) == 0.5:
                continue
            OP(cx, "dve", "tensor_tensor", out=gi[:, :], in0=pGf[:, 0:4], in1=bi_t[:, :], op=ALU.add, R=[pGb, bib], W=[gib])
            OP(cx, "dve", "tensor_tensor", out=l4[:, :], in0=pGf[:, 4:8], in1=bf_t[:, :], op=ALU.add, R=[pGb, bfb], W=[l4b])
            OP(cx, "act", "activation", out=l4[:, :], in_=l4[:, :], func=AF.Exp, scale=-1.0, R=[l4b], W=[l4b])
            OP(cx, "act", "activation", out=l4[:, :], in_=l4[:, :], func=AF.Ln, bias=1.0, R=[l4b], W=[l4b])
            if G.get("dbg_stop", 99) == 0.7:
                continue
            OP(cx, "dve", "tensor_copy", out=l4h[:, 0, :], in_=l4[:, :], R=[l4b], W=[l4hb])
            OP(cx, "dve", "tensor_tensor", out=l4h[:, 1, :], in0=l4[:, :], in1=l4h[:, 0, :], op=ALU.subtract, R=[l4b, l4hb], W=[l4hb])
            for z in range(2):
                OP(cx, "dve", "tensor_tensor", out=lbh[:, z, :, :], in0=ones3[:, :, :],
                   in1=l4h[:, z, :].unsqueeze(2).to_broadcast([128, 4, 128]), op=ALU.mult, R=[ones3b, l4hb], W=[lbhb])
            if G.get("dbg_stop", 99) == 0.8:
                continue
            for z in range(2):
                OP(cx, "pe", "matmul", pGf[:, 8:12], NU[:, :], l4h[:, z, :], start=(z == 0), stop=(z == 1), R=[NUb, l4hb], W=[pGb])
            for h in range(4):
                for z in range(2):
                    OP(cx, "pe", "matmul", pB[:, h, :], lbh[:, z, h, :], NU[:, :], start=(z == 0), stop=(z == 1), R=[lbhb, NUb], W=[pBb])
            if G.get("dbg_stop", 99) == 0.9:
                continue
            OP(cx, "act", "activation", out=eb[:, :, :], in_=pB[:, :, :], func=AF.Exp, R=[pBb], W=[ebb])
            OP(cx, "dve", "tensor_tensor", out=colb[:, :], in0=gi[:, :], in1=pGf[:, 8:12], op=ALU.subtract, R=[gib, pGb], W=[colbb])
            OP(cx, "dve", "tensor_copy", out=bl[:, :], in_=pB[:, :, 127], R=[pBb], W=[blb])
            OP(cx, "dve", "tensor_tensor", out=wk[:, :], in0=colb[:, :], in1=bl[:, :], op=ALU.add, R=[colbb, blb], W=[wkb])
            OP(cx, "act", "activation", out=wk[:, :], in_=wk[:, :], func=AF.Exp, R=[wkb], W=[wkb])
            OP(cx, "dve", "tensor_scalar", out=wk[:, :], in0=wk[:, :], scalar1=kscale, scalar2=None, op0=ALU.mult, R=[wkb], W=[wkb])
            if G.get("dbg_stop", 99) <= 1:
                continue
            for h in range(4):
                OP(cx, "pe", "matmul", pS[:, h, :], kT[:, h, ts], qT[:, h, ts], start=True, stop=True, R=[kTb, qTb], W=[pSb])
            for h in range(4):
                OP(cx, "act", "activation", out=DT[:, h, :], in_=pB[:, h, :], func=AF.Exp, bias=colb[:, h:h + 1], R=[pBb, colbb], W=[DTb])
            OP(cx, "pool", "tensor_tensor", out=DT[:, :, :], in0=DT[:, :, :], in1=U[:, :].unsqueeze(1).to_broadcast([128, 4, 128]),
               op=ALU.mult, R=[DTb, Ub], W=[DTb])
            OP(cx, "dve", "tensor_tensor", out=AT[:, :, :], in0=DT[:, :, :], in1=pS[:, :, :], op=ALU.mult, R=[DTb, pSb], W=[ATb])
            OP(cx, "pool", "tensor_tensor", out=qs[:, :, :], in0=qT[:, :, ts], in1=eb[:, :, :], op=ALU.mult, R=[qTb, ebb], W=[qsb])
            if G.get("dbg_stop", 99) <= 2:
                continue
            for c in range(KD):
                OP(cx, "pe", "matmul", pKf[:, :], hT[:, c, ts], win[:, c, 512:1024], start=(c == 0), stop=(c == KD - 1),
                   R=[winb, hb], W=[pKb])
            OP(cx, "dve", "tensor_tensor", out=kts[:, :, :], in0=pKf.rearrange("p (h d) -> p h d", d=128),
               in1=wk[:, :].unsqueeze(2).to_broadcast([128, 4, 128]), op=ALU.mult, R=[pKb, wkb], W=[ktsb])
            for r in range(2):
                for c in range(KD):
                    OP(cx, "pe", "matmul", pVf[:, :], hT[:, c, ts], win[:, c, 1024 + r * 512:1024 + (r + 1) * 512],
                       start=(c == 0), stop=(c == KD - 1), R=[winb, hb], W=[pVb])
                OP(cx, "act", "copy", out=vp[:, 2 * r:2 * r + 2, 0:256], in_=pVf.rearrange("p (h d) -> p h d", d=256), R=[pVb], W=[vpb])
            for r in range(2):
                for c in range(KD):
                    OP(cx, "pe", "matmul", pVf[:, :], hT[:, c, ts], win[:, c, 2048 + r * 512:2048 + (r + 1) * 512],
                       start=(c == 0), stop=(c == KD - 1), R=[winb, hb], W=[pVb])
                OP(cx, "act", "activation", out=og[:, r * 512:(r + 1) * 512], in_=pVf[:, :], func=AF.Sigmoid, R=[pVb], W=[ogb])
            if G.get("dbg_stop", 99) <= 3:
                continue
            for h in range(4):
                pn, pnb = pN[h % 2]
                pnf = flat(pn)
                OP(cx, "pe", "matmul", pnf[:, 0:257], qs[:, h, :], Cbf[:, h, :], start=True, stop=False, R=[qsb, Cbfb], W=[pnb])
                OP(cx, "pe", "matmul", pnf[:, 0:257], AT[:, h, :], vp[:, h, :], start=False, stop=True, R=[ATb, vpb], W=[pnb])
                OP(cx, "act", "activation", out=r4[:, h:h + 1], in_=pnf[:, 256:257], func=AF.Abs, R=[pnb], W=[r4b])
                OP(cx, "dve", "tensor_scalar", out=r4[:, h:h + 1], in0=r4[:, h:h + 1], scalar1=1.0, scalar2=None, op0=ALU.max,
                   R=[r4b], W=[r4b])
                OP(cx, "dve", "reciprocal", out=r4[:, h:h + 1], in_=r4[:, h:h + 1], R=[r4b], W=[r4b])
                OP(cx, "dve", "tensor_scalar", out=tmp[:, h * 256:(h + 1) * 256], in0=pnf[:, 0:256], scalar1=r4[:, h:h + 1], scalar2=None,
                   op0=ALU.mult, R=[pnb, r4b], W=[tmpb])
                OP(cx, "act", "activation", out=junk[:, :], in_=tmp[:, h * 256:(h + 1) * 256], func=AF.Square, accum_out=msq[:, h:h + 1],
                   R=[tmpb], W=[junkb, msqb])
            OP(cx, "act", "activation", out=msq[:, :], in_=msq[:, :], func=AF.Sqrt, scale=1.0 / 256, bias=eps_t[:, 0:1], R=[msqb, epsb], W=[msqb])
            OP(cx, "dve", "reciprocal", out=msq[:, :], in_=msq[:, :], R=[msqb], W=[msqb])
            for h in range(4):
                OP(cx, "dve", "scalar_tensor_tensor", out=tmp2[:, h * 256:(h + 1) * 256], in0=tmp[:, h * 256:(h + 1) * 256],
                   scalar=msq[:, h:h + 1], in1=nw[:, h * 256:(h + 1) * 256], op0=ALU.mult, op1=ALU.mult, R=[tmpb, msqb, nwb], W=[tmp2b])
            OP(cx, "pool", "tensor_tensor", out=hn[:, :], in0=tmp2[:, :], in1=og[:, :], op=ALU.mult, R=[tmp2b, ogb], W=[hnb])
            if G.get("dbg_stop", 99) <= 4:
                continue
            for h in range(4):
                OP(cx, "pe", "matmul", pAf[:, 0:257], kts[:, h, :], vp[:, h, :], start=True, stop=True, R=[ktsb, vpb], W=[pAb])
                OP(cx, "dve", "scalar_tensor_tensor", out=C32[:, h, :], in0=C32[:, h, :], scalar=eb[:, h, 127:128], in1=pAf[:, 0:257],
                   op0=ALU.mult, op1=ALU.add, R=[C32b, ebb, pAb], W=[C32b])
            OP(cx, "act", "copy", out=Cbf[:, :, :], in_=C32[:, :, :], R=[C32b], W=[Cbfb])
            if G.get("dbg_stop", 99) <= 5:
                continue
            for c in range(KD):
                OP(cx, "pe", "transpose", pAT[:, c, :], hn[:, c * 128:(c + 1) * 128], identb16[:, :], R=[hnb, identb16b], W=[pAb])
            OP(cx, "act", "copy", out=ymT[:, :, ts], in_=pAT[:, :, :], R=[pAb], W=[ymTb])
        tail.run(i, hT, hb, ymT, ymTb)
    sc.close()


def phase_dsa(cx, G, l):
    P = cx.P
    TT = G["TT"]
    S = G["S"]
    w = G["w"]
    n_sel = G["n_sel"]
    KSEL = n_sel // 128
    NIT = 16
    BIG = 1000.0
    sc = Scope(cx)
    wq, wqb = load_w(cx, sc, w["w_in"][l], KD, OFF_AQ, OFF_AQ + 512, "wq")
    wiq, wiqb = load_w(cx, sc, w["w_in"][l], KD, OFF_IQ, OFF_IQ + 256, "wiq")
    wkk = sc.sb([128, KD, 128], BF16, "wkk")
    wik = sc.sb([128, KD, 128], BF16, "wik")
    for dc in (0, 64):
        load_w(cx, sc, w["w_in"][l], KD, OFF_AK, OFF_AK + 64, "", dst=wkk, dcol=dc)
        load_w(cx, sc, w["w_in"][l], KD, OFF_IK, OFF_IK + 64, "", dst=wik, dcol=dc)
    wv, wvb = load_w(cx, sc, w["w_in"][l], KD, OFF_AV, OFF_AV + 64, "wv")
    wiw, wiwb = load_w(cx, sc, w["w_in"][l], KD, OFF_IW, OFF_IW + 4, "wiw")
    pA, pAb = sc.ps([128, 2, TT], F32, "pA")
    pL = [sc.ps([128, 2, TT], F32, "pL") for _ in range(2)]
    pMT, pMTb = sc.ps([128, 8, 128], BF16, "pMT")
    pST = [sc.ps([128, 2, TT], F32, "pST") for _ in range(2)]
    pO = [sc.ps([128, 4, 128], F32, "pO") for _ in range(2)]
    tail = Tail(cx, sc, G, l, 2, w["attn_w_out"][l], 4, pST, pL)
    identb, identbb = make_identity(cx, sc, BF16, "identb")
    ident4, ident4b = sc.sb([128, 4, 128], BF16, "ident4")
    bigi4, bigi4b = sc.sb([128, 4, 128], BF16, "bigi4")
    for h in range(4):
        OP(cx, "pool", "tensor_copy", out=ident4[:, h, :], in_=identb[:, :], R=[identbb], W=[ident4b])
    OP(cx, "pool", "tensor_scalar", out=bigi4[:, :, :], in0=ident4[:, :, :], scalar1=BIG, scalar2=None, op0=ALU.mult, R=[ident4b], W=[bigi4b])
    negb, negbb = sc.sb([128, 1], F32, "negb")
    OP(cx, "pool", "memset", negb[:, :], -BIG, W=[negbb])
    NEGU, NEGUb = sc.sb([128, 128], BF16, "NEGU")
    OP(cx, "pool", "memset", NEGU[:, :], -1000.0, W=[NEGUb])
    OP(cx, "pool", "affine_select", out=NEGU[:, :], in_=NEGU[:, :], pattern=[[1, 128]], compare_op=ALU.is_gt, fill=0.0, base=0,
       channel_multiplier=-1, R=[NEGUb], W=[NEGUb])
    LT, LTb = sc.sb([128, 128], BF16, "LT")
    OP(cx, "pool", "memset", LT[:, :], 1.0, W=[LTb])
    OP(cx, "pool", "affine_select", out=LT[:, :], in_=LT[:, :], pattern=[[-1, 128]], compare_op=ALU.is_ge, fill=0.0, base=0,
       channel_multiplier=1, R=[LTb], W=[LTb])
    hin = [sc.sb([128, KD, TT], BF16, "hin") for _ in range(3)]
    qTs = [sc.sb([128, 4, TT], BF16, "qT") for _ in range(2)]
    qiTs = [sc.sb([128, 2, TT], BF16, "qiT") for _ in range(2)]
    kT2s = [sc.sb([128, S], BF16, "kT2") for _ in range(2)]
    kiT2s = [sc.sb([128, S], BF16, "kiT2") for _ in range(2)]
    vpds = [sc.sb([128, S // 128, 65], BF16, "vpd") for _ in range(2)]
    for vpd, vpdb in vpds:
        OP(cx, "pool", "memset", vpd[:, :, :], 1.0, W=[vpdb])
    wf, wfb = sc.sb([128, 4], F32, "wf")
    dg, dgb = sc.sb([128, 4, 128], BF16, "dg")
    Rl = [sc.sb([128, 512], BF16, "Rl") for _ in range(4)]
    scs, scsb = sc.sb([128, S], F32, "scs")
    jnk, jnkb = sc.sb([128, S], BF16, "jnk")
    Mbs = [sc.sb([128, S], BF16, "Mb") for _ in range(2)]
    lo, lob = sc.sb([128, 1], F32, "lo")
    w0, w0b = sc.sb([128, 1], F32, "w0")
    mid, midb = sc.sb([128, 1], F32, "mid")
    cnt, cntb = sc.sb([128, 1], F32, "cnt")
    stp, stpb = sc.sb([128, 1], F32, "stp")
    PT = [sc.sb([128, 512], BF16, "PT") for _ in range(4)]
    rden, rdenb = sc.sb([128, 2, 4], F32, "rden")
    ha, hab = sc.sb([128, 4, 2, 64], BF16, "ha")
    yaTs = [sc.sb([128, 4, TT], BF16, "yaT") for _ in range(2)]
    pAf = flat(pA)
    tiles_per_seq = S // TT
    NT = G["NT"]

    def load_h(i):
        hT, hb = hin[i % 3]
        OP(cx, "sp", "dma_start", out=hT[:, :, :], in_=ht_ap(G, i), R=[G["HTb"][i]], W=[hb], chan="ld")

    def proj(i):
        hT, hb = hin[i % 3]
        qT, qTb = qTs[i % 2]
        qiT, qiTb = qiTs[i % 2]
        sq_ = (i // tiles_per_seq) % 2
        kT2, kT2b = kT2s[sq_]
        kiT2, kiT2b = kiT2s[sq_]
        ti = i % tiles_per_seq
        cs = slice(ti * TT, (ti + 1) * TT)
        for hp in range(2):
            for hh in range(2):
                c4 = hp * 2 + hh
                for c in range(KD):
                    OP(cx, "pe", "matmul", pA[:, hh, :], wq[:, c, c4 * 128:(c4 + 1) * 128], hT[:, c, :], start=(c == 0), stop=(c == KD - 1),
                       R=[wqb, hb], W=[pAb])
            OP(cx, "act", "mul", qT[:, 2 * hp:2 * hp + 2, :], pA[:, :, :], 0.125, R=[pAb], W=[qTb])
        for hh, (wt, wtb) in enumerate((wkk, wik)):
            for c in range(KD):
                OP(cx, "pe", "matmul", pA[:, hh, :], wt[:, c, :], hT[:, c, :], start=(c == 0), stop=(c == KD - 1), R=[wtb, hb], W=[pAb])
        OP(cx, "act", "copy", out=kT2[:, cs], in_=pA[:, 0, :], R=[pAb], W=[kT2b])
        OP(cx, "act", "copy", out=kiT2[:, cs], in_=pA[:, 1, :], R=[pAb], W=[kiT2b])
        for hh in range(2):
            for c in range(KD):
                OP(cx, "pe", "matmul", pA[:, hh, :], wiq[:, c, hh * 128:(hh + 1) * 128], hT[:, c, :], start=(c == 0), stop=(c == KD - 1),
                   R=[wiqb, hb], W=[pAb])
        OP(cx, "act", "copy", out=qiT[:, :, :], in_=pA[:, :, :], R=[pAb], W=[qiTb])

    def prep(i, a):
        hT, hb = hin[i % 3]
        qiT, qiTb = qiTs[i % 2]
        sq_ = (i // tiles_per_seq) % 2
        kiT2, kiT2b = kiT2s[sq_]
        vpd, vpdb = vpds[sq_]
        ti = i % tiles_per_seq
        ts = slice(a * 128, (a + 1) * 128)
        qi = ti * 2 + a
        SV = (qi + 1) * 128
        Mb, Mbb = Mbs[qi % 2]
        for c in range(KD):
            OP(cx, "pe", "matmul", pAf[:, 0:64], hT[:, c, ts], wv[:, c, :], start=(c == 0), stop=(c == KD - 1), R=[wvb, hb], W=[pAb])
        OP(cx, "act", "copy", out=vpd[:, qi, 0:64], in_=pAf[:, 0:64], R=[pAb], W=[vpdb])
        if qi < KSEL:
            if qi > 0:
                OP(cx, "pool", "memset", Mb[:, 0:qi * 128], 1.0, W=[Mbb])
            OP(cx, "pool", "tensor_copy", out=Mb[:, qi * 128:SV], in_=LT[:, :], R=[LTb], W=[Mbb])
            return
        for c in range(KD):
            OP(cx, "pe", "matmul", pAf[:, 64:68], hT[:, c, ts], wiw[:, c, :], start=(c == 0), stop=(c == KD - 1), R=[wiwb, hb], W=[pAb])
        OP(cx, "act", "mul", wf[:, :], pAf[:, 64:68], 1.0 / 16.0, R=[pAb], W=[wfb])
        OP(cx, "dve", "tensor_tensor", out=dg[:, :, :], in0=ident4[:, :, :], in1=wf[:, :].unsqueeze(2).to_broadcast([128, 4, 128]),
           op=ALU.mult, R=[ident4b, wfb], W=[dgb])
        for c0 in range(0, SV, 512):
            cw = min(512, SV - c0)
            for h in range(4):
                half, ch = h % 2, h // 2
                ps_ = slice(half * 64, (half + 1) * 64)
                pl, plb = pL[h % 2]
                OP(cx, "pe", "matmul", flat(pl)[:, 0:cw], qiT[ps_, ch, ts], kiT2[ps_, c0:c0 + cw], start=True, stop=True,
                   R=[qiTb, kiT2b], W=[plb])
                OP(cx, "act", "activation", out=Rl[h][0][:, 0:cw], in_=flat(pl)[:, 0:cw], func=AF.Relu, R=[plb], W=[Rl[h][1]])
            dcol = qi * 128 - c0
            has_diag = 0 <= dcol < cw
            for h in range(4):
                OP(cx, "pe", "matmul", pAf[:, 0:cw], dg[:, h, :], Rl[h][0][:, 0:cw], start=(h == 0), stop=(h == 3 and not has_diag),
                   R=[dgb, Rl[h][1]], W=[pAb])
            if has_diag:
                OP(cx, "pe", "matmul", pAf[:, dcol:dcol + 128], identb[:, :], NEGU[:, :], start=False, stop=True,
                   R=[identbb, NEGUb], W=[pAb])
            OP(cx, "act", "copy", out=scs[:, c0:c0 + cw], in_=pAf[:, 0:cw], R=[pAb], W=[scsb])
        OP(cx, "dve", "tensor_reduce", out=lo[:, :], in_=scs[:, 0:qi * 128], axis=AX.X, op=ALU.min, R=[scsb], W=[lob])
        OP(cx, "dve", "tensor_reduce", out=w0[:, :], in_=scs[:, 0:SV], axis=AX.X, op=ALU.max, R=[scsb], W=[w0b])
        OP(cx, "dve", "tensor_tensor", out=w0[:, :], in0=w0[:, :], in1=lo[:, :], op=ALU.subtract, R=[w0b, lob], W=[w0b])
        for it in range(1, NIT + 1):
            f = 2.0 ** -it
            OP(cx, "dve", "scalar_tensor_tensor", out=mid[:, :], in0=w0[:, :], scalar=f, in1=lo[:, :], op0=ALU.mult, op1=ALU.add,
               R=[w0b, lob], W=[midb])
            OP(cx, "dve", "tensor_scalar", out=jnk[:, 0:SV], in0=scs[:, 0:SV], scalar1=mid[:, 0:1], scalar2=0.0, op0=ALU.is_ge,
               op1=ALU.add, accum_out=cnt[:, 0:1], R=[scsb, midb], W=[jnkb, cntb])
            OP(cx, "dve", "tensor_scalar", out=stp[:, :], in0=cnt[:, :], scalar1=float(n_sel) - 0.5, scalar2=f, op0=ALU.is_ge,
               op1=ALU.mult, R=[cntb], W=[stpb])
            OP(cx, "dve", "scalar_tensor_tensor", out=lo[:, :], in0=stp[:, :], scalar=w0[:, 0:1], in1=lo[:, :], op0=ALU.mult,
               op1=ALU.add, R=[stpb, w0b, lob], W=[lob])
        OP(cx, "dve", "tensor_scalar", out=Mb[:, 0:SV], in0=scs[:, 0:SV], scalar1=lo[:, 0:1], scalar2=None, op0=ALU.is_ge,
           R=[scsb, lob], W=[Mbb])

    def attn(i, a):
        qT, qTb = qTs[i % 2]
        sq_ = (i // tiles_per_seq) % 2
        kT2, kT2b = kT2s[sq_]
        vpd, vpdb = vpds[sq_]
        yaT, yaTb = yaTs[i % 2]
        ti = i % tiles_per_seq
        ts = slice(a * 128, (a + 1) * 128)
        qi = ti * 2 + a
        nk = qi + 1
        Mb, Mbb = Mbs[qi % 2]
        def S_(kt):
            for half in range(2):
                ps_ = slice(half * 64, (half + 1) * 64)
                pst, pstb = pST[half]
                OP(cx, "pe", "matmul", flat(pst)[:, :], kT2[ps_, kt * 128:(kt + 1) * 128], qT[ps_, :, ts], start=True, stop=False,
                   R=[kT2b, qTb], W=[pstb])
                OP(cx, "pe", "matmul", flat(pst)[:, :], Mb[:, kt * 128:(kt + 1) * 128], bigi4[:, :, :], start=False, stop=True,
                   R=[Mbb, bigi4b], W=[pstb])

        def E_(kt):
            for half in range(2):
                pst, pstb = pST[half]
                pt_, ptb_ = PT[(kt % 2) * 2 + half]
                OP(cx, "act", "activation", out=pt_[:, :], in_=flat(pst)[:, :], func=AF.Exp, bias=negb[:, 0:1], R=[pstb, negbb], W=[ptb_])

        def V_(kt):
            for half in range(2):
                pt_, ptb_ = PT[(kt % 2) * 2 + half]
                po, pob = pO[half]
                for c in range(4):
                    OP(cx, "pe", "matmul", po[:, c, 0:65], pt_[:, c * 128:(c + 1) * 128], vpd[:, kt, :], start=(kt == 0 and c == 0),
                       stop=(kt == qi), skip_group_check=True, R=[ptb_, vpdb], W=[pob])

        S_(0)
        for kt in range(nk):
            E_(kt)
            if kt + 1 < nk:
                S_(kt + 1)
            V_(kt)
        for half in range(2):
            po, pob = pO[half]
            OP(cx, "dve", "reciprocal", out=rden[:, half, :], in_=po[:, :, 64], R=[pob], W=[rdenb])
            OP(cx, "dve", "tensor_tensor", out=ha[:, :, half, :], in0=po[:, :, 0:64],
               in1=rden[:, half, :].unsqueeze(2).to_broadcast([128, 4, 64]), op=ALU.mult, R=[pob, rdenb], W=[hab])
        haf = ha[:, :, :, :].rearrange("p c h d -> p (c h d)")
        for c in range(4):
            OP(cx, "pe", "transpose", pMT[:, 4 + c, :], haf[:, c * 128:(c + 1) * 128], identb[:, :], R=[hab, identbb], W=[pMTb])
        OP(cx, "act", "copy", out=yaT[:, :, ts], in_=pMT[:, 4:8, :], R=[pMTb], W=[yaTb])

    steps = [(i, a) for i in range(NT) for a in range(TT // 128)]
    load_h(0)
    if NT > 1:
        load_h(1)
    tail.load_x(0)
    proj(0)
    prep(0, 0)
    for k, (i, a) in enumerate(steps):
        if k + 1 < len(steps):
            i2, a2 = steps[k + 1]
            if a2 == 0:
                if i2 + 1 < NT:
                    load_h(i2 + 1)
                tail.load_x(i2)
                proj(i2)
            prep(i2, a2)
        attn(i, a)
        if a == TT // 128 - 1:
            hT, hb = hin[i % 3]
            tail.run(i, hT, hb, yaTs[i % 2][0], yaTs[i % 2][1])
    sc.close()


WNAMES = ["norm_ffn1", "ffn1_w_gu", "ffn1_w_down", "norm_mix", "w_in", "conv_w", "conv_w_out", "mlstm_b_i", "mlstm_b_f",
          "mlstm_norm", "mlstm_w_out", "attn_w_out", "w_o", "norm_ffn2", "ffn2_w_gu", "ffn2_w_down", "norm_ple",
          "ple_w_gate", "ple_w_proj", "final_norm"]


def build(S, nseq, depth, wshapes, phases=None, n_sel=256, dbg=None):
    nc = bass.Bass("TRN2", target_bir_lowering=False)
    NTC = S * nseq
    TT = 256
    G = {"S": S, "nseq": nseq, "NTC": NTC, "TT": TT, "NT": NTC // TT, "depth": depth, "n_sel": n_sel}
    G.update(dbg or {})
    G["x"] = nc.dram_tensor("x", [NTC, D], F32, kind="ExternalInput").ap()
    G["p"] = nc.dram_tensor("p", [depth, NTC, D_PLE], F32, kind="ExternalInput").ap()
    G["w"] = {k: nc.dram_tensor(k, list(wshapes[k]), F32, kind="ExternalInput").ap() for k in WNAMES}
    G["out"] = nc.dram_tensor("out", [NTC, D], F32, kind="ExternalOutput").ap()
    G["outb"] = Buf("out")
    if G.get("dbg_dump"):
        G["dbgo"] = nc.dram_tensor("dbgo", [128, 4096], F32, kind="ExternalOutput").ap()
    G["XT"] = nc.dram_tensor("XT", [D, NTC], F32).ap()
    G["XTb"] = [Buf("XT%d" % i) for i in range(G["NT"])]
    G["HT"] = nc.dram_tensor("HT", [D, NTC], BF16).ap()
    G["HTb"] = [Buf("HT%d" % i) for i in range(G["NT"])]
    with contextlib.ExitStack() as es:
        cx = Ctx(nc, es)
        phase_in(cx, G)
        for l in range(depth):
            if phases is None or "ffn1" in phases:
                phase_ffn(cx, G, l, 1)
            if phases is None or "conv" in phases or "mlstm" in phases or "dsa" in phases:
                phase_norm(cx, G, l)
            if phases is None or "conv" in phases:
                phase_conv(cx, G, l)
            if phases is None or "mlstm" in phases:
                phase_mlstm(cx, G, l)
            if phases is None or "dsa" in phases:
                phase_dsa(cx, G, l)
            if phases is None or "ffn2" in phases:
                phase_ffn(cx, G, l, 2)
            if phases is None or "ple" in phases:
                phase_ple(cx, G, l)
        phase_out(cx, G)
        cx.P.add("sp", lambda e: e.nop(), R=[G["outb"]], chan=None) if False else None
        cx.P.barrier()
        cx.P.emit()
    return nc


def kernel(**inputs):
    x = np.ascontiguousarray(inputs["x"], dtype=np.float32)
    p = np.ascontiguousarray(inputs["p"], dtype=np.float32)
    B, S, _ = x.shape
    depth = p.shape[0]
    nseq = B // N_CORES
    wshapes = {k: inputs[k].shape for k in WNAMES}
    nc = build(S, nseq, depth, wshapes)
    in_maps = []
    for c in range(N_CORES):
        m = {k: np.ascontiguousarray(inputs[k], dtype=np.float32) for k in WNAMES}
        m["x"] = x[c * nseq:(c + 1) * nseq].reshape(nseq * S, D)
        m["p"] = np.ascontiguousarray(p[:, c * nseq:(c + 1) * nseq].reshape(depth, nseq * S, D_PLE))
        in_maps.append(m)
    res = run_bass_kernel_spmd(nc, in_maps, core_ids=list(range(N_CORES)))
    out = np.concatenate([r["out"].reshape(nseq, S, D) for r in res.results], axis=0)
    return out.astype(np.float32)
```

```python
import contextlib
import numpy as np
import concourse.bass as bass
import concourse.mybir as mybir
from concourse.bass_utils import run_bass_kernel_spmd

F32 = mybir.dt.float32
BF16 = mybir.dt.bfloat16
AF = mybir.ActivationFunctionType
ALU = mybir.AluOpType
AX = mybir.AxisListType

D = 1024
KD = 8
FF = 2816
KF = 22
D_PLE = 256
N_IN = 8652
EPS = 1e-6
N_CORES = 8


class Buf:
    __slots__ = ("w", "r", "name", "excl")

    def __init__(self, name="", excl=False):
        self.w = None
        self.r = {}
        self.name = name
        self.excl = excl


class Stream:
    def __init__(self, name, sem, inc):
        self.name, self.sem, self.inc = name, sem, inc
        self.ops = []


class Op:
    __slots__ = ("fn", "waits", "sig", "stream", "sigcount")

    def __init__(self, fn, stream):
        self.fn, self.stream = fn, stream
        self.waits = {}
        self.sig = False
        self.sigcount = 0


class Prog:
    ENGS = ("pe", "act", "dve", "pool", "sp")

    def __init__(self, nc, es):
        self.nc = nc
        self.es = es
        self.q = {e: [] for e in self.ENGS}
        self.seen = {e: {} for e in self.ENGS}
        self.streams = {}
        self.chan_n = {}
        for e in ("pe", "act", "dve", "pool"):
            self.new_stream(e, 1)

    def new_stream(self, name, inc=16):
        sem = self.es.enter_context(self.nc.semaphore("s_" + name))
        self.streams[name] = Stream(name, sem, inc)
        return self.streams[name]

    NSLOT = 8

    def new_channel(self, name):
        self.chan_n[name] = 0
        for k in range(self.NSLOT):
            self.new_stream("%s%d" % (name, k), 16)

    def add(self, eng, fn, R=(), W=(), chan=None):
        if chan is not None:
            k = self.chan_n[chan]
            self.chan_n[chan] = k + 1
            st = self.streams["%s%d" % (chan, k % self.NSLOT)]
        else:
            st = self.streams[eng]
        op = Op(fn, st)
        if st.inc == 16:
            op.sig = True
        st.ops.append(op)
        idx = len(st.ops)
        seen = self.seen[eng]
        waits = op.waits

        def need(dep):
            if dep is None:
                return
            s, j = dep
            if s is st and eng == "pe":
                return
            if seen.get(s, 0) >= j:
                return
            if waits.get(s, 0) < j:
                waits[s] = j

        if st.inc == 16 and idx > 1:
            need((st, idx - 1))
        for b in R:
            need(b.w)
            if b.excl:
                for s, j in b.r.items():
                    if s is not st:
                        need((s, j))
        for b in W:
            need(b.w)
            for s, j in b.r.items():
                if s is st and st.inc == 1:
                    continue
                need((s, j))
        for s, j in waits.items():
            seen[s] = j
            s.ops[j - 1].sig = True
        for b in R:
            if b.r.get(st, 0) < idx:
                b.r[st] = idx
        for b in W:
            b.w = (st, idx)
            b.r = {}
        self.q[eng].append(op)
        return op

    def barrier(self):
        tails = [(s, len(s.ops)) for s in self.streams.values() if s.ops]
        for e in self.ENGS:
            op = Op(None, None)
            seen = self.seen[e]
            for s, j in tails:
                if seen.get(s, 0) >= j:
                    continue
                op.waits[s] = j
                seen[s] = j
                s.ops[j - 1].sig = True
            self.q[e].append(op)

    def emit(self):
        for st in self.streams.values():
            c = 0
            for op in st.ops:
                if op.sig:
                    c += 1
                op.sigcount = c
        nc = self.nc
        q = self.q

        def run(e, ops):
            for op in ops:
                for s, j in op.waits.items():
                    e.wait_ge(s.sem, s.ops[j - 1].sigcount * s.inc)
                if op.fn is None:
                    continue
                ins = op.fn(e)
                if op.sig:
                    ins.then_inc(op.stream.sem, op.stream.inc)

        for st in self.streams.values():
            nc.gpsimd.sem_clear(st.sem)
        with nc.Block() as block:
            @block.tensor
            def _(e):
                run(e, q["pe"])

            @block.scalar
            def _(e):
                run(e, q["act"])

            @block.vector
            def _(e):
                run(e, q["dve"])

            @block.gpsimd
            def _(e):
                run(e, q["pool"])

            @block.sync
            def _(e):
                run(e, q["sp"])
        for st in self.streams.values():
            nc.gpsimd.sem_clear(st.sem)


class Ctx:
    def __init__(self, nc, es):
        self.nc, self.es = nc, es
        self.P = Prog(nc, es)
        self.P.new_channel("ld")
        self.P.new_channel("st")
        self.P.new_channel("wl")
        self.P.new_channel("pl")
        self._n = 0

    def sb(self, shape, dt, name=None):
        self._n += 1
        return self.es.enter_context(self.nc.sbuf_tensor(f"{name or 't'}_{self._n}", list(shape), dt))

    def ps(self, shape, dt=F32, name=None):
        self._n += 1
        return self.es.enter_context(self.nc.psum_tensor(f"{name or 'p'}_{self._n}", list(shape), dt))


class Scope:
    def __init__(self, cx):
        self.cx = cx
        self.es = contextlib.ExitStack()
        self.bufs = []

    def sb(self, shape, dt, name="t"):
        cx = self.cx
        cx._n += 1
        t = self.es.enter_context(cx.nc.sbuf_tensor(f"{name}_{cx._n}", list(shape), dt))
        b = Buf(name)
        self.bufs.append(b)
        return t, b

    def ps(self, shape, dt=F32, name="p"):
        cx = self.cx
        cx._n += 1
        t = self.es.enter_context(cx.nc.psum_tensor(f"{name}_{cx._n}", list(shape), dt))
        b = Buf(name, excl=True)
        self.bufs.append(b)
        return t, b

    def close(self):
        self.cx.P.barrier()
        self.es.close()


def load_w(cx, sc, w2d, kc, n0, n1, name, dst=None, dcol=0):
    P = cx.P
    n = n1 - n0
    if dst is None:
        t, b = sc.sb([128, kc, n], BF16, name)
    else:
        t, b = dst
    for c in range(kc):
        P.add("pool", lambda e, c=c: e.dma_start(out=t[:, c, dcol:dcol + n], in_=w2d[c * 128:(c + 1) * 128, n0:n1]),
              W=[b], chan="pl")
    return t, b


def load_vec_cols(cx, sc, v1d, kc, name):
    t, b = sc.sb([128, kc], F32, name)
    cx.P.add("sp", lambda e: e.dma_start(out=t[:, :], in_=v1d.rearrange("(c p) -> p c", p=128),
                                         allow_slow_non_contiguous=True), W=[b], chan="wl")
    return t, b


def rmsnorm_T(cx, sc, st, xt, xb, g, gb, TT):
    P = cx.P
    sq, sqb = st["sq"]
    hT, hb = st["hT"]
    ssp, sspb = st["ssp"]
    rs, rsb = st["rs"]
    ones, onesb = st["ones"]
    P.add("act", lambda e: e.activation(out=sq[:, :, :], in_=xt[:, :, :], func=AF.Square), R=[xb], W=[sqb])
    for c in range(KD):
        P.add("pe", lambda e, c=c: e.matmul(ssp[:, 0:TT], ones[:, :], sq[:, c, :], start=(c == 0), stop=(c == KD - 1)),
              R=[sqb, onesb], W=[sspb])
    P.add("act", lambda e: e.activation(out=rs[:, 0:TT], in_=ssp[:, 0:TT], func=AF.Sqrt, scale=1.0 / D, bias=st["eps"][0][:, 0:1]),
          R=[sspb, st["eps"][1]], W=[rsb])
    P.add("dve", lambda e: e.reciprocal(out=rs[:, 0:TT], in_=rs[:, 0:TT]), R=[rsb], W=[rsb])
    for c in range(KD):
        P.add("dve", lambda e, c=c: e.scalar_tensor_tensor(out=hT[:, c, :], in0=xt[:, c, :], scalar=g[:, c:c + 1],
                                                           in1=rs[:, 0:TT], op0=ALU.mult, op1=ALU.mult),
              R=[xb, gb, rsb], W=[hb])
    return hT, hb


def norm_scratch(cx, sc, TT):
    st = {}
    st["sq"] = sc.sb([128, KD, TT], BF16, "sq")
    st["hT"] = sc.sb([128, KD, TT], BF16, "hT")
    st["ssp"] = sc.ps([128, 512], F32, "ssp")
    st["rs"] = sc.sb([128, TT], F32, "rs")
    st["ones"] = sc.sb([128, 128], BF16, "ones")
    st["eps"] = sc.sb([128, 1], F32, "eps")
    ones, onesb = st["ones"]
    eps, epsb = st["eps"]
    cx.P.add("pool", lambda e: e.memset(ones[:, :], 1.0), W=[onesb])
    cx.P.add("pool", lambda e: e.memset(eps[:, :], EPS), W=[epsb])
    return st


def make_identity(cx, sc, dt, name="ident"):
    t, b = sc.sb([128, 128], dt, name)
    cx.P.add("pool", lambda e: e.memset(t[:, :], 1.0), W=[b])
    cx.P.add("pool", lambda e: e.affine_select(out=t[:, :], in_=t[:, :], pattern=[[-1, 128]], compare_op=ALU.is_equal,
                                               fill=0.0, base=0, channel_multiplier=1), R=[b], W=[b])
    return t, b


def phase_in(cx, G):
    P = cx.P
    TT = G["TT"]
    sc = Scope(cx)
    ident, identb = make_identity(cx, sc, F32)
    xin = [sc.sb([128, TT // 128, D], F32, "xin") for _ in range(2)]
    xo = [sc.sb([128, KD, TT], F32, "xo") for _ in range(2)]
    pp = [sc.ps([128, 2, TT], F32, "tp") for _ in range(4)]
    x = G["x"]
    for i in range(G["NT"]):
        xi, xib = xin[i % 2]
        xo_t, xob = xo[i % 2]
        P.add("sp", lambda e, i=i, xi=xi: e.dma_start(
            out=xi[:, :, :], in_=x[i * TT:(i + 1) * TT, :].rearrange("(a p) d -> p a d", p=128)), W=[xib], chan="ld")
        for c2 in range(KD // 2):
            pt, ptb = pp[c2 % 4]
            for cc in range(2):
                c = c2 * 2 + cc
                for a in range(TT // 128):
                    P.add("pe", lambda e, c=c, cc=cc, a=a, pt=pt, xi=xi: e.transpose(
                        pt[:, cc, a * 128:(a + 1) * 128], xi[:, a, c * 128:(c + 1) * 128], ident[:, :]),
                        R=[xib, identb], W=[ptb])
            eng = "act" if c2 % 2 == 0 else "dve"
            if eng == "act":
                P.add("act", lambda e, c2=c2, pt=pt, xo_t=xo_t: e.copy(out=xo_t[:, 2 * c2:2 * c2 + 2, :], in_=pt[:, :, :]),
                      R=[ptb], W=[xob])
            else:
                P.add("dve", lambda e, c2=c2, pt=pt, xo_t=xo_t: e.tensor_copy(out=xo_t[:, 2 * c2:2 * c2 + 2, :], in_=pt[:, :, :]),
                      R=[ptb], W=[xob])
        P.add("sp", lambda e, i=i, xo_t=xo_t: e.dma_start(
            out=G["XT"][:, i * TT:(i + 1) * TT].rearrange("(c p) t -> p c t", p=128), in_=xo_t[:, :, :]),
            R=[xob], W=[G["XTb"][i]], chan="st")
    sc.close()


def phase_out(cx, G):
    P = cx.P
    TT = G["TT"]
    sc = Scope(cx)
    ident, identb = make_identity(cx, sc, F32)
    st = norm_scratch(cx, sc, TT)
    g, gb = load_vec_cols(cx, sc, G["w"]["final_norm"], KD, "gfin")
    xin = [sc.sb([128, KD, TT], F32, "xin") for _ in range(2)]
    yn = sc.sb([128, KD, TT], F32, "yn")
    yo = [sc.sb([128, D], F32, "yo") for _ in range(2)]
    pp = [sc.ps([128, 512], F32, "tp") for _ in range(4)]
    rs, rsb = st["rs"]
    sq, sqb = st["sq"]
    ssp, sspb = st["ssp"]
    ones, onesb = st["ones"]
    k = 0
    for i in range(G["NT"]):
        xt, xb = xin[i % 2]
        P.add("sp", lambda e, i=i, xt=xt: e.dma_start(
            out=xt[:, :, :], in_=G["XT"][:, i * TT:(i + 1) * TT].rearrange("(c p) t -> p c t", p=128)),
            R=[G["XTb"][i]], W=[xb], chan="ld")
        P.add("act", lambda e, xt=xt: e.activation(out=sq[:, :, :], in_=xt[:, :, :], func=AF.Square), R=[xb], W=[sqb])
        for c in range(KD):
            P.add("pe", lambda e, c=c: e.matmul(ssp[:, 0:TT], ones[:, :], sq[:, c, :], start=(c == 0), stop=(c == KD - 1)),
                  R=[sqb, onesb], W=[sspb])
        P.add("act", lambda e: e.activation(out=rs[:, 0:TT], in_=ssp[:, 0:TT], func=AF.Sqrt, scale=1.0 / D, bias=st["eps"][0][:, 0:1]),
              R=[sspb, st["eps"][1]], W=[rsb])
        P.add("dve", lambda e: e.reciprocal(out=rs[:, 0:TT], in_=rs[:, 0:TT]), R=[rsb], W=[rsb])
        y, yb = yn
        for c in range(KD):
            P.add("dve", lambda e, c=c, xt=xt: e.scalar_tensor_tensor(out=y[:, c, :], in0=xt[:, c, :], scalar=g[:, c:c + 1],
                                                                      in1=rs[:, 0:TT], op0=ALU.mult, op1=ALU.mult),
                  R=[xb, gb, rsb], W=[yb])
        for a in range(TT // 128):
            yt, ytb = yo[k % 2]
            for hh in range(2):
                pt, ptb = pp[(2 * k + hh) % 4]
                for c4 in range(4):
                    c = hh * 4 + c4
                    P.add("pe", lambda e, c=c, c4=c4, a=a, pt=pt: e.transpose(
                        pt[:, c4 * 128:(c4 + 1) * 128], y[:, c, a * 128:(a + 1) * 128], ident[:, :]),
                        R=[yb, identb], W=[ptb])
                if hh == 0:
                    P.add("act", lambda e, pt=pt, yt=yt: e.copy(out=yt[:, 0:512], in_=pt[:, :]), R=[ptb], W=[ytb])
                else:
                    P.add("dve", lambda e, pt=pt, yt=yt: e.tensor_copy(out=yt[:, 512:1024], in_=pt[:, :]), R=[ptb], W=[ytb])
            r0 = i * TT + a * 128
            P.add("sp", lambda e, r0=r0, yt=yt: e.dma_start(out=G["out"][r0:r0 + 128, :], in_=yt[:, :]),
                  R=[ytb], W=[G["outb"]], chan="st")
            k += 1
    sc.close()


def phase_ffn(cx, G, l, which):
    P = cx.P
    TT = G["TT"]
    w = G["w"]
    sc = Scope(cx)
    pre = "ffn1" if which == 1 else "ffn2"
    wgu, wgub = load_w(cx, sc, w[pre + "_w_gu"][l], KD, 0, 2 * FF, "wgu")
    wd, wdb = load_w(cx, sc, w[pre + "_w_down"][l], KF, 0, D, "wd")
    g, gb = load_vec_cols(cx, sc, w["norm_" + pre][l], KD, "gn")
    st = norm_scratch(cx, sc, TT)
    xin = [sc.sb([128, KD, TT], F32, "xin") for _ in range(2)]
    m, mb = sc.sb([128, KF, TT], BF16, "m")
    av = [sc.sb([128, TT], F32, "a") for _ in range(2)]
    pgu = [sc.ps([128, 2, TT], F32, "pgu") for _ in range(3)]
    pdn = [sc.ps([128, 2, TT], F32, "pdn") for _ in range(2)]

    def load(i):
        xt, xb = xin[i % 2]
        P.add("sp", lambda e: e.dma_start(
            out=xt[:, :, :], in_=G["XT"][:, i * TT:(i + 1) * TT].rearrange("(c p) t -> p c t", p=128)),
            R=[G["XTb"][i]], W=[xb], chan="ld")

    hTs = [st["hT"], sc.sb([128, KD, TT], BF16, "hT2")]

    def norm(i):
        st["hT"] = hTs[i % 2]
        return rmsnorm_T(cx, sc, st, xin[i % 2][0], xin[i % 2][1], g, gb, TT)

    load(0)
    nxt = norm(0)
    for i in range(G["NT"]):
        xt, xb = xin[i % 2]
        if i + 1 < G["NT"]:
            load(i + 1)
        hT, hb = nxt
        for j in range(KF):
            pg, pgb = pgu[j % 3]
            for half in range(2):
                col = half * FF + j * 128
                for c in range(KD):
                    P.add("pe", lambda e, c=c, col=col, half=half, pg=pg, hT=hT: e.matmul(
                        pg[:, half, :], wgu[:, c, col:col + 128], hT[:, c, :], start=(c == 0), stop=(c == KD - 1)),
                        R=[wgub, hb], W=[pgb])
            a, ab = av[j % 2]
            P.add("act", lambda e, pg=pg, a=a: e.activation(out=a[:, :], in_=pg[:, 0, :], func=AF.Silu), R=[pgb], W=[ab])
            P.add("dve", lambda e, pg=pg, a=a, j=j: e.tensor_tensor(out=m[:, j, :], in0=a[:, :], in1=pg[:, 1, :], op=ALU.mult),
                  R=[pgb, ab], W=[mb])
        if i + 1 < G["NT"]:
            nxt = norm(i + 1)
        for o2 in range(KD // 2):
            pd, pdb = pdn[o2 % 2]
            for oo in range(2):
                oc = o2 * 2 + oo
                for j in range(KF):
                    P.add("pe", lambda e, j=j, oc=oc, oo=oo, pd=pd: e.matmul(
                        pd[:, oo, :], wd[:, j, oc * 128:(oc + 1) * 128], m[:, j, :], start=(j == 0), stop=(j == KF - 1)),
                        R=[wdb, mb], W=[pdb])
            P.add("dve", lambda e, o2=o2, pd=pd, xt=xt: e.scalar_tensor_tensor(
                out=xt[:, 2 * o2:2 * o2 + 2, :], in0=pd[:, :, :], scalar=0.5, in1=xt[:, 2 * o2:2 * o2 + 2, :],
                op0=ALU.mult, op1=ALU.add), R=[pdb, xb], W=[xb])
        P.add("sp", lambda e, i=i, xt=xt: e.dma_start(
            out=G["XT"][:, i * TT:(i + 1) * TT].rearrange("(c p) t -> p c t", p=128), in_=xt[:, :, :]),
            R=[xb], W=[G["XTb"][i]], chan="st")
    sc.close()


OFF_CB, OFF_CC, OFF_CX = 0, 512, 1024
OFF_MQ, OFF_MK, OFF_MV, OFF_MO, OFF_MI, OFF_MF = 1536, 2048, 2560, 3584, 4608, 4612
OFF_AQ, OFF_AK, OFF_AV, OFF_IQ, OFF_IK, OFF_IW = 4616, 5128, 5192, 5256, 5512, 5576
OFF_G = 5580


def xt_ap(G, i):
    TT = G["TT"]
    return G["XT"][:, i * TT:(i + 1) * TT].rearrange("(c p) t -> p c t", p=128)


def ht_ap(G, i):
    TT = G["TT"]
    return G["HT"][:, i * TT:(i + 1) * TT].rearrange("(c p) t -> p c t", p=128)


def phase_norm(cx, G, l):
    P = cx.P
    TT = G["TT"]
    sc = Scope(cx)
    g, gb = load_vec_cols(cx, sc, G["w"]["norm_mix"][l], KD, "gn")
    st = norm_scratch(cx, sc, TT)
    xin = [sc.sb([128, KD, TT], F32, "xin") for _ in range(2)]
    ho = [sc.sb([128, KD, TT], BF16, "ho") for _ in range(2)]
    for i in range(G["NT"]):
        xt, xb = xin[i % 2]
        P.add("sp", lambda e, i=i, xt=xt: e.dma_start(out=xt[:, :, :], in_=xt_ap(G, i)), R=[G["XTb"][i]], W=[xb], chan="ld")
        st["hT"] = ho[i % 2]
        hT, hb = rmsnorm_T(cx, sc, st, xt, xb, g, gb, TT)
        P.add("sp", lambda e, i=i, hT=hT: e.dma_start(out=ht_ap(G, i), in_=hT[:, :, :]), R=[hb], W=[G["HTb"][i]], chan="st")
    sc.close()


class Tail:
    def __init__(self, cx, sc, G, l, br, wout2d, kin, psA, psB):
        w = G["w"]
        TT = G["TT"]
        self.cx, self.G, self.kin = cx, G, kin
        self.wg = load_w(cx, sc, w["w_in"][l], KD, OFF_G + br * D, OFF_G + (br + 1) * D, "wgate")
        self.wout = load_w(cx, sc, wout2d, kin, 0, D, "wout")
        self.wo = load_w(cx, sc, w["w_o"][l], KD, 0, D, "wo")
        self.sg = [sc.sb([128, TT], F32, "sg") for _ in range(2)]
        self.mg = sc.sb([128, KD, TT], BF16, "mg")
        self.xin = [sc.sb([128, KD, TT], F32, "xres") for _ in range(2)]
        self.psA, self.psB = psA, psB

    def load_x(self, i):
        G = self.G
        xt, xb = self.xin[i % 2]
        self.cx.P.add("sp", lambda e: e.dma_start(out=xt[:, :, :], in_=xt_ap(G, i)), R=[G["XTb"][i]], W=[xb], chan="ld")

    def run(self, i, hT, hb, yT, yb):
        P = self.cx.P
        G = self.G
        wg, wgb = self.wg
        wout, woutb = self.wout
        wo, wob = self.wo
        mg, mgb = self.mg
        xt, xb = self.xin[i % 2]
        kin = self.kin
        for oc in range(KD):
            pa, pab = self.psA[oc % len(self.psA)]
            for c in range(kin):
                P.add("pe", lambda e, c=c, oc=oc, pa=pa: e.matmul(pa[:, 0, :], wout[:, c, oc * 128:(oc + 1) * 128], yT[:, c, :],
                                                               start=(c == 0), stop=(c == kin - 1)), R=[woutb, yb], W=[pab])
            for c in range(KD):
                P.add("pe", lambda e, c=c, oc=oc, pa=pa: e.matmul(pa[:, 1, :], wg[:, c, oc * 128:(oc + 1) * 128], hT[:, c, :],
                                                               start=(c == 0), stop=(c == KD - 1)), R=[wgb, hb], W=[pab])
            sg, sgb = self.sg[oc % 2]
            P.add("act", lambda e, pa=pa, sg=sg: e.activation(out=sg[:, :], in_=pa[:, 1, :], func=AF.Sigmoid), R=[pab], W=[sgb])
            P.add("dve", lambda e, pa=pa, sg=sg, oc=oc: e.tensor_tensor(out=mg[:, oc, :], in0=sg[:, :], in1=pa[:, 0, :], op=ALU.mult),
                  R=[pab, sgb], W=[mgb])
        for o2 in range(KD // 2):
            po, pob = self.psB[o2 % len(self.psB)]
            for oo in range(2):
                oc = o2 * 2 + oo
                for c in range(KD):
                    P.add("pe", lambda e, c=c, oc=oc, oo=oo, po=po: e.matmul(po[:, oo, :], wo[:, c, oc * 128:(oc + 1) * 128], mg[:, c, :],
                                                                         start=(c == 0), stop=(c == KD - 1)), R=[wob, mgb], W=[pob])
            P.add("dve", lambda e, o2=o2, po=po: e.tensor_tensor(out=xt[:, 2 * o2:2 * o2 + 2, :], in0=xt[:, 2 * o2:2 * o2 + 2, :],
                                                              in1=po[:, :, :], op=ALU.add), R=[pob, xb], W=[xb])
        P.add("sp", lambda e: e.dma_start(out=xt_ap(G, i), in_=xt[:, :, :]), R=[xb], W=[G["XTb"][i]], chan="st")


def phase_conv(cx, G, l):
    P = cx.P
    TT = G["TT"]
    w = G["w"]
    sc = Scope(cx)
    win, winb = load_w(cx, sc, w["w_in"][l], KD, 0, 1536, "winc")
    psA = [sc.ps([128, 2, TT], F32, "psA") for _ in range(2)]
    psB = [sc.ps([128, 2, TT], F32, "psB") for _ in range(2)]
    pcx = [sc.ps([128, 2, TT], F32, "pcx") for _ in range(2)]
    pb_ = [sc.ps([128, 2, TT], F32, "pbb") for _ in range(2)]
    tail = Tail(cx, sc, G, l, 0, w["conv_w_out"][l], 4, psA, psB)
    cw, cwb = sc.sb([128, 4, 3], F32, "cw")
    for j in range(3):
        P.add("sp", lambda e, j=j: e.dma_start(out=cw[:, :, j], in_=w["conv_w"][l][j].rearrange("(c p) -> p c", p=128),
                                               allow_slow_non_contiguous=True), W=[cwb], chan="wl")
    hin = [sc.sb([128, KD, TT], BF16, "hin") for _ in range(2)]
    u = [sc.sb([128, TT + 2], F32, "u") for _ in range(4)]
    ccs = [sc.sb([128, TT], F32, "ccs") for _ in range(2)]
    yv = [sc.sb([128, TT], F32, "yv") for _ in range(2)]
    z, zb = sc.sb([128, 4, TT], BF16, "z")
    tiles_per_seq = G["S"] // TT

    def load(i):
        hT, hb = hin[i % 2]
        P.add("sp", lambda e: e.dma_start(out=hT[:, :, :], in_=ht_ap(G, i)), R=[G["HTb"][i]], W=[hb], chan="ld")
        tail.load_x(i)

    load(0)
    for i in range(G["NT"]):
        hT, hb = hin[i % 2]
        if i + 1 < G["NT"]:
            load(i + 1)
        for q in range(4):
            ut, ub = u[q]
            if i % tiles_per_seq == 0:
                P.add("pool", lambda e, ut=ut: e.memset(ut[:, 0:2], 0.0), W=[ub])
            p1, p1b = pcx[q % 2]
            p2, p2b = pb_[q % 2]
            for k_, (pt, ptb, slot, off) in enumerate(((p1, p1b, 0, OFF_CC), (p1, p1b, 1, OFF_CX), (p2, p2b, 0, OFF_CB))):
                for c in range(KD):
                    P.add("pe", lambda e, c=c, pt=pt, slot=slot, col=off + q * 128, hT=hT: e.matmul(
                        pt[:, slot, :], win[:, c, col:col + 128], hT[:, c, :], start=(c == 0), stop=(c == KD - 1)),
                        R=[winb, hb], W=[ptb])
            cs, csb = ccs[q % 2]
            P.add("act", lambda e, p1=p1, cs=cs: e.copy(out=cs[:, :], in_=p1[:, 0, :]), R=[p1b], W=[csb])
            P.add("dve", lambda e, p1=p1, cs=cs, ut=ut: e.tensor_tensor(out=ut[:, 2:TT + 2], in0=cs[:, :], in1=p1[:, 1, :], op=ALU.mult),
                  R=[p1b, csb], W=[ub])
            y, yb_ = yv[q % 2]
            P.add("dve", lambda e, y=y, ut=ut, q=q: e.tensor_scalar(out=y[:, :], in0=ut[:, 0:TT], scalar1=cw[:, q, 0:1], scalar2=None,
                                                                  op0=ALU.mult), R=[ub, cwb], W=[yb_])
            P.add("dve", lambda e, y=y, ut=ut, q=q: e.scalar_tensor_tensor(out=y[:, :], in0=ut[:, 1:TT + 1], scalar=cw[:, q, 1:2],
                                                                         in1=y[:, :], op0=ALU.mult, op1=ALU.add), R=[ub, cwb, yb_], W=[yb_])
            P.add("dve", lambda e, y=y, ut=ut, q=q: e.scalar_tensor_tensor(out=y[:, :], in0=ut[:, 2:TT + 2], scalar=cw[:, q, 2:3],
                                                                         in1=y[:, :], op0=ALU.mult, op1=ALU.add), R=[ub, cwb, yb_], W=[yb_])
            P.add("pool", lambda e, ut=ut: e.tensor_copy(out=ut[:, 0:2], in_=ut[:, TT:TT + 2]), R=[ub], W=[ub])
            P.add("dve", lambda e, y=y, p2=p2, q=q: e.tensor_tensor(out=z[:, q, :], in0=y[:, :], in1=p2[:, 0, :], op=ALU.mult),
                  R=[yb_, p2b], W=[zb])
        tail.run(i, hT, hb, z, zb)
    sc.close()


def phase_ple(cx, G, l):
    P = cx.P
    TT = G["TT"]
    w = G["w"]
    sc = Scope(cx)
    wg, wgb = load_w(cx, sc, w["ple_w_gate"][l], KD, 0, D, "wpg")
    wp, wpb = load_w(cx, sc, w["ple_w_proj"][l], 2, 0, D, "wpp")
    g, gb = load_vec_cols(cx, sc, w["norm_ple"][l], KD, "gn")
    st = norm_scratch(cx, sc, TT)
    ident, identb = make_identity(cx, sc, F32)
    xin = [sc.sb([128, KD, TT], F32, "xin") for _ in range(2)]
    pin = [sc.sb([128, TT // 128, D_PLE], F32, "pin") for _ in range(2)]
    pT, pTb = sc.sb([128, 2, TT], BF16, "pT")
    sgs = [sc.sb([128, TT], F32, "sg") for _ in range(2)]
    ptp = sc.ps([128, 2, TT], F32, "ptp")
    psA = [sc.ps([128, 2, TT], F32, "psA") for _ in range(3)]

    def load(i):
        xt, xb = xin[i % 2]
        P.add("sp", lambda e: e.dma_start(out=xt[:, :, :], in_=xt_ap(G, i)), R=[G["XTb"][i]], W=[xb], chan="ld")
        pi, pib = pin[i % 2]
        P.add("sp", lambda e: e.dma_start(out=pi[:, :, :], in_=G["p"][l, i * TT:(i + 1) * TT, :].rearrange("(a p) d -> p a d", p=128)),
              W=[pib], chan="ld")

    hTs = [st["hT"], sc.sb([128, KD, TT], BF16, "hT2")]

    def norm(i):
        st["hT"] = hTs[i % 2]
        return rmsnorm_T(cx, sc, st, xin[i % 2][0], xin[i % 2][1], g, gb, TT)

    load(0)
    nxt = norm(0)
    for i in range(G["NT"]):
        xt, xb = xin[i % 2]
        pi, pib = pin[i % 2]
        if i + 1 < G["NT"]:
            load(i + 1)
        hT, hb = nxt
        pt, ptb = ptp
        for kc in range(2):
            for a in range(TT // 128):
                P.add("pe", lambda e, kc=kc, a=a, pi=pi: e.transpose(pt[:, kc, a * 128:(a + 1) * 128], pi[:, a, kc * 128:(kc + 1) * 128], ident[:, :]),
                      R=[pib, identb], W=[ptb])
        P.add("act", lambda e: e.copy(out=pT[:, :, :], in_=pt[:, :, :]), R=[ptb], W=[pTb])
        for oc in range(KD):
            if oc == 4 and i + 1 < G["NT"]:
                nxt = norm(i + 1)
            pa, pab = psA[oc % 3]
            for c in range(KD):
                P.add("pe", lambda e, c=c, oc=oc, pa=pa, hT=hT: e.matmul(pa[:, 0, :], wg[:, c, oc * 128:(oc + 1) * 128], hT[:, c, :],
                                                               start=(c == 0), stop=(c == KD - 1)), R=[wgb, hb], W=[pab])
            for c in range(2):
                P.add("pe", lambda e, c=c, oc=oc, pa=pa: e.matmul(pa[:, 1, :], wp[:, c, oc * 128:(oc + 1) * 128], pT[:, c, :],
                                                               start=(c == 0), stop=(c == 1)), R=[wpb, pTb], W=[pab])
            sg, sgb = sgs[oc % 2]
            P.add("act", lambda e, pa=pa, sg=sg: e.activation(out=sg[:, :], in_=pa[:, 0, :], func=AF.Sigmoid), R=[pab], W=[sgb])
            if G.get("dbg_dump") and i == 0 and oc == 0:
                P.add("sp", lambda e, sg=sg: e.dma_start(out=G["dbgo"][:, 0:TT], in_=sg[:, :]), R=[sgb], W=[G["outb"]], chan="st")
                d2, d2b = sc.sb([128, TT], F32, "d2")
                P.add("act", lambda e, pa=pa: e.copy(out=d2[:, :], in_=pa[:, 1, :]), R=[pab], W=[d2b])
                P.add("sp", lambda e: e.dma_start(out=G["dbgo"][:, TT:2 * TT], in_=d2[:, :]), R=[d2b], W=[G["outb"]], chan="st")
                d3, d3b = sc.sb([128, 2, TT], F32, "d3")
                P.add("act", lambda e: e.copy(out=d3[:, :, :], in_=pT[:, :, :]), R=[pTb], W=[d3b])
                P.add("sp", lambda e: e.dma_start(out=G["dbgo"][:, 2 * TT:4 * TT], in_=d3[:, :, :].rearrange("p a t -> p (a t)")), R=[d3b], W=[G["outb"]], chan="st")
            P.add("dve", lambda e, pa=pa, sg=sg: e.tensor_tensor(out=sg[:, :], in0=sg[:, :], in1=pa[:, 1, :], op=ALU.mult),
                  R=[pab, sgb], W=[sgb])
            if not G.get("dbg_skip_add"):
                P.add("pool", lambda e, sg=sg, oc=oc, xt=xt: e.tensor_tensor(out=xt[:, oc, :], in0=xt[:, oc, :], in1=sg[:, :], op=ALU.add),
                      R=[sgb, xb], W=[xb])
        P.add("sp", lambda e, i=i, xt=xt: e.dma_start(out=xt_ap(G, i), in_=xt[:, :, :]), R=[xb], W=[G["XTb"][i]], chan="st")
    sc.close()


import math


def OP(cx, eng, method, *args, R=(), W=(), chan=None, **kw):
    return cx.P.add(eng, lambda e: getattr(e, method)(*args, **kw), R=R, W=W, chan=chan)


def flat(t):
    return t[:, :, :].rearrange("p a t -> p (a t)")


def make_tri(cx, sc, dt, val, name):
    t, b = sc.sb([128, 128], dt, name)
    OP(cx, "pool", "memset", t[:, :], val, W=[b])
    OP(cx, "pool", "affine_select", out=t[:, :], in_=t[:, :], pattern=[[1, 128]], compare_op=ALU.is_ge, fill=0.0, base=0,
       channel_multiplier=-1, R=[b], W=[b])
    return t, b


def phase_mlstm(cx, G, l):
    P = cx.P
    TT = G["TT"]
    w = G["w"]
    sc = Scope(cx)
    win, winb = load_w(cx, sc, w["w_in"][l], KD, OFF_MQ, OFF_AQ, "winm")
    pA, pAb = sc.ps([128, 2, TT], F32, "pA")
    pK, pKb = sc.ps([128, 2, TT], F32, "pK")
    pV, pVb = sc.ps([128, 2, TT], F32, "pV")
    pG, pGb = sc.ps([128, 2, TT], F32, "pG")
    pB, pBb = sc.ps([128, 4, 128], F32, "pB")
    pS, pSb = sc.ps([128, 4, 128], F32, "pS")
    pN = [sc.ps([128, 2, TT], F32, "pN") for _ in range(2)]
    tail = Tail(cx, sc, G, l, 1, w["mlstm_w_out"][l], 8, [(pK, pKb), (pV, pVb)], pN)
    identb16, identb16b = make_identity(cx, sc, BF16, "identb")
    NU, NUb = make_tri(cx, sc, BF16, -1.0, "NU")
    U, Ub = make_tri(cx, sc, F32, 1.0, "U")
    ones3, ones3b = sc.sb([128, 4, 128], BF16, "ones3")
    l4h, l4hb = sc.sb([128, 2, 4], BF16, "l4h")
    lbh, lbhb = sc.sb([128, 2, 4, 128], BF16, "lbh")
    OP(cx, "pool", "memset", ones3[:, :, :], 1.0, W=[ones3b])
    nw, nwb = sc.sb([128, D], F32, "nw")
    OP(cx, "sp", "dma_start", out=nw[:, :], in_=w["mlstm_norm"][l].partition_broadcast(128), W=[nwb], chan="wl")
    bi_t, bib = sc.sb([128, 4], F32, "bi")
    bf_t, bfb = sc.sb([128, 4], F32, "bf")
    OP(cx, "sp", "dma_start", out=bi_t[:, :], in_=w["mlstm_b_i"][l].partition_broadcast(128), W=[bib], chan="wl")
    OP(cx, "sp", "dma_start", out=bf_t[:, :], in_=w["mlstm_b_f"][l].partition_broadcast(128), W=[bfb], chan="wl")
    eps_t, epsb = sc.sb([128, 1], F32, "eps")
    OP(cx, "pool", "memset", eps_t[:, :], EPS, W=[epsb])
    hin = [sc.sb([128, KD, TT], BF16, "hin") for _ in range(2)]
    qT, qTb = sc.sb([128, 4, TT], BF16, "qT")
    kT, kTb = sc.sb([128, 4, TT], BF16, "kT")
    vp, vpb = sc.sb([128, 4, 257], BF16, "vp")
    OP(cx, "pool", "memset", vp[:, :, :], 1.0, W=[vpb])
    og, ogb = sc.sb([128, D], F32, "og")
    gi, gib = sc.sb([128, 4], F32, "gi")
    l4, l4b = sc.sb([128, 4], F32, "l4")
    colb, colbb = sc.sb([128, 4], F32, "colb")
    bl, blb = sc.sb([128, 4], F32, "bl")
    wk, wkb = sc.sb([128, 4], F32, "wk")
    r4, r4b = sc.sb([128, 4], F32, "r4")
    msq, msqb = sc.sb([128, 4], F32, "msq")
    lb, lbb = sc.sb([128, 4, 128], F32, "lb")
    eb, ebb = sc.sb([128, 4, 128], F32, "eb")
    DT, DTb = sc.sb([128, 4, 128], F32, "DT")
    AT, ATb = sc.sb([128, 4, 128], BF16, "AT")
    qs, qsb = sc.sb([128, 4, 128], BF16, "qs")
    kts, ktsb = sc.sb([128, 4, 128], BF16, "kts")
    C32, C32b = sc.sb([128, 4, 257], F32, "C32")
    Cbf, Cbfb = sc.sb([128, 4, 257], BF16, "Cbf")
    tmp, tmpb = sc.sb([128, D], F32, "tmp")
    tmp2, tmp2b = sc.sb([128, D], F32, "tmp2")
    junk, junkb = sc.sb([128, 256], F32, "junk")
    hn, hnb = sc.sb([128, D], BF16, "hn")
    ymT, ymTb = sc.sb([128, KD, TT], BF16, "ymT")
    pAf, pKf, pVf, pGf = flat(pA), flat(pK), flat(pV), flat(pG)
    pAT = pAf.bitcast(BF16).rearrange("p (c t) -> p c t", t=128)
    tiles_per_seq = G["S"] // TT
    kscale = 128 ** -0.5

    def load(i):
        hT, hb = hin[i % 2]
        OP(cx, "sp", "dma_start", out=hT[:, :, :], in_=ht_ap(G, i), R=[G["HTb"][i]], W=[hb], chan="ld")
        tail.load_x(i)

    load(0)
    for i in range(G["NT"]):
        hT, hb = hin[i % 2]
        if i + 1 < G["NT"]:
            load(i + 1)
        if i % tiles_per_seq == 0:
            OP(cx, "pool", "memset", C32[:, :, :], 0.0, W=[C32b])
            OP(cx, "pool", "memset", Cbf[:, :, :], 0.0, W=[Cbfb])
        for which, dst, dstb, off in ((0, qT, qTb, 0), (1, kT, kTb, 512)):
            for hp in range(2):
                for hh in range(2):
                    h = hp * 2 + hh
                    for c in range(KD):
                        OP(cx, "pe", "matmul", pA[:, hh, :], win[:, c, off + h * 128:off + (h + 1) * 128], hT[:, c, :],
                           start=(c == 0), stop=(c == KD - 1), R=[winb, hb], W=[pAb])
                if which == 0:
                    OP(cx, "act", "copy", out=dst[:, 2 * hp:2 * hp + 2, :], in_=pA[:, :, :], R=[pAb], W=[dstb])
                else:
                    OP(cx, "act", "mul", dst[:, 2 * hp:2 * hp + 2, :], pA[:, :, :], kscale, R=[pAb], W=[dstb])
        for a in range(TT // 128):
            ts = slice(a * 128, (a + 1) * 128)
            if G.get("dbg_stop", 99) <= 0:
                continue
            for c in range(KD):
                OP(cx, "pe", "matmul", pGf[:, 0:8], hT[:, c, ts], win[:, c, 3072:3080], start=(c == 0), stop=(c == KD - 1),
                   R=[winb, hb], W=[pGb])
            if G.get("dbg_stop", 99) == 0.5:
                continue
            OP(cx, "dve", "tensor_tensor", out=gi[:, :], in0=pGf[:, 0:4], in1=bi_t[:, :], op=ALU.add, R=[pGb, bib], W=[gib])
            OP(cx, "dve", "tensor_tensor", out=l4[:, :], in0=pGf[:, 4:8], in1=bf_t[:, :], op=ALU.add, R=[pGb, bfb], W=[l4b])
            OP(cx, "act", "activation", out=l4[:, :], in_=l4[:, :], func=AF.Exp, scale=-1.0, R=[l4b], W=[l4b])
            OP(cx, "act", "activation", out=l4[:, :], in_=l4[:, :], func=AF.Ln, bias=1.0, R=[l4b], W=[l4b])
            if G.get("dbg_stop", 99) == 0.7:
                continue
            OP(cx, "dve", "tensor_copy", out=l4h[:, 0, :], in_=l4[:, :], R=[l4b], W=[l4hb])
            OP(cx, "dve", "tensor_tensor", out=l4h[:, 1, :], in0=l4[:, :], in1=l4h[:, 0, :], op=ALU.subtract, R=[l4b, l4hb], W=[l4hb])
            for z in range(2):
                OP(cx, "dve", "tensor_tensor", out=lbh[:, z, :, :], in0=ones3[:, :, :],
                   in1=l4h[:, z, :].unsqueeze(2).to_broadcast([128, 4, 128]), op=ALU.mult, R=[ones3b, l4hb], W=[lbhb])
            if G.get("dbg_stop", 99) == 0.8:
                continue
            for z in range(2):
                OP(cx, "pe", "matmul", pGf[:, 8:12], NU[:, :], l4h[:, z, :], start=(z == 0), stop=(z == 1), R=[NUb, l4hb], W=[pGb])
            for h in range(4):
                for z in range(2):
                    OP(cx, "pe", "matmul", pB[:, h, :], lbh[:, z, h, :], NU[:, :], start=(z == 0), stop=(z == 1), R=[lbhb, NUb], W=[pBb])
            if G.get("dbg_stop", 99) == 0.9:
                continue
            OP(cx, "act", "activation", out=eb[:, :, :], in_=pB[:, :, :], func=AF.Exp, R=[pBb], W=[ebb])
            OP(cx, "dve", "tensor_tensor", out=colb[:, :], in0=gi[:, :], in1=pGf[:, 8:12], op=ALU.subtract, R=[gib, pGb], W=[colbb])
            OP(cx, "dve", "tensor_copy", out=bl[:, :], in_=pB[:, :, 127], R=[pBb], W=[blb])
            OP(cx, "dve", "tensor_tensor", out=wk[:, :], in0=colb[:, :], in1=bl[:, :], op=ALU.add, R=[colbb, blb], W=[wkb])
            OP(cx, "act", "activation", out=wk[:, :], in_=wk[:, :], func=AF.Exp, R=[wkb], W=[wkb])
            OP(cx, "dve", "tensor_scalar", out=wk[:, :], in0=wk[:, :], scalar1=kscale, scalar2=None, op0=ALU.mult, R=[wkb], W=[wkb])
            if G.get("dbg_stop", 99) <= 1:
                continue
            for h in range(4):
                OP(cx, "pe", "matmul", pS[:, h, :], kT[:, h, ts], qT[:, h, ts], start=True, stop=True, R=[kTb, qTb], W=[pSb])
            for h in range(4):
                OP(cx, "act", "activation", out=DT[:, h, :], in_=pB[:, h, :], func=AF.Exp, bias=colb[:, h:h + 1], R=[pBb, colbb], W=[DTb])
            OP(cx, "pool", "tensor_tensor", out=DT[:, :, :], in0=DT[:, :, :], in1=U[:, :].unsqueeze(1).to_broadcast([128, 4, 128]),
               op=ALU.mult, R=[DTb, Ub], W=[DTb])
            OP(cx, "dve", "tensor_tensor", out=AT[:, :, :], in0=DT[:, :, :], in1=pS[:, :, :], op=ALU.mult, R=[DTb, pSb], W=[ATb])
            OP(cx, "pool", "tensor_tensor", out=qs[:, :, :], in0=qT[:, :, ts], in1=eb[:, :, :], op=ALU.mult, R=[qTb, ebb], W=[qsb])
            if G.get("dbg_stop", 99) <= 2:
                continue
            for c in range(KD):
                OP(cx, "pe", "matmul", pKf[:, :], hT[:, c, ts], win[:, c, 512:1024], start=(c == 0), stop=(c == KD - 1),
                   R=[winb, hb], W=[pKb])
            OP(cx, "dve", "tensor_tensor", out=kts[:, :, :], in0=pKf.rearrange("p (h d) -> p h d", d=128),
               in1=wk[:, :].unsqueeze(2).to_broadcast([128, 4, 128]), op=ALU.mult, R=[pKb, wkb], W=[ktsb])
            for r in range(2):
                for c in range(KD):
                    OP(cx, "pe", "matmul", pVf[:, :], hT[:, c, ts], win[:, c, 1024 + r * 512:1024 + (r + 1) * 512],
                       start=(c == 0), stop=(c == KD - 1), R=[winb, hb], W=[pVb])
                OP(cx, "act", "copy", out=vp[:, 2 * r:2 * r + 2, 0:256], in_=pVf.rearrange("p (h d) -> p h d", d=256), R=[pVb], W=[vpb])
            for r in range(2):
                for c in range(KD):
                    OP(cx, "pe", "matmul", pVf[:, :], hT[:, c, ts], win[:, c, 2048 + r * 512:2048 + (r + 1) * 512],
                       start=(c == 0), stop=(c == KD - 1), R=[winb, hb], W=[pVb])
                OP(cx, "act", "activation", out=og[:, r * 512:(r + 1) * 512], in_=pVf[:, :], func=AF.Sigmoid, R=[pVb], W=[ogb])
            if G.get("dbg_stop", 99) <= 3:
                continue
            for h in range(4):
                pn, pnb = pN[h % 2]
                pnf = flat(pn)
                OP(cx, "pe", "matmul", pnf[:, 0:257], qs[:, h, :], Cbf[:, h, :], start=True, stop=False, R=[qsb, Cbfb], W=[pnb])
                OP(cx, "pe", "matmul", pnf[:, 0:257], AT[:, h, :], vp[:, h, :], start=False, stop=True, R=[ATb, vpb], W=[pnb])
                OP(cx, "act", "activation", out=r4[:, h:h + 1], in_=pnf[:, 256:257], func=AF.Abs, R=[pnb], W=[r4b])
                OP(cx, "dve", "tensor_scalar", out=r4[:, h:h + 1], in0=r4[:, h:h + 1], scalar1=1.0, scalar2=None, op0=ALU.max,
                   R=[r4b], W=[r4b])
                OP(cx, "dve", "reciprocal", out=r4[:, h:h + 1], in_=r4[:, h:h + 1], R=[r4b], W=[r4b])
                OP(cx, "dve", "tensor_scalar", out=tmp[:, h * 256:(h + 1) * 256], in0=pnf[:, 0:256], scalar1=r4[:, h:h + 1], scalar2=None,
                   op0=ALU.mult, R=[pnb, r4b], W=[tmpb])
                OP(cx, "act", "activation", out=junk[:, :], in_=tmp[:, h * 256:(h + 1) * 256], func=AF.Square, accum_out=msq[:, h:h + 1],
                   R=[tmpb], W=[junkb, msqb])
            OP(cx, "act", "activation", out=msq[:, :], in_=msq[:, :], func=AF.Sqrt, scale=1.0 / 256, bias=eps_t[:, 0:1], R=[msqb, epsb], W=[msqb])
            OP(cx, "dve", "reciprocal", out=msq[:, :], in_=msq[:, :], R=[msqb], W=[msqb])
            for h in range(4):
                OP(cx, "dve", "scalar_tensor_tensor", out=tmp2[:, h * 256:(h + 1) * 256], in0=tmp[:, h * 256:(h + 1) * 256],
                   scalar=msq[:, h:h + 1], in1=nw[:, h * 256:(h + 1) * 256], op0=ALU.mult, op1=ALU.mult, R=[tmpb, msqb, nwb], W=[tmp2b])
            OP(cx, "pool", "tensor_tensor", out=hn[:, :], in0=tmp2[:, :], in1=og[:, :], op=ALU.mult, R=[tmp2b, ogb], W=[hnb])
            if G.get("dbg_stop", 99) <= 4:
                continue
            for h in range(4):
                OP(cx, "pe", "matmul", pAf[:, 0:257], kts[:, h, :], vp[:, h, :], start=True, stop=True, R=[ktsb, vpb], W=[pAb])
                OP(cx, "dve", "scalar_tensor_tensor", out=C32[:, h, :], in0=C32[:, h, :], scalar=eb[:, h, 127:128], in1=pAf[:, 0:257],
                   op0=ALU.mult, op1=ALU.add, R=[C32b, ebb, pAb], W=[C32b])
            OP(cx, "act", "copy", out=Cbf[:, :, :], in_=C32[:, :, :], R=[C32b], W=[Cbfb])
            if G.get("dbg_stop", 99) <= 5:
                continue
            for c in range(KD):
                OP(cx, "pe", "transpose", pAT[:, c, :], hn[:, c * 128:(c + 1) * 128], identb16[:, :], R=[hnb, identb16b], W=[pAb])
            OP(cx, "act", "copy", out=ymT[:, :, ts], in_=pAT[:, :, :], R=[pAb], W=[ymTb])
        tail.run(i, hT, hb, ymT, ymTb)
    sc.close()


def phase_dsa(cx, G, l):
    P = cx.P
    TT = G["TT"]
    S = G["S"]
    w = G["w"]
    n_sel = G["n_sel"]
    KSEL = n_sel // 128
    NIT = 16
    BIG = 1000.0
    sc = Scope(cx)
    wq, wqb = load_w(cx, sc, w["w_in"][l], KD, OFF_AQ, OFF_AQ + 512, "wq")
    wiq, wiqb = load_w(cx, sc, w["w_in"][l], KD, OFF_IQ, OFF_IQ + 256, "wiq")
    wkk = sc.sb([128, KD, 128], BF16, "wkk")
    wik = sc.sb([128, KD, 128], BF16, "wik")
    for dc in (0, 64):
        load_w(cx, sc, w["w_in"][l], KD, OFF_AK, OFF_AK + 64, "", dst=wkk, dcol=dc)
        load_w(cx, sc, w["w_in"][l], KD, OFF_IK, OFF_IK + 64, "", dst=wik, dcol=dc)
    wv, wvb = load_w(cx, sc, w["w_in"][l], KD, OFF_AV, OFF_AV + 64, "wv")
    wiw, wiwb = load_w(cx, sc, w["w_in"][l], KD, OFF_IW, OFF_IW + 4, "wiw")
    pA, pAb = sc.ps([128, 2, TT], F32, "pA")
    pL = [sc.ps([128, 2, TT], F32, "pL") for _ in range(2)]
    pMT, pMTb = sc.ps([128, 8, 128], BF16, "pMT")
    pST = [sc.ps([128, 2, TT], F32, "pST") for _ in range(2)]
    pO = [sc.ps([128, 4, 128], F32, "pO") for _ in range(2)]
    tail = Tail(cx, sc, G, l, 2, w["attn_w_out"][l], 4, pST, pL)
    identb, identbb = make_identity(cx, sc, BF16, "identb")
    ident4, ident4b = sc.sb([128, 4, 128], BF16, "ident4")
    bigi4, bigi4b = sc.sb([128, 4, 128], BF16, "bigi4")
    for h in range(4):
        OP(cx, "pool", "tensor_copy", out=ident4[:, h, :], in_=identb[:, :], R=[identbb], W=[ident4b])
    OP(cx, "pool", "tensor_scalar", out=bigi4[:, :, :], in0=ident4[:, :, :], scalar1=BIG, scalar2=None, op0=ALU.mult, R=[ident4b], W=[bigi4b])
    negb, negbb = sc.sb([128, 1], F32, "negb")
    OP(cx, "pool", "memset", negb[:, :], -BIG, W=[negbb])
    NEGU, NEGUb = sc.sb([128, 128], BF16, "NEGU")
    OP(cx, "pool", "memset", NEGU[:, :], -1000.0, W=[NEGUb])
    OP(cx, "pool", "affine_select", out=NEGU[:, :], in_=NEGU[:, :], pattern=[[1, 128]], compare_op=ALU.is_gt, fill=0.0, base=0,
       channel_multiplier=-1, R=[NEGUb], W=[NEGUb])
    LT, LTb = sc.sb([128, 128], BF16, "LT")
    OP(cx, "pool", "memset", LT[:, :], 1.0, W=[LTb])
    OP(cx, "pool", "affine_select", out=LT[:, :], in_=LT[:, :], pattern=[[-1, 128]], compare_op=ALU.is_ge, fill=0.0, base=0,
       channel_multiplier=1, R=[LTb], W=[LTb])
    hin = [sc.sb([128, KD, TT], BF16, "hin") for _ in range(3)]
    qTs = [sc.sb([128, 4, TT], BF16, "qT") for _ in range(2)]
    qiTs = [sc.sb([128, 2, TT], BF16, "qiT") for _ in range(2)]
    kT2s = [sc.sb([128, S], BF16, "kT2") for _ in range(2)]
    kiT2s = [sc.sb([128, S], BF16, "kiT2") for _ in range(2)]
    vpds = [sc.sb([128, S // 128, 65], BF16, "vpd") for _ in range(2)]
    for vpd, vpdb in vpds:
        OP(cx, "pool", "memset", vpd[:, :, :], 1.0, W=[vpdb])
    wf, wfb = sc.sb([128, 4], F32, "wf")
    dg, dgb = sc.sb([128, 4, 128], BF16, "dg")
    Rl = [sc.sb([128, 512], BF16, "Rl") for _ in range(4)]
    scs, scsb = sc.sb([128, S], F32, "scs")
    jnk, jnkb = sc.sb([128, S], BF16, "jnk")
    Mbs = [sc.sb([128, S], BF16, "Mb") for _ in range(2)]
    lo, lob = sc.sb([128, 1], F32, "lo")
    w0, w0b = sc.sb([128, 1], F32, "w0")
    mid, midb = sc.sb([128, 1], F32, "mid")
    cnt, cntb = sc.sb([128, 1], F32, "cnt")
    stp, stpb = sc.sb([128, 1], F32, "stp")
    PT = [sc.sb([128, 512], BF16, "PT") for _ in range(4)]
    rden, rdenb = sc.sb([128, 2, 4], F32, "rden")
    ha, hab = sc.sb([128, 4, 2, 64], BF16, "ha")
    yaTs = [sc.sb([128, 4, TT], BF16, "yaT") for _ in range(2)]
    pAf = flat(pA)
    tiles_per_seq = S // TT
    NT = G["NT"]

    def load_h(i):
        hT, hb = hin[i % 3]
        OP(cx, "sp", "dma_start", out=hT[:, :, :], in_=ht_ap(G, i), R=[G["HTb"][i]], W=[hb], chan="ld")

    def proj(i):
        hT, hb = hin[i % 3]
        qT, qTb = qTs[i % 2]
        qiT, qiTb = qiTs[i % 2]
        sq_ = (i // tiles_per_seq) % 2
        kT2, kT2b = kT2s[sq_]
        kiT2, kiT2b = kiT2s[sq_]
        ti = i % tiles_per_seq
        cs = slice(ti * TT, (ti + 1) * TT)
        for hp in range(2):
            for hh in range(2):
                c4 = hp * 2 + hh
                for c in range(KD):
                    OP(cx, "pe", "matmul", pA[:, hh, :], wq[:, c, c4 * 128:(c4 + 1) * 128], hT[:, c, :], start=(c == 0), stop=(c == KD - 1),
                       R=[wqb, hb], W=[pAb])
            OP(cx, "act", "mul", qT[:, 2 * hp:2 * hp + 2, :], pA[:, :, :], 0.125, R=[pAb], W=[qTb])
        for hh, (wt, wtb) in enumerate((wkk, wik)):
            for c in range(KD):
                OP(cx, "pe", "matmul", pA[:, hh, :], wt[:, c, :], hT[:, c, :], start=(c == 0), stop=(c == KD - 1), R=[wtb, hb], W=[pAb])
        OP(cx, "act", "copy", out=kT2[:, cs], in_=pA[:, 0, :], R=[pAb], W=[kT2b])
        OP(cx, "act", "copy", out=kiT2[:, cs], in_=pA[:, 1, :], R=[pAb], W=[kiT2b])
        for hh in range(2):
            for c in range(KD):
                OP(cx, "pe", "matmul", pA[:, hh, :], wiq[:, c, hh * 128:(hh + 1) * 128], hT[:, c, :], start=(c == 0), stop=(c == KD - 1),
                   R=[wiqb, hb], W=[pAb])
        OP(cx, "act", "copy", out=qiT[:, :, :], in_=pA[:, :, :], R=[pAb], W=[qiTb])

    def prep(i, a):
        hT, hb = hin[i % 3]
        qiT, qiTb = qiTs[i % 2]
        sq_ = (i // tiles_per_seq) % 2
        kiT2, kiT2b = kiT2s[sq_]
        vpd, vpdb = vpds[sq_]
        ti = i % tiles_per_seq
        ts = slice(a * 128, (a + 1) * 128)
        qi = ti * 2 + a
        SV = (qi + 1) * 128
        Mb, Mbb = Mbs[qi % 2]
        for c in range(KD):
            OP(cx, "pe", "matmul", pAf[:, 0:64], hT[:, c, ts], wv[:, c, :], start=(c == 0), stop=(c == KD - 1), R=[wvb, hb], W=[pAb])
        OP(cx, "act", "copy", out=vpd[:, qi, 0:64], in_=pAf[:, 0:64], R=[pAb], W=[vpdb])
        if qi < KSEL:
            if qi > 0:
                OP(cx, "pool", "memset", Mb[:, 0:qi * 128], 1.0, W=[Mbb])
            OP(cx, "pool", "tensor_copy", out=Mb[:, qi * 128:SV], in_=LT[:, :], R=[LTb], W=[Mbb])
            return
        for c in range(KD):
            OP(cx, "pe", "matmul", pAf[:, 64:68], hT[:, c, ts], wiw[:, c, :], start=(c == 0), stop=(c == KD - 1), R=[wiwb, hb], W=[pAb])
        OP(cx, "act", "mul", wf[:, :], pAf[:, 64:68], 1.0 / 16.0, R=[pAb], W=[wfb])
        OP(cx, "dve", "tensor_tensor", out=dg[:, :, :], in0=ident4[:, :, :], in1=wf[:, :].unsqueeze(2).to_broadcast([128, 4, 128]),
           op=ALU.mult, R=[ident4b, wfb], W=[dgb])
        for c0 in range(0, SV, 512):
            cw = min(512, SV - c0)
            for h in range(4):
                half, ch = h % 2, h // 2
                ps_ = slice(half * 64, (half + 1) * 64)
                pl, plb = pL[h % 2]
                OP(cx, "pe", "matmul", flat(pl)[:, 0:cw], qiT[ps_, ch, ts], kiT2[ps_, c0:c0 + cw], start=True, stop=True,
                   R=[qiTb, kiT2b], W=[plb])
                OP(cx, "act", "activation", out=Rl[h][0][:, 0:cw], in_=flat(pl)[:, 0:cw], func=AF.Relu, R=[plb], W=[Rl[h][1]])
            dcol = qi * 128 - c0
            has_diag = 0 <= dcol < cw
            for h in range(4):
                OP(cx, "pe", "matmul", pAf[:, 0:cw], dg[:, h, :], Rl[h][0][:, 0:cw], start=(h == 0), stop=(h == 3 and not has_diag),
                   R=[dgb, Rl[h][1]], W=[pAb])
            if has_diag:
                OP(cx, "pe", "matmul", pAf[:, dcol:dcol + 128], identb[:, :], NEGU[:, :], start=False, stop=True,
                   R=[identbb, NEGUb], W=[pAb])
            OP(cx, "act", "copy", out=scs[:, c0:c0 + cw], in_=pAf[:, 0:cw], R=[pAb], W=[scsb])
        OP(cx, "dve", "tensor_reduce", out=lo[:, :], in_=scs[:, 0:qi * 128], axis=AX.X, op=ALU.min, R=[scsb], W=[lob])
        OP(cx, "dve", "tensor_reduce", out=w0[:, :], in_=scs[:, 0:SV], axis=AX.X, op=ALU.max, R=[scsb], W=[w0b])
        OP(cx, "dve", "tensor_tensor", out=w0[:, :], in0=w0[:, :], in1=lo[:, :], op=ALU.subtract, R=[w0b, lob], W=[w0b])
        for it in range(1, NIT + 1):
            f = 2.0 ** -it
            OP(cx, "dve", "scalar_tensor_tensor", out=mid[:, :], in0=w0[:, :], scalar=f, in1=lo[:, :], op0=ALU.mult, op1=ALU.add,
               R=[w0b, lob], W=[midb])
            OP(cx, "dve", "tensor_scalar", out=jnk[:, 0:SV], in0=scs[:, 0:SV], scalar1=mid[:, 0:1], scalar2=0.0, op0=ALU.is_ge,
               op1=ALU.add, accum_out=cnt[:, 0:1], R=[scsb, midb], W=[jnkb, cntb])
            OP(cx, "dve", "tensor_scalar", out=stp[:, :], in0=cnt[:, :], scalar1=float(n_sel) - 0.5, scalar2=f, op0=ALU.is_ge,
               op1=ALU.mult, R=[cntb], W=[stpb])
            OP(cx, "dve", "scalar_tensor_tensor", out=lo[:, :], in0=stp[:, :], scalar=w0[:, 0:1], in1=lo[:, :], op0=ALU.mult,
               op1=ALU.add, R=[stpb, w0b, lob], W=[lob])
        OP(cx, "dve", "tensor_scalar", out=Mb[:, 0:SV], in0=scs[:, 0:SV], scalar1=lo[:, 0:1], scalar2=None, op0=ALU.is_ge,
           R=[scsb, lob], W=[Mbb])

    def attn(i, a):
        qT, qTb = qTs[i % 2]
        sq_ = (i // tiles_per_seq) % 2
        kT2, kT2b = kT2s[sq_]
        vpd, vpdb = vpds[sq_]
        yaT, yaTb = yaTs[i % 2]
        ti = i % tiles_per_seq
        ts = slice(a * 128, (a + 1) * 128)
        qi = ti * 2 + a
        nk = qi + 1
        Mb, Mbb = Mbs[qi % 2]
        def S_(kt):
            for half in range(2):
                ps_ = slice(half * 64, (half + 1) * 64)
                pst, pstb = pST[half]
                OP(cx, "pe", "matmul", flat(pst)[:, :], kT2[ps_, kt * 128:(kt + 1) * 128], qT[ps_, :, ts], start=True, stop=False,
                   R=[kT2b, qTb], W=[pstb])
                OP(cx, "pe", "matmul", flat(pst)[:, :], Mb[:, kt * 128:(kt + 1) * 128], bigi4[:, :, :], start=False, stop=True,
                   R=[Mbb, bigi4b], W=[pstb])

        def E_(kt):
            for half in range(2):
                pst, pstb = pST[half]
                pt_, ptb_ = PT[(kt % 2) * 2 + half]
                OP(cx, "act", "activation", out=pt_[:, :], in_=flat(pst)[:, :], func=AF.Exp, bias=negb[:, 0:1], R=[pstb, negbb], W=[ptb_])

        def V_(kt):
            for half in range(2):
                pt_, ptb_ = PT[(kt % 2) * 2 + half]
                po, pob = pO[half]
                for c in range(4):
                    OP(cx, "pe", "matmul", po[:, c, 0:65], pt_[:, c * 128:(c + 1) * 128], vpd[:, kt, :], start=(kt == 0 and c == 0),
                       stop=(kt == qi), skip_group_check=True, R=[ptb_, vpdb], W=[pob])

        S_(0)
        for kt in range(nk):
            E_(kt)
            if kt + 1 < nk:
                S_(kt + 1)
            V_(kt)
        for half in range(2):
            po, pob = pO[half]
            OP(cx, "dve", "reciprocal", out=rden[:, half, :], in_=po[:, :, 64], R=[pob], W=[rdenb])
            OP(cx, "dve", "tensor_tensor", out=ha[:, :, half, :], in0=po[:, :, 0:64],
               in1=rden[:, half, :].unsqueeze(2).to_broadcast([128, 4, 64]), op=ALU.mult, R=[pob, rdenb], W=[hab])
        haf = ha[:, :, :, :].rearrange("p c h d -> p (c h d)")
        for c in range(4):
            OP(cx, "pe", "transpose", pMT[:, 4 + c, :], haf[:, c * 128:(c + 1) * 128], identb[:, :], R=[hab, identbb], W=[pMTb])
        OP(cx, "act", "copy", out=yaT[:, :, ts], in_=pMT[:, 4:8, :], R=[pMTb], W=[yaTb])

    steps = [(i, a) for i in range(NT) for a in range(TT // 128)]
    load_h(0)
    if NT > 1:
        load_h(1)
    tail.load_x(0)
    proj(0)
    prep(0, 0)
    for k, (i, a) in enumerate(steps):
        if k + 1 < len(steps):
            i2, a2 = steps[k + 1]
            if a2 == 0:
                if i2 + 1 < NT:
                    load_h(i2 + 1)
                tail.load_x(i2)
                proj(i2)
            prep(i2, a2)
        attn(i, a)
        if a == TT // 128 - 1:
            hT, hb = hin[i % 3]
            tail.run(i, hT, hb, yaTs[i % 2][0], yaTs[i % 2][1])
    sc.close()


WNAMES = ["norm_ffn1", "ffn1_w_gu", "ffn1_w_down", "norm_mix", "w_in", "conv_w", "conv_w_out", "mlstm_b_i", "mlstm_b_f",
          "mlstm_norm", "mlstm_w_out", "attn_w_out", "w_o", "norm_ffn2", "ffn2_w_gu", "ffn2_w_down", "norm_ple",
          "ple_w_gate", "ple_w_proj", "final_norm"]


def build(S, nseq, depth, wshapes, phases=None, n_sel=256, dbg=None):
    nc = bass.Bass("TRN2", target_bir_lowering=False)
    NTC = S * nseq
    TT = 256
    G = {"S": S, "nseq": nseq, "NTC": NTC, "TT": TT, "NT": NTC // TT, "depth": depth, "n_sel": n_sel}
    G.update(dbg or {})
    G["x"] = nc.dram_tensor("x", [NTC, D], F32, kind="ExternalInput").ap()
    G["p"] = nc.dram_tensor("p", [depth, NTC, D_PLE], F32, kind="ExternalInput").ap()
    G["w"] = {k: nc.dram_tensor(k, list(wshapes[k]), F32, kind="ExternalInput").ap() for k in WNAMES}
    G["out"] = nc.dram_tensor("out", [NTC, D], F32, kind="ExternalOutput").ap()
    G["outb"] = Buf("out")
    if G.get("dbg_dump"):
        G["dbgo"] = nc.dram_tensor("dbgo", [128, 4096], F32, kind="ExternalOutput").ap()
    G["XT"] = nc.dram_tensor("XT", [D, NTC], F32).ap()
    G["XTb"] = [Buf("XT%d" % i) for i in range(G["NT"])]
    G["HT"] = nc.dram_tensor("HT", [D, NTC], BF16).ap()
    G["HTb"] = [Buf("HT%d" % i) for i in range(G["NT"])]
    with contextlib.ExitStack() as es:
        cx = Ctx(nc, es)
        phase_in(cx, G)
        for l in range(depth):
            if phases is None or "ffn1" in phases:
                phase_ffn(cx, G, l, 1)
            if phases is None or "conv" in phases or "mlstm" in phases or "dsa" in phases:
                phase_norm(cx, G, l)
            if phases is None or "conv" in phases:
                phase_conv(cx, G, l)
            if phases is None or "mlstm" in phases:
                phase_mlstm(cx, G, l)
            if phases is None or "dsa" in phases:
                phase_dsa(cx, G, l)
            if phases is None or "ffn2" in phases:
                phase_ffn(cx, G, l, 2)
            if phases is None or "ple" in phases:
                phase_ple(cx, G, l)
        phase_out(cx, G)
        cx.P.add("sp", lambda e: e.nop(), R=[G["outb"]], chan=None) if False else None
        cx.P.barrier()
        cx.P.emit()
    return nc


def kernel(**inputs):
    x = np.ascontiguousarray(inputs["x"], dtype=np.float32)
    p = np.ascontiguousarray(inputs["p"], dtype=np.float32)
    B, S, _ = x.shape
    depth = p.shape[0]
    nseq = B // N_CORES
    wshapes = {k: inputs[k].shape for k in WNAMES}
    nc = build(S, nseq, depth, wshapes)
    in_maps = []
    for c in range(N_CORES):
        m = {k: np.ascontiguousarray(inputs[k], dtype=np.float32) for k in WNAMES}
        m["x"] = x[c * nseq:(c + 1) * nseq].reshape(nseq * S, D)
        m["p"] = np.ascontiguousarray(p[:, c * nseq:(c + 1) * nseq].reshape(depth, nseq * S, D_PLE))
        in_maps.append(m)
    res = run_bass_kernel_spmd(nc, in_maps, core_ids=list(range(N_CORES)))
    out = np.concatenate([r["out"].reshape(nseq, S, D) for r in res.results], axis=0)
    return out.astype(np.float32)
```

```python
import contextlib
import numpy as np
import concourse.bass as bass
import concourse.mybir as mybir
from concourse.bass_utils import run_bass_kernel_spmd

F32 = mybir.dt.float32
BF16 = mybir.dt.bfloat16
AF = mybir.ActivationFunctionType
ALU = mybir.AluOpType
AX = mybir.AxisListType

D = 1024
KD = 8
FF = 2816
KF = 22
D_PLE = 256
N_IN = 8652
EPS = 1e-6
N_CORES = 8


class Buf:
    __slots__ = ("w", "r", "name", "excl")

    def __init__(self, name="", excl=False):
        self.w = None
        self.r = {}
        self.name = name
        self.excl = excl


class Stream:
    def __init__(self, name, sem, inc):
        self.name, self.sem, self.inc = name, sem, inc
        self.ops = []


class Op:
    __slots__ = ("fn", "waits", "sig", "stream", "sigcount")

    def __init__(self, fn, stream):
        self.fn, self.stream = fn, stream
        self.waits = {}
        self.sig = False
        self.sigcount = 0


class Prog:
    ENGS = ("pe", "act", "dve", "pool", "sp")

    def __init__(self, nc, es):
        self.nc = nc
        self.es = es
        self.q = {e: [] for e in self.ENGS}
        self.seen = {e: {} for e in self.ENGS}
        self.streams = {}
        self.chan_n = {}
        for e in ("pe", "act", "dve", "pool"):
            self.new_stream(e, 1)

    def new_stream(self, name, inc=16):
        sem = self.es.enter_context(self.nc.semaphore("s_" + name))
        self.streams[name] = Stream(name, sem, inc)
        return self.streams[name]

    NSLOT = 8

    def new_channel(self, name):
        self.chan_n[name] = 0
        for k in range(self.NSLOT):
            self.new_stream("%s%d" % (name, k), 16)

    def add(self, eng, fn, R=(), W=(), chan=None):
        if chan is not None:
            k = self.chan_n[chan]
            self.chan_n[chan] = k + 1
            st = self.streams["%s%d" % (chan, k % self.NSLOT)]
        else:
            st = self.streams[eng]
        op = Op(fn, st)
        if st.inc == 16:
            op.sig = True
        st.ops.append(op)
        idx = len(st.ops)
        seen = self.seen[eng]
        waits = op.waits

        def need(dep):
            if dep is None:
                return
            s, j = dep
            if s is st and eng == "pe":
                return
            if seen.get(s, 0) >= j:
                return
            if waits.get(s, 0) < j:
                waits[s] = j

        if st.inc == 16 and idx > 1:
            need((st, idx - 1))
        for b in R:
            need(b.w)
            if b.excl:
                for s, j in b.r.items():
                    if s is not st:
                        need((s, j))
        for b in W:
            need(b.w)
            for s, j in b.r.items():
                if s is st and st.inc == 1:
                    continue
                need((s, j))
        for s, j in waits.items():
            seen[s] = j
            s.ops[j - 1].sig = True
        for b in R:
            if b.r.get(st, 0) < idx:
                b.r[st] = idx
        for b in W:
            b.w = (st, idx)
            b.r = {}
        self.q[eng].append(op)
        return op

    def barrier(self):
        tails = [(s, len(s.ops)) for s in self.streams.values() if s.ops]
        for e in self.ENGS:
            op = Op(None, None)
            seen = self.seen[e]
            for s, j in tails:
                if seen.get(s, 0) >= j:
                    continue
                op.waits[s] = j
                seen[s] = j
                s.ops[j - 1].sig = True
            self.q[e].append(op)

    def emit(self):
        for st in self.streams.values():
            c = 0
            for op in st.ops:
                if op.sig:
                    c += 1
                op.sigcount = c
        nc = self.nc
        q = self.q

        def run(e, ops):
            for op in ops:
                for s, j in op.waits.items():
                    e.wait_ge(s.sem, s.ops[j - 1].sigcount * s.inc)
                if op.fn is None:
                    continue
                ins = op.fn(e)
                if op.sig:
                    ins.then_inc(op.stream.sem, op.stream.inc)

        for st in self.streams.values():
            nc.gpsimd.sem_clear(st.sem)
        with nc.Block() as block:
            @block.tensor
            def _(e):
                run(e, q["pe"])

            @block.scalar
            def _(e):
                run(e, q["act"])

            @block.vector
            def _(e):
                run(e, q["dve"])

            @block.gpsimd
            def _(e):
                run(e, q["pool"])

            @block.sync
            def _(e):
                run(e, q["sp"])
        for st in self.streams.values():
            nc.gpsimd.sem_clear(st.sem)


class Ctx:
    def __init__(self, nc, es):
        self.nc, self.es = nc, es
        self.P = Prog(nc, es)
        self.P.new_channel("ld")
        self.P.new_channel("st")
        self.P.new_channel("wl")
        self.P.new_channel("pl")
        self._n = 0

    def sb(self, shape, dt, name=None):
        self._n += 1
        return self.es.enter_context(self.nc.sbuf_tensor(f"{name or 't'}_{self._n}", list(shape), dt))

    def ps(self, shape, dt=F32, name=None):
        self._n += 1
        return self.es.enter_context(self.nc.psum_tensor(f"{name or 'p'}_{self._n}", list(shape), dt))


class Scope:
    def __init__(self, cx):
        self.cx = cx
        self.es = contextlib.ExitStack()
        self.bufs = []

    def sb(self, shape, dt, name="t"):
        cx = self.cx
        cx._n += 1
        t = self.es.enter_context(cx.nc.sbuf_tensor(f"{name}_{cx._n}", list(shape), dt))
        b = Buf(name)
        self.bufs.append(b)
        return t, b

    def ps(self, shape, dt=F32, name="p"):
        cx = self.cx
        cx._n += 1
        t = self.es.enter_context(cx.nc.psum_tensor(f"{name}_{cx._n}", list(shape), dt))
        b = Buf(name, excl=True)
        self.bufs.append(b)
        return t, b

    def close(self):
        self.cx.P.barrier()
        self.es.close()


def load_w(cx, sc, w2d, kc, n0, n1, name, dst=None, dcol=0):
    P = cx.P
    n = n1 - n0
    if dst is None:
        t, b = sc.sb([128, kc, n], BF16, name)
    else:
        t, b = dst
    for c in range(kc):
        P.add("pool", lambda e, c=c: e.dma_start(out=t[:, c, dcol:dcol + n], in_=w2d[c * 128:(c + 1) * 128, n0:n1]),
              W=[b], chan="pl")
    return t, b


def load_vec_cols(cx, sc, v1d, kc, name):
    t, b = sc.sb([128, kc], F32, name)
    cx.P.add("sp", lambda e: e.dma_start(out=t[:, :], in_=v1d.rearrange("(c p) -> p c", p=128),
                                         allow_slow_non_contiguous=True), W=[b], chan="wl")
    return t, b


def rmsnorm_T(cx, sc, st, xt, xb, g, gb, TT):
    P = cx.P
    sq, sqb = st["sq"]
    hT, hb = st["hT"]
    ssp, sspb = st["ssp"]
    rs, rsb = st["rs"]
    ones, onesb = st["ones"]
    P.add("act", lambda e: e.activation(out=sq[:, :, :], in_=xt[:, :, :], func=AF.Square), R=[xb], W=[sqb])
    for c in range(KD):
        P.add("pe", lambda e, c=c: e.matmul(ssp[:, 0:TT], ones[:, :], sq[:, c, :], start=(c == 0), stop=(c == KD - 1)),
              R=[sqb, onesb], W=[sspb])
    P.add("act", lambda e: e.activation(out=rs[:, 0:TT], in_=ssp[:, 0:TT], func=AF.Sqrt, scale=1.0 / D, bias=st["eps"][0][:, 0:1]),
          R=[sspb, st["eps"][1]], W=[rsb])
    P.add("dve", lambda e: e.reciprocal(out=rs[:, 0:TT], in_=rs[:, 0:TT]), R=[rsb], W=[rsb])
    for c in range(KD):
        P.add("dve", lambda e, c=c: e.scalar_tensor_tensor(out=hT[:, c, :], in0=xt[:, c, :], scalar=g[:, c:c + 1],
                                                           in1=rs[:, 0:TT], op0=ALU.mult, op1=ALU.mult),
              R=[xb, gb, rsb], W=[hb])
    return hT, hb


def norm_scratch(cx, sc, TT):
    st = {}
    st["sq"] = sc.sb([128, KD, TT], BF16, "sq")
    st["hT"] = sc.sb([128, KD, TT], BF16, "hT")
    st["ssp"] = sc.ps([128, 512], F32, "ssp")
    st["rs"] = sc.sb([128, TT], F32, "rs")
    st["ones"] = sc.sb([128, 128], BF16, "ones")
    st["eps"] = sc.sb([128, 1], F32, "eps")
    ones, onesb = st["ones"]
    eps, epsb = st["eps"]
    cx.P.add("pool", lambda e: e.memset(ones[:, :], 1.0), W=[onesb])
    cx.P.add("pool", lambda e: e.memset(eps[:, :], EPS), W=[epsb])
    return st


def make_identity(cx, sc, dt, name="ident"):
    t, b = sc.sb([128, 128], dt, name)
    cx.P.add("pool", lambda e: e.memset(t[:, :], 1.0), W=[b])
    cx.P.add("pool", lambda e: e.affine_select(out=t[:, :], in_=t[:, :], pattern=[[-1, 128]], compare_op=ALU.is_equal,
                                               fill=0.0, base=0, channel_multiplier=1), R=[b], W=[b])
    return t, b


def phase_in(cx, G):
    P = cx.P
    TT = G["TT"]
    sc = Scope(cx)
    ident, identb = make_identity(cx, sc, F32)
    xin = [sc.sb([128, TT // 128, D], F32, "xin") for _ in range(2)]
    xo = [sc.sb([128, KD, TT], F32, "xo") for _ in range(2)]
    pp = [sc.ps([128, 2, TT], F32, "tp") for _ in range(4)]
    x = G["x"]
    for i in range(G["NT"]):
        xi, xib = xin[i % 2]
        xo_t, xob = xo[i % 2]
        P.add("sp", lambda e, i=i, xi=xi: e.dma_start(
            out=xi[:, :, :], in_=x[i * TT:(i + 1) * TT, :].rearrange("(a p) d -> p a d", p=128)), W=[xib], chan="ld")
        for c2 in range(KD // 2):
            pt, ptb = pp[c2 % 4]
            for cc in range(2):
                c = c2 * 2 + cc
                for a in range(TT // 128):
                    P.add("pe", lambda e, c=c, cc=cc, a=a, pt=pt, xi=xi: e.transpose(
                        pt[:, cc, a * 128:(a + 1) * 128], xi[:, a, c * 128:(c + 1) * 128], ident[:, :]),
                        R=[xib, identb], W=[ptb])
            eng = "act" if c2 % 2 == 0 else "dve"
            if eng == "act":
                P.add("act", lambda e, c2=c2, pt=pt, xo_t=xo_t: e.copy(out=xo_t[:, 2 * c2:2 * c2 + 2, :], in_=pt[:, :, :]),
                      R=[ptb], W=[xob])
            else:
                P.add("dve", lambda e, c2=c2, pt=pt, xo_t=xo_t: e.tensor_copy(out=xo_t[:, 2 * c2:2 * c2 + 2, :], in_=pt[:, :, :]),
                      R=[ptb], W=[xob])
        P.add("sp", lambda e, i=i, xo_t=xo_t: e.dma_start(
            out=G["XT"][:, i * TT:(i + 1) * TT].rearrange("(c p) t -> p c t", p=128), in_=xo_t[:, :, :]),
            R=[xob], W=[G["XTb"][i]], chan="st")
    sc.close()


def phase_out(cx, G):
    P = cx.P
    TT = G["TT"]
    sc = Scope(cx)
    ident, identb = make_identity(cx, sc, F32)
    st = norm_scratch(cx, sc, TT)
    g, gb = load_vec_cols(cx, sc, G["w"]["final_norm"], KD, "gfin")
    xin = [sc.sb([128, KD, TT], F32, "xin") for _ in range(2)]
    yn = sc.sb([128, KD, TT], F32, "yn")
    yo = [sc.sb([128, D], F32, "yo") for _ in range(2)]
    pp = [sc.ps([128, 512], F32, "tp") for _ in range(4)]
    rs, rsb = st["rs"]
    sq, sqb = st["sq"]
    ssp, sspb = st["ssp"]
    ones, onesb = st["ones"]
    k = 0
    for i in range(G["NT"]):
        xt, xb = xin[i % 2]
        P.add("sp", lambda e, i=i, xt=xt: e.dma_start(
            out=xt[:, :, :], in_=G["XT"][:, i * TT:(i + 1) * TT].rearrange("(c p) t -> p c t", p=128)),
            R=[G["XTb"][i]], W=[xb], chan="ld")
        P.add("act", lambda e, xt=xt: e.activation(out=sq[:, :, :], in_=xt[:, :, :], func=AF.Square), R=[xb], W=[sqb])
        for c in range(KD):
            P.add("pe", lambda e, c=c: e.matmul(ssp[:, 0:TT], ones[:, :], sq[:, c, :], start=(c == 0), stop=(c == KD - 1)),
                  R=[sqb, onesb], W=[sspb])
        P.add("act", lambda e: e.activation(out=rs[:, 0:TT], in_=ssp[:, 0:TT], func=AF.Sqrt, scale=1.0 / D, bias=st["eps"][0][:, 0:1]),
              R=[sspb, st["eps"][1]], W=[rsb])
        P.add("dve", lambda e: e.reciprocal(out=rs[:, 0:TT], in_=rs[:, 0:TT]), R=[rsb], W=[rsb])
        y, yb = yn
        for c in range(KD):
            P.add("dve", lambda e, c=c, xt=xt: e.scalar_tensor_tensor(out=y[:, c, :], in0=xt[:, c, :], scalar=g[:, c:c + 1],
                                                                      in1=rs[:, 0:TT], op0=ALU.mult, op1=ALU.mult),
                  R=[xb, gb, rsb], W=[yb])
        for a in range(TT // 128):
            yt, ytb = yo[k % 2]
            for hh in range(2):
                pt, ptb = pp[(2 * k + hh) % 4]
                for c4 in range(4):
                    c = hh * 4 + c4
                    P.add("pe", lambda e, c=c, c4=c4, a=a, pt=pt: e.transpose(
                        pt[:, c4 * 128:(c4 + 1) * 128], y[:, c, a * 128:(a + 1) * 128], ident[:, :]),
                        R=[yb, identb], W=[ptb])
                if hh == 0:
                    P.add("act", lambda e, pt=pt, yt=yt: e.copy(out=yt[:, 0:512], in_=pt[:, :]), R=[ptb], W=[ytb])
                else:
                    P.add("dve", lambda e, pt=pt, yt=yt: e.tensor_copy(out=yt[:, 512:1024], in_=pt[:, :]), R=[ptb], W=[ytb])
            r0 = i * TT + a * 128
            P.add("sp", lambda e, r0=r0, yt=yt: e.dma_start(out=G["out"][r0:r0 + 128, :], in_=yt[:, :]),
                  R=[ytb], W=[G["outb"]], chan="st")
            k += 1
    sc.close()


def phase_ffn(cx, G, l, which):
    P = cx.P
    TT = G["TT"]
    w = G["w"]
    sc = Scope(cx)
    pre = "ffn1" if which == 1 else "ffn2"
    wgu, wgub = load_w(cx, sc, w[pre + "_w_gu"][l], KD, 0, 2 * FF, "wgu")
    wd, wdb = load_w(cx, sc, w[pre + "_w_down"][l], KF, 0, D, "wd")
    g, gb = load_vec_cols(cx, sc, w["norm_" + pre][l], KD, "gn")
    st = norm_scratch(cx, sc, TT)
    xin = [sc.sb([128, KD, TT], F32, "xin") for _ in range(2)]
    m, mb = sc.sb([128, KF, TT], BF16, "m")
    av = [sc.sb([128, TT], F32, "a") for _ in range(2)]
    pgu = [sc.ps([128, 2, TT], F32, "pgu") for _ in range(3)]
    pdn = [sc.ps([128, 2, TT], F32, "pdn") for _ in range(2)]

    def load(i):
        xt, xb = xin[i % 2]
        P.add("sp", lambda e: e.dma_start(
            out=xt[:, :, :], in_=G["XT"][:, i * TT:(i + 1) * TT].rearrange("(c p) t -> p c t", p=128)),
            R=[G["XTb"][i]], W=[xb], chan="ld")

    hTs = [st["hT"], sc.sb([128, KD, TT], BF16, "hT2")]

    def norm(i):
        st["hT"] = hTs[i % 2]
        return rmsnorm_T(cx, sc, st, xin[i % 2][0], xin[i % 2][1], g, gb, TT)

    load(0)
    nxt = norm(0)
    for i in range(G["NT"]):
        xt, xb = xin[i % 2]
        if i + 1 < G["NT"]:
            load(i + 1)
        hT, hb = nxt
        for j in range(KF):
            pg, pgb = pgu[j % 3]
            for half in range(2):
                col = half * FF + j * 128
                for c in range(KD):
                    P.add("pe", lambda e, c=c, col=col, half=half, pg=pg, hT=hT: e.matmul(
                        pg[:, half, :], wgu[:, c, col:col + 128], hT[:, c, :], start=(c == 0), stop=(c == KD - 1)),
                        R=[wgub, hb], W=[pgb])
            a, ab = av[j % 2]
            P.add("act", lambda e, pg=pg, a=a: e.activation(out=a[:, :], in_=pg[:, 0, :], func=AF.Silu), R=[pgb], W=[ab])
            P.add("dve", lambda e, pg=pg, a=a, j=j: e.tensor_tensor(out=m[:, j, :], in0=a[:, :], in1=pg[:, 1, :], op=ALU.mult),
                  R=[pgb, ab], W=[mb])
        if i + 1 < G["NT"]:
            nxt = norm(i + 1)
        for o2 in range(KD // 2):
            pd, pdb = pdn[o2 % 2]
            for oo in range(2):
                oc = o2 * 2 + oo
                for j in range(KF):
                    P.add("pe", lambda e, j=j, oc=oc, oo=oo, pd=pd: e.matmul(
                        pd[:, oo, :], wd[:, j, oc * 128:(oc + 1) * 128], m[:, j, :], start=(j == 0), stop=(j == KF - 1)),
                        R=[wdb, mb], W=[pdb])
            P.add("dve", lambda e, o2=o2, pd=pd, xt=xt: e.scalar_tensor_tensor(
                out=xt[:, 2 * o2:2 * o2 + 2, :], in0=pd[:, :, :], scalar=0.5, in1=xt[:, 2 * o2:2 * o2 + 2, :],
                op0=ALU.mult, op1=ALU.add), R=[pdb, xb], W=[xb])
        P.add("sp", lambda e, i=i, xt=xt: e.dma_start(
            out=G["XT"][:, i * TT:(i + 1) * TT].rearrange("(c p) t -> p c t", p=128), in_=xt[:, :, :]),
            R=[xb], W=[G["XTb"][i]], chan="st")
    sc.close()


OFF_CB, OFF_CC, OFF_CX = 0, 512, 1024
OFF_MQ, OFF_MK, OFF_MV, OFF_MO, OFF_MI, OFF_MF = 1536, 2048, 2560, 3584, 4608, 4612
OFF_AQ, OFF_AK, OFF_AV, OFF_IQ, OFF_IK, OFF_IW = 4616, 5128, 5192, 5256, 5512, 5576
OFF_G = 5580


def xt_ap(G, i):
    TT = G["TT"]
    return G["XT"][:, i * TT:(i + 1) * TT].rearrange("(c p) t -> p c t", p=128)


def ht_ap(G, i):
    TT = G["TT"]
    return G["HT"][:, i * TT:(i + 1) * TT].rearrange("(c p) t -> p c t", p=128)


def phase_norm(cx, G, l):
    P = cx.P
    TT = G["TT"]
    sc = Scope(cx)
    g, gb = load_vec_cols(cx, sc, G["w"]["norm_mix"][l], KD, "gn")
    st = norm_scratch(cx, sc, TT)
    xin = [sc.sb([128, KD, TT], F32, "xin") for _ in range(2)]
    ho = [sc.sb([128, KD, TT], BF16, "ho") for _ in range(2)]
    for i in range(G["NT"]):
        xt, xb = xin[i % 2]
        P.add("sp", lambda e, i=i, xt=xt: e.dma_start(out=xt[:, :, :], in_=xt_ap(G, i)), R=[G["XTb"][i]], W=[xb], chan="ld")
        st["hT"] = ho[i % 2]
        hT, hb = rmsnorm_T(cx, sc, st, xt, xb, g, gb, TT)
        P.add("sp", lambda e, i=i, hT=hT: e.dma_start(out=ht_ap(G, i), in_=hT[:, :, :]), R=[hb], W=[G["HTb"][i]], chan="st")
    sc.close()


class Tail:
    def __init__(self, cx, sc, G, l, br, wout2d, kin, psA, psB):
        w = G["w"]
        TT = G["TT"]
        self.cx, self.G, self.kin = cx, G, kin
        self.wg = load_w(cx, sc, w["w_in"][l], KD, OFF_G + br * D, OFF_G + (br + 1) * D, "wgate")
        self.wout = load_w(cx, sc, wout2d, kin, 0, D, "wout")
        self.wo = load_w(cx, sc, w["w_o"][l], KD, 0, D, "wo")
        self.sg = [sc.sb([128, TT], F32, "sg") for _ in range(2)]
        self.mg = sc.sb([128, KD, TT], BF16, "mg")
        self.xin = [sc.sb([128, KD, TT], F32, "xres") for _ in range(2)]
        self.psA, self.psB = psA, psB

    def load_x(self, i):
        G = self.G
        xt, xb = self.xin[i % 2]
        self.cx.P.add("sp", lambda e: e.dma_start(out=xt[:, :, :], in_=xt_ap(G, i)), R=[G["XTb"][i]], W=[xb], chan="ld")

    def run(self, i, hT, hb, yT, yb):
        P = self.cx.P
        G = self.G
        wg, wgb = self.wg
        wout, woutb = self.wout
        wo, wob = self.wo
        mg, mgb = self.mg
        xt, xb = self.xin[i % 2]
        kin = self.kin
        for oc in range(KD):
            pa, pab = self.psA[oc % len(self.psA)]
            for c in range(kin):
                P.add("pe", lambda e, c=c, oc=oc, pa=pa: e.matmul(pa[:, 0, :], wout[:, c, oc * 128:(oc + 1) * 128], yT[:, c, :],
                                                               start=(c == 0), stop=(c == kin - 1)), R=[woutb, yb], W=[pab])
            for c in range(KD):
                P.add("pe", lambda e, c=c, oc=oc, pa=pa: e.matmul(pa[:, 1, :], wg[:, c, oc * 128:(oc + 1) * 128], hT[:, c, :],
                                                               start=(c == 0), stop=(c == KD - 1)), R=[wgb, hb], W=[pab])
            sg, sgb = self.sg[oc % 2]
            P.add("act", lambda e, pa=pa, sg=sg: e.activation(out=sg[:, :], in_=pa[:, 1, :], func=AF.Sigmoid), R=[pab], W=[sgb])
            P.add("dve", lambda e, pa=pa, sg=sg, oc=oc: e.tensor_tensor(out=mg[:, oc, :], in0=sg[:, :], in1=pa[:, 0, :], op=ALU.mult),
                  R=[pab, sgb], W=[mgb])
        for o2 in range(KD // 2):
            po, pob = self.psB[o2 % len(self.psB)]
            for oo in range(2):
                oc = o2 * 2 + oo
                for c in range(KD):
                    P.add("pe", lambda e, c=c, oc=oc, oo=oo, po=po: e.matmul(po[:, oo, :], wo[:, c, oc * 128:(oc + 1) * 128], mg[:, c, :],
                                                                         start=(c == 0), stop=(c == KD - 1)), R=[wob, mgb], W=[pob])
            P.add("dve", lambda e, o2=o2, po=po: e.tensor_tensor(out=xt[:, 2 * o2:2 * o2 + 2, :], in0=xt[:, 2 * o2:2 * o2 + 2, :],
                                                              in1=po[:, :, :], op=ALU.add), R=[pob, xb], W=[xb])
        P.add("sp", lambda e: e.dma_start(out=xt_ap(G, i), in_=xt[:, :, :]), R=[xb], W=[G["XTb"][i]], chan="st")


def phase_conv(cx, G, l):
    P = cx.P
    TT = G["TT"]
    w = G["w"]
    sc = Scope(cx)
    win, winb = load_w(cx, sc, w["w_in"][l], KD, 0, 1536, "winc")
    psA = [sc.ps([128, 2, TT], F32, "psA") for _ in range(2)]
    psB = [sc.ps([128, 2, TT], F32, "psB") for _ in range(2)]
    pcx = [sc.ps([128, 2, TT], F32, "pcx") for _ in range(2)]
    pb_ = [sc.ps([128, 2, TT], F32, "pbb") for _ in range(2)]
    tail = Tail(cx, sc, G, l, 0, w["conv_w_out"][l], 4, psA, psB)
    cw, cwb = sc.sb([128, 4, 3], F32, "cw")
    for j in range(3):
        P.add("sp", lambda e, j=j: e.dma_start(out=cw[:, :, j], in_=w["conv_w"][l][j].rearrange("(c p) -> p c", p=128),
                                               allow_slow_non_contiguous=True), W=[cwb], chan="wl")
    hin = [sc.sb([128, KD, TT], BF16, "hin") for _ in range(2)]
    u = [sc.sb([128, TT + 2], F32, "u") for _ in range(4)]
    ccs = [sc.sb([128, TT], F32, "ccs") for _ in range(2)]
    yv = [sc.sb([128, TT], F32, "yv") for _ in range(2)]
    z, zb = sc.sb([128, 4, TT], BF16, "z")
    tiles_per_seq = G["S"] // TT

    def load(i):
        hT, hb = hin[i % 2]
        P.add("sp", lambda e: e.dma_start(out=hT[:, :, :], in_=ht_ap(G, i)), R=[G["HTb"][i]], W=[hb], chan="ld")
        tail.load_x(i)

    load(0)
    for i in range(G["NT"]):
        hT, hb = hin[i % 2]
        if i + 1 < G["NT"]:
            load(i + 1)
        for q in range(4):
            ut, ub = u[q]
            if i % tiles_per_seq == 0:
                P.add("pool", lambda e, ut=ut: e.memset(ut[:, 0:2], 0.0), W=[ub])
            p1, p1b = pcx[q % 2]
            p2, p2b = pb_[q % 2]
            for k_, (pt, ptb, slot, off) in enumerate(((p1, p1b, 0, OFF_CC), (p1, p1b, 1, OFF_CX), (p2, p2b, 0, OFF_CB))):
                for c in range(KD):
                    P.add("pe", lambda e, c=c, pt=pt, slot=slot, col=off + q * 128, hT=hT: e.matmul(
                        pt[:, slot, :], win[:, c, col:col + 128], hT[:, c, :], start=(c == 0), stop=(c == KD - 1)),
                        R=[winb, hb], W=[ptb])
            cs, csb = ccs[q % 2]
            P.add("act", lambda e, p1=p1, cs=cs: e.copy(out=cs[:, :], in_=p1[:, 0, :]), R=[p1b], W=[csb])
            P.add("dve", lambda e, p1=p1, cs=cs, ut=ut: e.tensor_tensor(out=ut[:, 2:TT + 2], in0=cs[:, :], in1=p1[:, 1, :], op=ALU.mult),
                  R=[p1b, csb], W=[ub])
            y, yb_ = yv[q % 2]
            P.add("dve", lambda e, y=y, ut=ut, q=q: e.tensor_scalar(out=y[:, :], in0=ut[:, 0:TT], scalar1=cw[:, q, 0:1], scalar2=None,
                                                                  op0=ALU.mult), R=[ub, cwb], W=[yb_])
            P.add("dve", lambda e, y=y, ut=ut, q=q: e.scalar_tensor_tensor(out=y[:, :], in0=ut[:, 1:TT + 1], scalar=cw[:, q, 1:2],
                                                                         in1=y[:, :], op0=ALU.mult, op1=ALU.add), R=[ub, cwb, yb_], W=[yb_])
            P.add("dve", lambda e, y=y, ut=ut, q=q: e.scalar_tensor_tensor(out=y[:, :], in0=ut[:, 2:TT + 2], scalar=cw[:, q, 2:3],
                                                                         in1=y[:, :], op0=ALU.mult, op1=ALU.add), R=[ub, cwb, yb_], W=[yb_])
            P.add("pool", lambda e, ut=ut: e.tensor_copy(out=ut[:, 0:2], in_=ut[:, TT:TT + 2]), R=[ub], W=[ub])
            P.add("dve", lambda e, y=y, p2=p2, q=q: e.tensor_tensor(out=z[:, q, :], in0=y[:, :], in1=p2[:, 0, :], op=ALU.mult),
                  R=[yb_, p2b], W=[zb])
        tail.run(i, hT, hb, z, zb)
    sc.close()


def phase_ple(cx, G, l):
    P = cx.P
    TT = G["TT"]
    w = G["w"]
    sc = Scope(cx)
    wg, wgb = load_w(cx, sc, w["ple_w_gate"][l], KD, 0, D, "wpg")
    wp, wpb = load_w(cx, sc, w["ple_w_proj"][l], 2, 0, D, "wpp")
    g, gb = load_vec_cols(cx, sc, w["norm_ple"][l], KD, "gn")
    st = norm_scratch(cx, sc, TT)
    ident, identb = make_identity(cx, sc, F32)
    xin = [sc.sb([128, KD, TT], F32, "xin") for _ in range(2)]
    pin = [sc.sb([128, TT // 128, D_PLE], F32, "pin") for _ in range(2)]
    pT, pTb = sc.sb([128, 2, TT], BF16, "pT")
    sgs = [sc.sb([128, TT], F32, "sg") for _ in range(2)]
    ptp = sc.ps([128, 2, TT], F32, "ptp")
    psA = [sc.ps([128, 2, TT], F32, "psA") for _ in range(3)]

    def load(i):
        xt, xb = xin[i % 2]
        P.add("sp", lambda e: e.dma_start(out=xt[:, :, :], in_=xt_ap(G, i)), R=[G["XTb"][i]], W=[xb], chan="ld")
        pi, pib = pin[i % 2]
        P.add("sp", lambda e: e.dma_start(out=pi[:, :, :], in_=G["p"][l, i * TT:(i + 1) * TT, :].rearrange("(a p) d -> p a d", p=128)),
              W=[pib], chan="ld")

    hTs = [st["hT"], sc.sb([128, KD, TT], BF16, "hT2")]

    def norm(i):
        st["hT"] = hTs[i % 2]
        return rmsnorm_T(cx, sc, st, xin[i % 2][0], xin[i % 2][1], g, gb, TT)

    load(0)
    nxt = norm(0)
    for i in range(G["NT"]):
        xt, xb = xin[i % 2]
        pi, pib = pin[i % 2]
        if i + 1 < G["NT"]:
            load(i + 1)
        hT, hb = nxt
        pt, ptb = ptp
        for kc in range(2):
            for a in range(TT // 128):
                P.add("pe", lambda e, kc=kc, a=a, pi=pi: e.transpose(pt[:, kc, a * 128:(a + 1) * 128], pi[:, a, kc * 128:(kc + 1) * 128], ident[:, :]),
                      R=[pib, identb], W=[ptb])
        P.add("act", lambda e: e.copy(out=pT[:, :, :], in_=pt[:, :, :]), R=[ptb], W=[pTb])
        for oc in range(KD):
            if oc == 4 and i + 1 < G["NT"]:
                nxt = norm(i + 1)
            pa, pab = psA[oc % 3]
            for c in range(KD):
                P.add("pe", lambda e, c=c, oc=oc, pa=pa, hT=hT: e.matmul(pa[:, 0, :], wg[:, c, oc * 128:(oc + 1) * 128], hT[:, c, :],
                                                               start=(c == 0), stop=(c == KD - 1)), R=[wgb, hb], W=[pab])
            for c in range(2):
                P.add("pe", lambda e, c=c, oc=oc, pa=pa: e.matmul(pa[:, 1, :], wp[:, c, oc * 128:(oc + 1) * 128], pT[:, c, :],
                                                               start=(c == 0), stop=(c == 1)), R=[wpb, pTb], W=[pab])
            sg, sgb = sgs[oc % 2]
            P.add("act", lambda e, pa=pa, sg=sg: e.activation(out=sg[:, :], in_=pa[:, 0, :], func=AF.Sigmoid), R=[pab], W=[sgb])
            if G.get("dbg_dump") and i == 0 and oc == 0:
                P.add("sp", lambda e, sg=sg: e.dma_start(out=G["dbgo"][:, 0:TT], in_=sg[:, :]), R=[sgb], W=[G["outb"]], chan="st")
                d2, d2b = sc.sb([128, TT], F32, "d2")
                P.add("act", lambda e, pa=pa: e.copy(out=d2[:, :], in_=pa[:, 1, :]), R=[pab], W=[d2b])
                P.add("sp", lambda e: e.dma_start(out=G["dbgo"][:, TT:2 * TT], in_=d2[:, :]), R=[d2b], W=[G["outb"]], chan="st")
                d3, d3b = sc.sb([128, 2, TT], F32, "d3")
                P.add("act", lambda e: e.copy(out=d3[:, :, :], in_=pT[:, :, :]), R=[pTb], W=[d3b])
                P.add("sp", lambda e: e.dma_start(out=G["dbgo"][:, 2 * TT:4 * TT], in_=d3[:, :, :].rearrange("p a t -> p (a t)")), R=[d3b], W=[G["outb"]], chan="st")
            P.add("dve", lambda e, pa=pa, sg=sg: e.tensor_tensor(out=sg[:, :], in0=sg[:, :], in1=pa[:, 1, :], op=ALU.mult),
                  R=[pab, sgb], W=[sgb])
            if not G.get("dbg_skip_add"):
                P.add("pool", lambda e, sg=sg, oc=oc, xt=xt: e.tensor_tensor(out=xt[:, oc, :], in0=xt[:, oc, :], in1=sg[:, :], op=ALU.add),
                      R=[sgb, xb], W=[xb])
        P.add("sp", lambda e, i=i, xt=xt: e.dma_start(out=xt_ap(G, i), in_=xt[:, :, :]), R=[xb], W=[G["XTb"][i]], chan="st")
    sc.close()


import math


def OP(cx, eng, method, *args, R=(), W=(), chan=None, **kw):
    return cx.P.add(eng, lambda e: getattr(e, method)(*args, **kw), R=R, W=W, chan=chan)


def flat(t):
    return t[:, :, :].rearrange("p a t -> p (a t)")


def make_tri(cx, sc, dt, val, name):
    t, b = sc.sb([128, 128], dt, name)
    OP(cx, "pool", "memset", t[:, :], val, W=[b])
    OP(cx, "pool", "affine_select", out=t[:, :], in_=t[:, :], pattern=[[1, 128]], compare_op=ALU.is_ge, fill=0.0, base=0,
       channel_multiplier=-1, R=[b], W=[b])
    return t, b


def phase_mlstm(cx, G, l):
    P = cx.P
    TT = G["TT"]
    w = G["w"]
    sc = Scope(cx)
    win, winb = load_w(cx, sc, w["w_in"][l], KD, OFF_MQ, OFF_AQ, "winm")
    pA, pAb = sc.ps([128, 2, TT], F32, "pA")
    pK, pKb = sc.ps([128, 2, TT], F32, "pK")
    pV, pVb = sc.ps([128, 2, TT], F32, "pV")
    pG, pGb = sc.ps([128, 2, TT], F32, "pG")
    pB, pBb = sc.ps([128, 4, 128], F32, "pB")
    pS, pSb = sc.ps([128, 4, 128], F32, "pS")
    pN = [sc.ps([128, 2, TT], F32, "pN") for _ in range(2)]
    tail = Tail(cx, sc, G, l, 1, w["mlstm_w_out"][l], 8, [(pK, pKb), (pV, pVb)], pN)
    identb16, identb16b = make_identity(cx, sc, BF16, "identb")
    NU, NUb = make_tri(cx, sc, BF16, -1.0, "NU")
    U, Ub = make_tri(cx, sc, F32, 1.0, "U")
    ones3, ones3b = sc.sb([128, 4, 128], BF16, "ones3")
    l4h, l4hb = sc.sb([128, 2, 4], BF16, "l4h")
    lbh, lbhb = sc.sb([128, 2, 4, 128], BF16, "lbh")
    OP(cx, "pool", "memset", ones3[:, :, :], 1.0, W=[ones3b])
    nw, nwb = sc.sb([128, D], F32, "nw")
    OP(cx, "sp", "dma_start", out=nw[:, :], in_=w["mlstm_norm"][l].partition_broadcast(128), W=[nwb], chan="wl")
    bi_t, bib = sc.sb([128, 4], F32, "bi")
    bf_t, bfb = sc.sb([128, 4], F32, "bf")
    OP(cx, "sp", "dma_start", out=bi_t[:, :], in_=w["mlstm_b_i"][l].partition_broadcast(128), W=[bib], chan="wl")
    OP(cx, "sp", "dma_start", out=bf_t[:, :], in_=w["mlstm_b_f"][l].partition_broadcast(128), W=[bfb], chan="wl")
    eps_t, epsb = sc.sb([128, 1], F32, "eps")
    OP(cx, "pool", "memset", eps_t[:, :], EPS, W=[epsb])
    hin = [sc.sb([128, KD, TT], BF16, "hin") for _ in range(2)]
    qT, qTb = sc.sb([128, 4, TT], BF16, "qT")
    kT, kTb = sc.sb([128, 4, TT], BF16, "kT")
    vp, vpb = sc.sb([128, 4, 257], BF16, "vp")
    OP(cx, "pool", "memset", vp[:, :, :], 1.0, W=[vpb])
    og, ogb = sc.sb([128, D], F32, "og")
    gi, gib = sc.sb([128, 4], F32, "gi")
    l4, l4b = sc.sb([128, 4], F32, "l4")
    colb, colbb = sc.sb([128, 4], F32, "colb")
    bl, blb = sc.sb([128, 4], F32, "bl")
    wk, wkb = sc.sb([128, 4], F32, "wk")
    r4, r4b = sc.sb([128, 4], F32, "r4")
    msq, msqb = sc.sb([128, 4], F32, "msq")
    lb, lbb = sc.sb([128, 4, 128], F32, "lb")
    eb, ebb = sc.sb([128, 4, 128], F32, "eb")
    DT, DTb = sc.sb([128, 4, 128], F32, "DT")
    AT, ATb = sc.sb([128, 4, 128], BF16, "AT")
    qs, qsb = sc.sb([128, 4, 128], BF16, "qs")
    kts, ktsb = sc.sb([128, 4, 128], BF16, "kts")
    C32, C32b = sc.sb([128, 4, 257], F32, "C32")
    Cbf, Cbfb = sc.sb([128, 4, 257], BF16, "Cbf")
    tmp, tmpb = sc.sb([128, D], F32, "tmp")
    tmp2, tmp2b = sc.sb([128, D], F32, "tmp2")
    junk, junkb = sc.sb([128, 256], F32, "junk")
    hn, hnb = sc.sb([128, D], BF16, "hn")
    ymT, ymTb = sc.sb([128, KD, TT], BF16, "ymT")
    pAf, pKf, pVf, pGf = flat(pA), flat(pK), flat(pV), flat(pG)
    pAT = pAf.bitcast(BF16).rearrange("p (c t) -> p c t", t=128)
    tiles_per_seq = G["S"] // TT
    kscale = 128 ** -0.5

    def load(i):
        hT, hb = hin[i % 2]
        OP(cx, "sp", "dma_start", out=hT[:, :, :], in_=ht_ap(G, i), R=[G["HTb"][i]], W=[hb], chan="ld")
        tail.load_x(i)

    load(0)
    for i in range(G["NT"]):
        hT, hb = hin[i % 2]
        if i + 1 < G["NT"]:
            load(i + 1)
        if i % tiles_per_seq == 0:
            OP(cx, "pool", "memset", C32[:, :, :], 0.0, W=[C32b])
            OP(cx, "pool", "memset", Cbf[:, :, :], 0.0, W=[Cbfb])
        for which, dst, dstb, off in ((0, qT, qTb, 0), (1, kT, kTb, 512)):
            for hp in range(2):
                for hh in range(2):
                    h = hp * 2 + hh
                    for c in range(KD):
                        OP(cx, "pe", "matmul", pA[:, hh, :], win[:, c, off + h * 128:off + (h + 1) * 128], hT[:, c, :],
                           start=(c == 0), stop=(c == KD - 1), R=[winb, hb], W=[pAb])
                if which == 0:
                    OP(cx, "act", "copy", out=dst[:, 2 * hp:2 * hp + 2, :], in_=pA[:, :, :], R=[pAb], W=[dstb])
                else:
                    OP(cx, "act", "mul", dst[:, 2 * hp:2 * hp + 2, :], pA[:, :, :], kscale, R=[pAb], W=[dstb])
        for a in range(TT // 128):
            ts = slice(a * 128, (a + 1) * 128)
            if G.get("dbg_stop", 99) <= 0:
                continue
            for c in range(KD):
                OP(cx, "pe", "matmul", pGf[:, 0:8], hT[:, c, ts], win[:, c, 3072:3080], start=(c == 0), stop=(c == KD - 1),
                   R=[winb, hb], W=[pGb])
            if G.get("dbg_stop", 99) == 0.5:
                continue
            OP(cx, "dve", "tensor_tensor", out=gi[:, :], in0=pGf[:, 0:4], in1=bi_t[:, :], op=ALU.add, R=[pGb, bib], W=[gib])
            OP(cx, "dve", "tensor_tensor", out=l4[:, :], in0=pGf[:, 4:8], in1=bf_t[:, :], op=ALU.add, R=[pGb, bfb], W=[l4b])
            OP(cx, "act", "activation", out=l4[:, :], in_=l4[:, :], func=AF.Exp, scale=-1.0, R=[l4b], W=[l4b])
            OP(cx, "act", "activation", out=l4[:, :], in_=l4[:, :], func=AF.Ln, bias=1.0, R=[l4b], W=[l4b])
            if G.get("dbg_stop", 99) == 0.7:
                continue
            OP(cx, "dve", "tensor_copy", out=l4h[:, 0, :], in_=l4[:, :], R=[l4b], W=[l4hb])
            OP(cx, "dve", "tensor_tensor", out=l4h[:, 1, :], in0=l4[:, :], in1=l4h[:, 0, :], op=ALU.subtract, R=[l4b, l4hb], W=[l4hb])
            for z in range(2):
                OP(cx, "dve", "tensor_tensor", out=lbh[:, z, :, :], in0=ones3[:, :, :],
                   in1=l4h[:, z, :].unsqueeze(2).to_broadcast([128, 4, 128]), op=ALU.mult, R=[ones3b, l4hb], W=[lbhb])
            if G.get("dbg_stop", 99) == 0.8:
                continue
            for z in range(2):
                OP(cx, "pe", "matmul", pGf[:, 8:12], NU[:, :], l4h[:, z, :], start=(z == 0), stop=(z == 1), R=[NUb, l4hb], W=[pGb])
            for h in range(4):
                for z in range(2):
                    OP(cx, "pe", "matmul", pB[:, h, :], lbh[:, z, h, :], NU[:, :], start=(z == 0), stop=(z == 1), R=[lbhb, NUb], W=[pBb])
            if G.get("dbg_stop", 99) == 0.9:
                continue
            OP(cx, "act", "activation", out=eb[:, :, :], in_=pB[:, :, :], func=AF.Exp, R=[pBb], W=[ebb])
            OP(cx, "dve", "tensor_tensor", out=colb[:, :], in0=gi[:, :], in1=pGf[:, 8:12], op=ALU.subtract, R=[gib, pGb], W=[colbb])
            OP(cx, "dve", "tensor_copy", out=bl[:, :], in_=pB[:, :, 127], R=[pBb], W=[blb])
            OP(cx, "dve", "tensor_tensor", out=wk[:, :], in0=colb[:, :], in1=bl[:, :], op=ALU.add, R=[colbb, blb], W=[wkb])
            OP(cx, "act", "activation", out=wk[:, :], in_=wk[:, :], func=AF.Exp, R=[wkb], W=[wkb])
            OP(cx, "dve", "tensor_scalar", out=wk[:, :], in0=wk[:, :], scalar1=kscale, scalar2=None, op0=ALU.mult, R=[wkb], W=[wkb])
            if G.get("dbg_stop", 99) <= 1:
                continue
            for h in range(4):
                OP(cx, "pe", "matmul", pS[:, h, :], kT[:, h, ts], qT[:, h, ts], start=True, stop=True, R=[kTb, qTb], W=[pSb])
            for h in range(4):
                OP(cx, "act", "activation", out=DT[:, h, :], in_=pB[:, h, :], func=AF.Exp, bias=colb[:, h:h + 1], R=[pBb, colbb], W=[DTb])
            OP(cx, "pool", "tensor_tensor", out=DT[:, :, :], in0=DT[:, :, :], in1=U[:, :].unsqueeze(1).to_broadcast([128, 4, 128]),
               op=ALU.mult, R=[DTb, Ub], W=[DTb])
            OP(cx, "dve", "tensor_tensor", out=AT[:, :, :], in0=DT[:, :, :], in1=pS[:, :, :], op=ALU.mult, R=[DTb, pSb], W=[ATb])
            OP(cx, "pool", "tensor_tensor", out=qs[:, :, :], in0=qT[:, :, ts], in1=eb[:, :, :], op=ALU.mult, R=[qTb, ebb], W=[qsb])
            if G.get("dbg_stop", 99) <= 2:
                continue
            for c in range(KD):
                OP(cx, "pe", "matmul", pKf[:, :], hT[:, c, ts], win[:, c, 512:1024], start=(c == 0), stop=(c == KD - 1),
                   R=[winb, hb], W=[pKb])
            OP(cx, "dve", "tensor_tensor", out=kts[:, :, :], in0=pKf.rearrange("p (h d) -> p h d", d=128),
               in1=wk[:, :].unsqueeze(2).to_broadcast([128, 4, 128]), op=ALU.mult, R=[pKb, wkb], W=[ktsb])
            for r in range(2):
                for c in range(KD):
                    OP(cx, "pe", "matmul", pVf[:, :], hT[:, c, ts], win[:, c, 1024 + r * 512:1024 + (r + 1) * 512],
                       start=(c == 0), stop=(c == KD - 1), R=[winb, hb], W=[pVb])
                OP(cx, "act", "copy", out=vp[:, 2 * r:2 * r + 2, 0:256], in_=pVf.rearrange("p (h d) -> p h d", d=256), R=[pVb], W=[vpb])
            for r in range(2):
                for c in range(KD):
                    OP(cx, "pe", "matmul", pVf[:, :], hT[:, c, ts], win[:, c, 2048 + r * 512:2048 + (r + 1) * 512],
                       start=(c == 0), stop=(c == KD - 1), R=[winb, hb], W=[pVb])
                OP(cx, "act", "activation", out=og[:, r * 512:(r + 1) * 512], in_=pVf[:, :], func=AF.Sigmoid, R=[pVb], W=[ogb])
            if G.get("dbg_stop", 99) <= 3:
                continue
            for h in range(4):
                pn, pnb = pN[h % 2]
                pnf = flat(pn)
                OP(cx, "pe", "matmul", pnf[:, 0:257], qs[:, h, :], Cbf[:, h, :], start=True, stop=False, R=[qsb, Cbfb], W=[pnb])
                OP(cx, "pe", "matmul", pnf[:, 0:257], AT[:, h, :], vp[:, h, :], start=False, stop=True, R=[ATb, vpb], W=[pnb])
                OP(cx, "act", "activation", out=r4[:, h:h + 1], in_=pnf[:, 256:257], func=AF.Abs, R=[pnb], W=[r4b])
                OP(cx, "dve", "tensor_scalar", out=r4[:, h:h + 1], in0=r4[:, h:h + 1], scalar1=1.0, scalar2=None, op0=ALU.max,
                   R=[r4b], W=[r4b])
                OP(cx, "dve", "reciprocal", out=r4[:, h:h + 1], in_=r4[:, h:h + 1], R=[r4b], W=[r4b])
                OP(cx, "dve", "tensor_scalar", out=tmp[:, h * 256:(h + 1) * 256], in0=pnf[:, 0:256], scalar1=r4[:, h:h + 1], scalar2=None,
                   op0=ALU.mult, R=[pnb, r4b], W=[tmpb])
                OP(cx, "act", "activation", out=junk[:, :], in_=tmp[:, h * 256:(h + 1) * 256], func=AF.Square, accum_out=msq[:, h:h + 1],
                   R=[tmpb], W=[junkb, msqb])
            OP(cx, "act", "activation", out=msq[:, :], in_=msq[:, :], func=AF.Sqrt, scale=1.0 / 256, bias=eps_t[:, 0:1], R=[msqb, epsb], W=[msqb])
            OP(cx, "dve", "reciprocal", out=msq[:, :], in_=msq[:, :], R=[msqb], W=[msqb])
            for h in range(4):
                OP(cx, "dve", "scalar_tensor_tensor", out=tmp2[:, h * 256:(h + 1) * 256], in0=tmp[:, h * 256:(h + 1) * 256],
                   scalar=msq[:, h:h + 1], in1=nw[:, h * 256:(h + 1) * 256], op0=ALU.mult, op1=ALU.mult, R=[tmpb, msqb, nwb], W=[tmp2b])
            OP(cx, "pool", "tensor_tensor", out=hn[:, :], in0=tmp2[:, :], in1=og[:, :], op=ALU.mult, R=[tmp2b, ogb], W=[hnb])
            if G.get("dbg_stop", 99) <= 4:
                continue
            for h in range(4):
                OP(cx, "pe", "matmul", pAf[:, 0:257], kts[:, h, :], vp[:, h, :], start=True, stop=True, R=[ktsb, vpb], W=[pAb])
                OP(cx, "dve", "scalar_tensor_tensor", out=C32[:, h, :], in0=C32[:, h, :], scalar=eb[:, h, 127:128], in1=pAf[:, 0:257],
                   op0=ALU.mult, op1=ALU.add, R=[C32b, ebb, pAb], W=[C32b])
            OP(cx, "act", "copy", out=Cbf[:, :, :], in_=C32[:, :, :], R=[C32b], W=[Cbfb])
            if G.get("dbg_stop", 99) <= 5:
                continue
            for c in range(KD):
                OP(cx, "pe", "transpose", pAT[:, c, :], hn[:, c * 128:(c + 1) * 128], identb16[:, :], R=[hnb, identb16b], W=[pAb])
            OP(cx, "act", "copy", out=ymT[:, :, ts], in_=pAT[:, :, :], R=[pAb], W=[ymTb])
        tail.run(i, hT, hb, ymT, ymTb)
    sc.close()


def phase_dsa(cx, G, l):
    P = cx.P
    TT = G["TT"]
    S = G["S"]
    w = G["w"]
    n_sel = G["n_sel"]
    KSEL = n_sel // 128
    NIT = 16
    BIG = 1000.0
    sc = Scope(cx)
    wq, wqb = load_w(cx, sc, w["w_in"][l], KD, OFF_AQ, OFF_AQ + 512, "wq")
    wiq, wiqb = load_w(cx, sc, w["w_in"][l], KD, OFF_IQ, OFF_IQ + 256, "wiq")
    wkk = sc.sb([128, KD, 128], BF16, "wkk")
    wik = sc.sb([128, KD, 128], BF16, "wik")
    for dc in (0, 64):
        load_w(cx, sc, w["w_in"][l], KD, OFF_AK, OFF_AK + 64, "", dst=wkk, dcol=dc)
        load_w(cx, sc, w["w_in"][l], KD, OFF_IK, OFF_IK + 64, "", dst=wik, dcol=dc)
    wv, wvb = load_w(cx, sc, w["w_in"][l], KD, OFF_AV, OFF_AV + 64, "wv")
    wiw, wiwb = load_w(cx, sc, w["w_in"][l], KD, OFF_IW, OFF_IW + 4, "wiw")
    pA, pAb = sc.ps([128, 2, TT], F32, "pA")
    pL = [sc.ps([128, 2, TT], F32, "pL") for _ in range(3)]
    pST = [sc.ps([128, 2, TT], F32, "pST") for _ in range(2)]
    pO = [sc.ps([128, 4, 128], F32, "pO") for _ in range(2)]
    tail = Tail(cx, sc, G, l, 2, w["attn_w_out"][l], 4, pST, pL)
    identb, identbb = make_identity(cx, sc, BF16, "identb")
    ident4, ident4b = sc.sb([128, 4, 128], BF16, "ident4")
    bigi4, bigi4b = sc.sb([128, 4, 128], BF16, "bigi4")
    for h in range(4):
        OP(cx, "pool", "tensor_copy", out=ident4[:, h, :], in_=identb[:, :], R=[identbb], W=[ident4b])
    OP(cx, "pool", "tensor_scalar", out=bigi4[:, :, :], in0=ident4[:, :, :], scalar1=BIG, scalar2=None, op0=ALU.mult, R=[ident4b], W=[bigi4b])
    negb, negbb = sc.sb([128, 1], F32, "negb")
    OP(cx, "pool", "memset", negb[:, :], -BIG, W=[negbb])
    NEGU, NEGUb = sc.sb([128, 128], BF16, "NEGU")
    OP(cx, "pool", "memset", NEGU[:, :], -1000.0, W=[NEGUb])
    OP(cx, "pool", "affine_select", out=NEGU[:, :], in_=NEGU[:, :], pattern=[[1, 128]], compare_op=ALU.is_gt, fill=0.0, base=0,
       channel_multiplier=-1, R=[NEGUb], W=[NEGUb])
    LT, LTb = sc.sb([128, 128], BF16, "LT")
    OP(cx, "pool", "memset", LT[:, :], 1.0, W=[LTb])
    OP(cx, "pool", "affine_select", out=LT[:, :], in_=LT[:, :], pattern=[[-1, 128]], compare_op=ALU.is_ge, fill=0.0, base=0,
       channel_multiplier=1, R=[LTb], W=[LTb])
    hin = [sc.sb([128, KD, TT], BF16, "hin") for _ in range(3)]
    qTs = [sc.sb([128, 4, TT], BF16, "qT") for _ in range(2)]
    qiTs = [sc.sb([128, 2, TT], BF16, "qiT") for _ in range(2)]
    kT2s = [sc.sb([128, S], BF16, "kT2") for _ in range(2)]
    kiT2s = [sc.sb([128, S], BF16, "kiT2") for _ in range(2)]
    vpds = [sc.sb([128, S // 128, 65], BF16, "vpd") for _ in range(2)]
    for vpd, vpdb in vpds:
        OP(cx, "pool", "memset", vpd[:, :, :], 1.0, W=[vpdb])
    wf, wfb = sc.sb([128, 4], F32, "wf")
    dg, dgb = sc.sb([128, 4, 128], BF16, "dg")
    Rl = [sc.sb([128, 512], BF16, "Rl") for _ in range(4)]
    scs, scsb = sc.sb([128, S], F32, "scs")
    jnk, jnkb = sc.sb([128, S], BF16, "jnk")
    Mbs = [sc.sb([128, S], BF16, "Mb") for _ in range(2)]
    lo, lob = sc.sb([128, 1], F32, "lo")
    w0, w0b = sc.sb([128, 1], F32, "w0")
    mid, midb = sc.sb([128, 1], F32, "mid")
    cnt, cntb = sc.sb([128, 1], F32, "cnt")
    stp, stpb = sc.sb([128, 1], F32, "stp")
    PT = [sc.sb([128, 512], BF16, "PT") for _ in range(4)]
    rden, rdenb = sc.sb([128, 2, 4], F32, "rden")
    ha, hab = sc.sb([128, 4, 2, 64], BF16, "ha")
    yaTs = [sc.sb([128, 4, TT], BF16, "yaT") for _ in range(2)]
    pAf = flat(pA)
    pAT = pAf.bitcast(BF16).rearrange("p (c t) -> p c t", t=128)
    tiles_per_seq = S // TT
    NT = G["NT"]

    def load_h(i):
        hT, hb = hin[i % 3]
        OP(cx, "sp", "dma_start", out=hT[:, :, :], in_=ht_ap(G, i), R=[G["HTb"][i]], W=[hb], chan="ld")

    def proj(i):
        hT, hb = hin[i % 3]
        qT, qTb = qTs[i % 2]
        qiT, qiTb = qiTs[i % 2]
        sq_ = (i // tiles_per_seq) % 2
        kT2, kT2b = kT2s[sq_]
        kiT2, kiT2b = kiT2s[sq_]
        ti = i % tiles_per_seq
        cs = slice(ti * TT, (ti + 1) * TT)
        for hp in range(2):
            for hh in range(2):
                c4 = hp * 2 + hh
                for c in range(KD):
                    OP(cx, "pe", "matmul", pA[:, hh, :], wq[:, c, c4 * 128:(c4 + 1) * 128], hT[:, c, :], start=(c == 0), stop=(c == KD - 1),
                       R=[wqb, hb], W=[pAb])
            OP(cx, "act", "mul", qT[:, 2 * hp:2 * hp + 2, :], pA[:, :, :], 0.125, R=[pAb], W=[qTb])
        for hh, (wt, wtb) in enumerate((wkk, wik)):
            for c in range(KD):
                OP(cx, "pe", "matmul", pA[:, hh, :], wt[:, c, :], hT[:, c, :], start=(c == 0), stop=(c == KD - 1), R=[wtb, hb], W=[pAb])
        OP(cx, "act", "copy", out=kT2[:, cs], in_=pA[:, 0, :], R=[pAb], W=[kT2b])
        OP(cx, "act", "copy", out=kiT2[:, cs], in_=pA[:, 1, :], R=[pAb], W=[kiT2b])
        for hh in range(2):
            for c in range(KD):
                OP(cx, "pe", "matmul", pA[:, hh, :], wiq[:, c, hh * 128:(hh + 1) * 128], hT[:, c, :], start=(c == 0), stop=(c == KD - 1),
                   R=[wiqb, hb], W=[pAb])
        OP(cx, "act", "copy", out=qiT[:, :, :], in_=pA[:, :, :], R=[pAb], W=[qiTb])

    def prep(i, a):
        hT, hb = hin[i % 3]
        qiT, qiTb = qiTs[i % 2]
        sq_ = (i // tiles_per_seq) % 2
        kiT2, kiT2b = kiT2s[sq_]
        vpd, vpdb = vpds[sq_]
        ti = i % tiles_per_seq
        ts = slice(a * 128, (a + 1) * 128)
        qi = ti * 2 + a
        SV = (qi + 1) * 128
        Mb, Mbb = Mbs[qi % 2]
        for c in range(KD):
            OP(cx, "pe", "matmul", pAf[:, 0:64], hT[:, c, ts], wv[:, c, :], start=(c == 0), stop=(c == KD - 1), R=[wvb, hb], W=[pAb])
        OP(cx, "act", "copy", out=vpd[:, qi, 0:64], in_=pAf[:, 0:64], R=[pAb], W=[vpdb])
        if qi < KSEL:
            if qi > 0:
                OP(cx, "pool", "memset", Mb[:, 0:qi * 128], 1.0, W=[Mbb])
            OP(cx, "pool", "tensor_copy", out=Mb[:, qi * 128:SV], in_=LT[:, :], R=[LTb], W=[Mbb])
            return
        for c in range(KD):
            OP(cx, "pe", "matmul", pAf[:, 64:68], hT[:, c, ts], wiw[:, c, :], start=(c == 0), stop=(c == KD - 1), R=[wiwb, hb], W=[pAb])
        OP(cx, "act", "mul", wf[:, :], pAf[:, 64:68], 1.0 / 16.0, R=[pAb], W=[wfb])
        OP(cx, "dve", "tensor_tensor", out=dg[:, :, :], in0=ident4[:, :, :], in1=wf[:, :].unsqueeze(2).to_broadcast([128, 4, 128]),
           op=ALU.mult, R=[ident4b, wfb], W=[dgb])
        for c0 in range(0, SV, 512):
            cw = min(512, SV - c0)
            for h in range(4):
                half, ch = h % 2, h // 2
                ps_ = slice(half * 64, (half + 1) * 64)
                pl, plb = pL[h % 3]
                OP(cx, "pe", "matmul", flat(pl)[:, 0:cw], qiT[ps_, ch, ts], kiT2[ps_, c0:c0 + cw], start=True, stop=True,
                   R=[qiTb, kiT2b], W=[plb])
                OP(cx, "act", "activation", out=Rl[h][0][:, 0:cw], in_=flat(pl)[:, 0:cw], func=AF.Relu, R=[plb], W=[Rl[h][1]])
            dcol = qi * 128 - c0
            has_diag = 0 <= dcol < cw
            for h in range(4):
                OP(cx, "pe", "matmul", pAf[:, 0:cw], dg[:, h, :], Rl[h][0][:, 0:cw], start=(h == 0), stop=(h == 3 and not has_diag),
                   R=[dgb, Rl[h][1]], W=[pAb])
            if has_diag:
                OP(cx, "pe", "matmul", pAf[:, dcol:dcol + 128], identb[:, :], NEGU[:, :], start=False, stop=True,
                   R=[identbb, NEGUb], W=[pAb])
            OP(cx, "act", "copy", out=scs[:, c0:c0 + cw], in_=pAf[:, 0:cw], R=[pAb], W=[scsb])
        OP(cx, "dve", "tensor_reduce", out=lo[:, :], in_=scs[:, 0:qi * 128], axis=AX.X, op=ALU.min, R=[scsb], W=[lob])
        OP(cx, "dve", "tensor_reduce", out=w0[:, :], in_=scs[:, 0:SV], axis=AX.X, op=ALU.max, R=[scsb], W=[w0b])
        OP(cx, "dve", "tensor_tensor", out=w0[:, :], in0=w0[:, :], in1=lo[:, :], op=ALU.subtract, R=[w0b, lob], W=[w0b])
        for it in range(1, NIT + 1):
            f = 2.0 ** -it
            OP(cx, "dve", "scalar_tensor_tensor", out=mid[:, :], in0=w0[:, :], scalar=f, in1=lo[:, :], op0=ALU.mult, op1=ALU.add,
               R=[w0b, lob], W=[midb])
            OP(cx, "dve", "tensor_scalar", out=jnk[:, 0:SV], in0=scs[:, 0:SV], scalar1=mid[:, 0:1], scalar2=0.0, op0=ALU.is_ge,
               op1=ALU.add, accum_out=cnt[:, 0:1], R=[scsb, midb], W=[jnkb, cntb])
            OP(cx, "dve", "tensor_scalar", out=stp[:, :], in0=cnt[:, :], scalar1=float(n_sel) - 0.5, scalar2=f, op0=ALU.is_ge,
               op1=ALU.mult, R=[cntb], W=[stpb])
            OP(cx, "dve", "scalar_tensor_tensor", out=lo[:, :], in0=stp[:, :], scalar=w0[:, 0:1], in1=lo[:, :], op0=ALU.mult,
               op1=ALU.add, R=[stpb, w0b, lob], W=[lob])
        OP(cx, "dve", "tensor_scalar", out=Mb[:, 0:SV], in0=scs[:, 0:SV], scalar1=lo[:, 0:1], scalar2=None, op0=ALU.is_ge,
           R=[scsb, lob], W=[Mbb])

    def attn(i, a):
        qT, qTb = qTs[i % 2]
        sq_ = (i // tiles_per_seq) % 2
        kT2, kT2b = kT2s[sq_]
        vpd, vpdb = vpds[sq_]
        yaT, yaTb = yaTs[i % 2]
        ti = i % tiles_per_seq
        ts = slice(a * 128, (a + 1) * 128)
        qi = ti * 2 + a
        nk = qi + 1
        Mb, Mbb = Mbs[qi % 2]
        def S_(kt):
            for half in range(2):
                ps_ = slice(half * 64, (half + 1) * 64)
                pst, pstb = pST[half]
                OP(cx, "pe", "matmul", flat(pst)[:, :], kT2[ps_, kt * 128:(kt + 1) * 128], qT[ps_, :, ts], start=True, stop=False,
                   R=[kT2b, qTb], W=[pstb])
                OP(cx, "pe", "matmul", flat(pst)[:, :], Mb[:, kt * 128:(kt + 1) * 128], bigi4[:, :, :], start=False, stop=True,
                   R=[Mbb, bigi4b], W=[pstb])

        def E_(kt):
            for half in range(2):
                pst, pstb = pST[half]
                pt_, ptb_ = PT[(kt % 2) * 2 + half]
                OP(cx, "act", "activation", out=pt_[:, :], in_=flat(pst)[:, :], func=AF.Exp, bias=negb[:, 0:1], R=[pstb, negbb], W=[ptb_])

        def V_(kt):
            for half in range(2):
                pt_, ptb_ = PT[(kt % 2) * 2 + half]
                po, pob = pO[half]
                for c in range(4):
                    OP(cx, "pe", "matmul", po[:, c, 0:65], pt_[:, c * 128:(c + 1) * 128], vpd[:, kt, :], start=(kt == 0 and c == 0),
                       stop=(kt == qi), skip_group_check=True, R=[ptb_, vpdb], W=[pob])

        S_(0)
        for kt in range(nk):
            E_(kt)
            if kt + 1 < nk:
                S_(kt + 1)
            V_(kt)
        for half in range(2):
            po, pob = pO[half]
            OP(cx, "dve", "reciprocal", out=rden[:, half, :], in_=po[:, :, 64], R=[pob], W=[rdenb])
            OP(cx, "dve", "tensor_tensor", out=ha[:, :, half, :], in0=po[:, :, 0:64],
               in1=rden[:, half, :].unsqueeze(2).to_broadcast([128, 4, 64]), op=ALU.mult, R=[pob, rdenb], W=[hab])
        haf = ha[:, :, :, :].rearrange("p c h d -> p (c h d)")
        for c in range(4):
            OP(cx, "pe", "transpose", pAT[:, c, :], haf[:, c * 128:(c + 1) * 128], identb[:, :], R=[hab, identbb], W=[pAb])
        OP(cx, "act", "copy", out=yaT[:, :, ts], in_=pAT[:, 0:4, :], R=[pAb], W=[yaTb])

    steps = [(i, a) for i in range(NT) for a in range(TT // 128)]
    load_h(0)
    if NT > 1:
        load_h(1)
    tail.load_x(0)
    proj(0)
    prep(0, 0)
    for k, (i, a) in enumerate(steps):
        if k + 1 < len(steps):
            i2, a2 = steps[k + 1]
            if a2 == 0:
                if i2 + 1 < NT:
                    load_h(i2 + 1)
                tail.load_x(i2)
                proj(i2)
            prep(i2, a2)
        attn(i, a)
        if a == TT // 128 - 1:
            hT, hb = hin[i % 3]
            tail.run(i, hT, hb, yaTs[i % 2][0], yaTs[i % 2][1])
    sc.close()


WNAMES = ["norm_ffn1", "ffn1_w_gu", "ffn1_w_down", "norm_mix", "w_in", "conv_w", "conv_w_out", "mlstm_b_i", "mlstm_b_f",
          "mlstm_norm", "mlstm_w_out", "attn_w_out", "w_o", "norm_ffn2", "ffn2_w_gu", "ffn2_w_down", "norm_ple",
          "ple_w_gate", "ple_w_proj", "final_norm"]


def build(S, nseq, depth, wshapes, phases=None, n_sel=256, dbg=None):
    nc = bass.Bass("TRN2", target_bir_lowering=False)
    NTC = S * nseq
    TT = 256
    G = {"S": S, "nseq": nseq, "NTC": NTC, "TT": TT, "NT": NTC // TT, "depth": depth, "n_sel": n_sel}
    G.update(dbg or {})
    G["x"] = nc.dram_tensor("x", [NTC, D], F32, kind="ExternalInput").ap()
    G["p"] = nc.dram_tensor("p", [depth, NTC, D_PLE], F32, kind="ExternalInput").ap()
    G["w"] = {k: nc.dram_tensor(k, list(wshapes[k]), F32, kind="ExternalInput").ap() for k in WNAMES}
    G["out"] = nc.dram_tensor("out", [NTC, D], F32, kind="ExternalOutput").ap()
    G["outb"] = Buf("out")
    if G.get("dbg_dump"):
        G["dbgo"] = nc.dram_tensor("dbgo", [128, 4096], F32, kind="ExternalOutput").ap()
    G["XT"] = nc.dram_tensor("XT", [D, NTC], F32).ap()
    G["XTb"] = [Buf("XT%d" % i) for i in range(G["NT"])]
    G["HT"] = nc.dram_tensor("HT", [D, NTC], BF16).ap()
    G["HTb"] = [Buf("HT%d" % i) for i in range(G["NT"])]
    with contextlib.ExitStack() as es:
        cx = Ctx(nc, es)
        phase_in(cx, G)
        for l in range(depth):
            if phases is None or "ffn1" in phases:
                phase_ffn(cx, G, l, 1)
            if phases is None or "conv" in phases or "mlstm" in phases or "dsa" in phases:
                phase_norm(cx, G, l)
            if phases is None or "conv" in phases:
                phase_conv(cx, G, l)
            if phases is None or "mlstm" in phases:
                phase_mlstm(cx, G, l)
            if phases is None or "dsa" in phases:
                phase_dsa(cx, G, l)
            if phases is None or "ffn2" in phases:
                phase_ffn(cx, G, l, 2)
            if phases is None or "ple" in phases:
                phase_ple(cx, G, l)
        phase_out(cx, G)
        cx.P.add("sp", lambda e: e.nop(), R=[G["outb"]], chan=None) if False else None
        cx.P.barrier()
        cx.P.emit()
    return nc


def kernel(**inputs):
    x = np.ascontiguousarray(inputs["x"], dtype=np.float32)
    p = np.ascontiguousarray(inputs["p"], dtype=np.float32)
    B, S, _ = x.shape
    depth = p.shape[0]
    nseq = B // N_CORES
    wshapes = {k: inputs[k].shape for k in WNAMES}
    nc = build(S, nseq, depth, wshapes)
    in_maps = []
    for c in range(N_CORES):
        m = {k: np.ascontiguousarray(inputs[k], dtype=np.float32) for k in WNAMES}
        m["x"] = x[c * nseq:(c + 1) * nseq].reshape(nseq * S, D)
        m["p"] = np.ascontiguousarray(p[:, c * nseq:(c + 1) * nseq].reshape(depth, nseq * S, D_PLE))
        in_maps.append(m)
    res = run_bass_kernel_spmd(nc, in_maps, core_ids=list(range(N_CORES)))
    out = np.concatenate([r["out"].reshape(nseq, S, D) for r in res.results], axis=0)
    return out.astype(np.float32)
```
